# Optimizing a Trainium2 kernel written in Bass

```python
import math
import jax
import jax.numpy as jnp
from jax import lax
import numpy as np

D_MODEL = 1024
BATCH = 16
SEQ = 2048
DEPTH = 4

N_EVEN = (DEPTH + 1) // 2
N_ODD = DEPTH // 2
MIX_W = D_MODEL
A_WIDTH = MIX_W // 2
A_GROUPS = 8
A_GDIM = A_WIDTH // A_GROUPS
CHUNK = 128
B_WIDTH = MIX_W // 2
B_HEADS = 8
B_HDIM = B_WIDTH // B_HEADS
QBLOCK = 128
C_HEADS = 4
C_V = MIX_W // C_HEADS
C_QK = C_V // 2
C_CHUNK = 128
CONV_W = 4
D_FF = 4 * D_MODEL
EV_IN = 2 * A_WIDTH + 3 * B_WIDTH
OD_IN = 2 * C_HEADS * C_QK + 2 * C_HEADS * C_V + 2 * C_HEADS
EPS = 1e-6

kernel_name = 'hybrid_gmlp_stickbreak_mlstm_trunk'


def _rmsnorm(x, g):
    xf = x.astype(jnp.float32)
    ms = jnp.mean(xf * xf, axis=-1, keepdims=True)
    return (xf * lax.rsqrt(ms + EPS) * g.astype(jnp.float32)).astype(x.dtype)


def _layernorm(x, g, b):
    xf = x.astype(jnp.float32)
    mu = jnp.mean(xf, axis=-1, keepdims=True)
    var = jnp.mean(jnp.square(xf - mu), axis=-1, keepdims=True)
    y = (xf - mu) * lax.rsqrt(var + EPS) * g.astype(jnp.float32) + b.astype(jnp.float32)
    return y.astype(x.dtype)


def _stick_breaking(q, k, v):
    seq = q.shape[2]
    scale = q.shape[-1] ** -0.5
    outs = []
    for blk in range(seq // QBLOCK):
        t0 = blk * QBLOCK
        t1 = t0 + QBLOCK
        z = jnp.einsum('bhtd,bhsd->bhts', q[:, :, t0:t1], k[:, :, :t1]) * scale
        mask = jnp.arange(t1)[None, :] < jnp.arange(t0, t1)[:, None]
        log1m = jnp.where(mask, -jax.nn.softplus(z), 0.0)
        suffix = lax.cumsum(log1m, axis=3, reverse=True)
        a = jnp.where(mask, jnp.exp(z + suffix), 0.0)
        outs.append(jnp.einsum('bhts,bhsd->bhtd', a, v[:, :, :t1]))
    return jnp.concatenate(outs, axis=2)


def _even_mixer(h, w_in, w_out, ln_g, ln_b, sg_w, sg_b, qn_g, kn_g):
    bsz, seq, _ = h.shape
    nc = seq // CHUNK
    p = h @ w_in
    za = jax.nn.gelu(p[..., :2 * A_WIDTH])
    zb = p[..., 2 * A_WIDTH:]
    u = za[..., :A_WIDTH].reshape(bsz, nc, CHUNK, A_GROUPS, A_GDIM)
    vg = _layernorm(za[..., A_WIDTH:], ln_g, ln_b).reshape(bsz, nc, CHUNK, A_GROUPS, A_GDIM)
    w_causal = sg_w * jnp.tril(jnp.ones((CHUNK, CHUNK), sg_w.dtype))
    mixed = jnp.einsum('gts,bnsgc->bntgc', w_causal, vg) + sg_b.T[:, :, None]
    y_a = (u * mixed).reshape(bsz, seq, A_WIDTH)
    q = _rmsnorm(zb[..., :B_WIDTH].reshape(bsz, seq, B_HEADS, B_HDIM), qn_g)
    k = _rmsnorm(zb[..., B_WIDTH:2 * B_WIDTH].reshape(bsz, seq, B_HEADS, B_HDIM), kn_g)
    v = zb[..., 2 * B_WIDTH:].reshape(bsz, seq, B_HEADS, B_HDIM)
    to_bhsd = lambda t: t.astype(jnp.float32).transpose(0, 2, 1, 3)
    o = _stick_breaking(to_bhsd(q), to_bhsd(k), to_bhsd(v))
    y_b = o.transpose(0, 2, 1, 3).reshape(bsz, seq, B_WIDTH).astype(h.dtype)
    return jnp.concatenate([y_a, y_b], axis=-1) @ w_out


def _causal_conv(x, w, b):
    ch = x.shape[-1]
    y = lax.conv_general_dilated(x, w[:, None, :], window_strides=(1,),
                                 padding=[(CONV_W - 1, 0)],
                                 dimension_numbers=('NWC', 'WIO', 'NWC'),
                                 feature_group_count=ch)
    return y + b


def _mlstm(q, k, v, i_pre, f_pre):
    bsz, seq, nh, dqk = q.shape
    dv = v.shape[-1]
    nc = seq // C_CHUNK
    f32 = jnp.float32
    q = q.astype(f32)
    k = k.astype(f32) * (dqk ** -0.5)
    v = v.astype(f32)
    logf = jax.nn.log_sigmoid(f_pre.astype(f32))
    logi = i_pre.astype(f32)

    def chunk4(a):
        return a.reshape(bsz, nc, C_CHUNK, nh, a.shape[-1]).transpose(1, 0, 3, 2, 4)

    def chunk3(a):
        return a.reshape(bsz, nc, C_CHUNK, nh).transpose(1, 0, 3, 2)

    tril = jnp.tril(jnp.ones((C_CHUNK, C_CHUNK), bool))

    def step(carry, inp):
        c_st, n_st, m_st = carry
        qc, kc, vc, lf, li = inp
        bcum = jnp.cumsum(lf, axis=-1)
        dmat = jnp.where(tril, bcum[..., :, None] - bcum[..., None, :] + li[..., None, :], -jnp.inf)
        g = bcum + m_st[..., None]
        m_t = jnp.maximum(g, jnp.max(dmat, axis=-1))
        wts = jnp.exp(dmat - m_t[..., None])
        inter = jnp.exp(g - m_t)
        s = jnp.einsum('bhtd,bhsd->bhts', qc, kc) * wts
        num = jnp.einsum('bhts,bhsv->bhtv', s, vc) + inter[..., None] * jnp.einsum('bhtd,bhdv->bhtv', qc, c_st)
        den = jnp.sum(s, axis=-1) + inter * jnp.einsum('bhtd,bhd->bht', qc, n_st)
        h = num / jnp.maximum(jnp.abs(den), jnp.exp(-m_t))[..., None]
        b_last = bcum[..., -1]
        wk = b_last[..., None] - bcum + li
        m_new = jnp.maximum(b_last + m_st, jnp.max(wk, axis=-1))
        decay = jnp.exp(b_last + m_st - m_new)
        wkx = jnp.exp(wk - m_new[..., None])
        c_new = decay[..., None, None] * c_st + jnp.einsum('bhs,bhsd,bhsv->bhdv', wkx, kc, vc)
        n_new = decay[..., None] * n_st + jnp.einsum('bhs,bhsd->bhd', wkx, kc)
        return (c_new, n_new, m_new), h

    init = (jnp.zeros((bsz, nh, dqk, dv), f32), jnp.zeros((bsz, nh, dqk), f32), jnp.zeros((bsz, nh), f32))
    _, hs = lax.scan(step, init, (chunk4(q), chunk4(k), chunk4(v), chunk3(logf), chunk3(logi)))
    return hs.transpose(1, 0, 3, 2, 4).reshape(bsz, seq, nh, dv)


def _odd_mixer(h, w_in, conv_w, conv_b, i_b, f_b, on_g, w_out):
    bsz, seq, _ = h.shape
    p = h @ w_in
    n_qk = 2 * C_HEADS * C_QK
    n_v = C_HEADS * C_V
    qk = jax.nn.silu(_causal_conv(p[..., :n_qk], conv_w, conv_b))
    q = qk[..., :C_HEADS * C_QK].reshape(bsz, seq, C_HEADS, C_QK)
    k = qk[..., C_HEADS * C_QK:].reshape(bsz, seq, C_HEADS, C_QK)
    v = p[..., n_qk:n_qk + n_v].reshape(bsz, seq, C_HEADS, C_V)
    og = p[..., n_qk + n_v:n_qk + 2 * n_v].reshape(bsz, seq, C_HEADS, C_V)
    gates = p[..., n_qk + 2 * n_v:]
    i_pre = gates[..., :C_HEADS] + i_b
    f_pre = gates[..., C_HEADS:] + f_b
    hc = _mlstm(q, k, v, i_pre, f_pre)
    hc = _rmsnorm(hc, on_g.reshape(C_HEADS, C_V)).astype(h.dtype)
    y = (hc * jax.nn.sigmoid(og)).reshape(bsz, seq, MIX_W)
    return y @ w_out


def _sqrelu_mlp(h, w1, w2):
    return jnp.square(jax.nn.relu(h @ w1)) @ w2


def setup_inputs(seed: int = 0) -> dict:
    key = jax.random.key(seed)
    ks = jax.random.split(key, 24)
    nrm = lambda k, shape, s: jax.random.normal(k, shape, jnp.float32) * s
    gain = lambda k, shape: 1.0 + 0.05 * jax.random.normal(k, shape, jnp.float32)
    f_base = jnp.linspace(3.0, 6.0, C_HEADS, dtype=jnp.float32)
    return {
        'x': nrm(ks[0], (BATCH, SEQ, D_MODEL), 1.0),
        'mix_norm_g': gain(ks[1], (DEPTH, D_MODEL)),
        'mlp_norm_g': gain(ks[2], (DEPTH, D_MODEL)),
        'mlp_w1': nrm(ks[3], (DEPTH, D_MODEL, D_FF), D_MODEL ** -0.5),
        'mlp_w2': nrm(ks[4], (DEPTH, D_FF, D_MODEL), D_FF ** -0.5),
        'ev_w_in': nrm(ks[5], (N_EVEN, D_MODEL, EV_IN), D_MODEL ** -0.5),
        'ev_w_out': nrm(ks[6], (N_EVEN, MIX_W, D_MODEL), MIX_W ** -0.5),
        'sg_ln_g': gain(ks[7], (N_EVEN, A_WIDTH)),
        'sg_ln_b': nrm(ks[8], (N_EVEN, A_WIDTH), 0.02),
        'sg_w': nrm(ks[9], (N_EVEN, A_GROUPS, CHUNK, CHUNK), CHUNK ** -0.5),
        'sg_b': 1.0 + nrm(ks[10], (N_EVEN, A_GROUPS, CHUNK), 0.1),
        'sb_q_norm_g': gain(ks[11], (N_EVEN, B_HDIM)),
        'sb_k_norm_g': gain(ks[12], (N_EVEN, B_HDIM)),
        'od_w_in': nrm(ks[13], (N_ODD, D_MODEL, OD_IN), D_MODEL ** -0.5),
        'od_conv_w': nrm(ks[14], (N_ODD, CONV_W, 2 * C_HEADS * C_QK), CONV_W ** -0.5),
        'od_conv_b': nrm(ks[15], (N_ODD, 2 * C_HEADS * C_QK), 0.02),
        'od_i_b': nrm(ks[16], (N_ODD, C_HEADS), 0.1),
        'od_f_b': f_base[None, :] + nrm(ks[17], (N_ODD, C_HEADS), 0.1),
        'od_out_norm_g': gain(ks[18], (N_ODD, MIX_W)),
        'od_w_out': nrm(ks[19], (N_ODD, MIX_W, D_MODEL), MIX_W ** -0.5),
    }


def reference(x, mix_norm_g, mlp_norm_g, mlp_w1, mlp_w2, ev_w_in, ev_w_out,
              sg_ln_g, sg_ln_b, sg_w, sg_b, sb_q_norm_g, sb_k_norm_g,
              od_w_in, od_conv_w, od_conv_b, od_i_b, od_f_b, od_out_norm_g, od_w_out):
    for layer in range(DEPTH):
        h = _rmsnorm(x, mix_norm_g[layer])
        j = layer // 2
        if layer % 2 == 0:
            x = x + _even_mixer(h, ev_w_in[j], ev_w_out[j], sg_ln_g[j], sg_ln_b[j],
                                sg_w[j], sg_b[j], sb_q_norm_g[j], sb_k_norm_g[j])
        else:
            x = x + _odd_mixer(h, od_w_in[j], od_conv_w[j], od_conv_b[j], od_i_b[j],
                               od_f_b[j], od_out_norm_g[j], od_w_out[j])
        h = _rmsnorm(x, mlp_norm_g[layer])
        x = x + _sqrelu_mlp(h, mlp_w1[layer], mlp_w2[layer])
    return x
```

```python
from contextlib import ExitStack
from concourse.bass_utils import run_bass_kernel_spmd
import numpy as np
import concourse.bass as bass
import concourse.mybir as mybir

F32 = mybir.dt.float32
BF16 = mybir.dt.bfloat16
AF = mybir.ActivationFunctionType
ALU = mybir.AluOpType
AX = mybir.AxisListType


_UID = [0]


def uname(name):
    _UID[0] += 1
    return "%s_%d" % (name, _UID[0])


class Buf:
    __slots__ = ("name", "w", "r", "sem", "semval", "persist")

    def __init__(self, name, fence, persist=False):
        self.persist = persist
        self.name = name
        self.w = None
        self.r = list(fence)
        self.sem = None
        self.semval = 0


class EngState:
    def __init__(self, name, obj, sem):
        self.name = name
        self.obj = obj
        self.sem = sem
        self.count = 0
        self.know = {}
        self.last = None


class Sched:
    def __init__(self, nc, stack):
        self.nc = nc
        self.stack = stack
        self.E = {}
        for name, obj in (("pe", nc.tensor), ("act", nc.scalar), ("dve", nc.vector),
                          ("pool", nc.gpsimd), ("sp", nc.sync)):
            sem = stack.enter_context(nc.semaphore("sem_" + name))
            self.E[name] = EngState(name, obj, sem)
        self.fence_recs = []
        self.dma_recs = []
        self.pending_pe = []
        self.sem_pool = []
        self.phase_bufs = []
        self.nsem = 0
        self.nwaits = 0
        self.nops = 0

    def buf(self, name, persist=False):
        return Buf(name, self.fence_recs, persist)

    def bufs(self, name, n, persist=False):
        return [Buf("%s%d" % (name, i), self.fence_recs, persist) for i in range(n)]

    def _waits(self, eng, reads, writes):
        st = self.E[eng]
        deps = []
        for b in reads:
            if b.w is not None:
                deps.append(b.w)
        for b in writes:
            if b.w is not None:
                deps.append(b.w)
            deps.extend(b.r)
        pesem = self.E["pe"].sem
        seen = set()
        for rec in deps:
            if id(rec) in seen:
                continue
            seen.add(id(rec))
            s, v, clk = rec
            if eng == "pe" and s is pesem:
                continue
            assert v is not None, "dependency on unsignaled PE op"
            if st.know.get(s, 0) >= v:
                continue
            st.obj.wait_ge(s, v)
            self.nwaits += 1
            for ks, kv in clk.items():
                if st.know.get(ks, 0) < kv:
                    st.know[ks] = kv

    def op(self, eng, fn, reads=(), writes=(), signal=True):
        st = self.E[eng]
        self._waits(eng, reads, writes)
        ins = fn(st.obj)
        self.nops += 1
        if signal:
            st.count += 1
            ins.then_inc(st.sem, 1)
            clk = dict(st.know)
            clk[st.sem] = st.count
            rec = [st.sem, st.count, clk]
            if eng == "pe":
                for p in self.pending_pe:
                    p[1] = st.count
                    p[2] = clk
                self.pending_pe = []
            st.last = rec
        else:
            assert eng == "pe"
            rec = [st.sem, None, None]
            self.pending_pe.append(rec)
        for b in reads:
            b.r.append(rec)
        for b in writes:
            b.w = rec
            b.r = []
        return ins

    def dma(self, q, out, in_, reads=(), writes=(), **kw):
        st = self.E[q]
        self._waits(q, reads, writes)
        owner = writes[0] if writes else reads[0]
        if owner.sem is None:
            if self.sem_pool and not owner.persist:
                owner.sem, owner.semval = self.sem_pool.pop()
            else:
                self.nsem += 1
                owner.sem = self.stack.enter_context(self.nc.semaphore("dsem%d" % self.nsem))
            if not owner.persist:
                self.phase_bufs.append(owner)
        owner.semval += 16
        ins = st.obj.dma_start(out=out, in_=in_, **kw)
        ins.then_inc(owner.sem, 16)
        clk = dict(st.know)
        clk[owner.sem] = owner.semval
        rec = [owner.sem, owner.semval, clk]
        self.dma_recs.append(rec)
        for b in reads:
            b.r.append(rec)
        for b in writes:
            b.w = rec
            b.r = []
        return ins

    def fence(self):
        sp = self.E["sp"]
        for rec in self.dma_recs:
            s, v, clk = rec
            if sp.know.get(s, 0) >= v:
                continue
            sp.obj.wait_ge(s, v)
            for ks, kv in clk.items():
                if sp.know.get(ks, 0) < kv:
                    sp.know[ks] = kv
        self.dma_recs = []
        for b in self.phase_bufs:
            self.sem_pool.append((b.sem, b.semval))
            b.sem = None
        self.phase_bufs = []
        ins = sp.obj.nop()
        sp.count += 1
        ins.then_inc(sp.sem, 1)
        clk = dict(sp.know)
        clk[sp.sem] = sp.count
        sp.last = [sp.sem, sp.count, clk]
        assert not self.pending_pe
        recs = []
        for name, st in self.E.items():
            if st.last is not None:
                recs.append(st.last)
        self.fence_recs = recs

    def finish(self):
        self.fence()


EPS = 1e-6

def rstd_ops(S, st, st_b, n, inv_n):
    S.op("dve", lambda e: e.tensor_scalar(out=st[:, n:2 * n], in0=st[:, 0:n], scalar1=inv_n, scalar2=EPS,
                                          op0=ALU.mult, op1=ALU.add), reads=[st_b], writes=[st_b])
    S.op("act", lambda e: e.sqrt(out=st[:, 2 * n:3 * n], in_=st[:, n:2 * n]), reads=[st_b], writes=[st_b])
    S.op("dve", lambda e: e.reciprocal(out=st[:, 3 * n:4 * n], in_=st[:, 2 * n:3 * n]), reads=[st_b], writes=[st_b])


def norm_transpose(S, C, X, Xb, ti, gbc, gbc_b, tmp, k, pb, out_ap, out_b):
    ps, psb, identb = C["ps"], C["psb"], C["identb"]
    junk, junk_b, xs, xs_b, st, st_b = tmp
    S.op("act", lambda e: e.activation(out=junk[:, :], in_=X[:, ti, :], func=AF.Square, accum_out=st[k][:, 0:1]),
         reads=[Xb[ti]], writes=[junk_b, st_b[k]])
    rstd_ops(S, st[k], st_b[k], 1, 1.0 / 1024)
    S.op("dve", lambda e: e.scalar_tensor_tensor(out=xs[k][:, :], in0=X[:, ti, :], scalar=st[k][:, 3:4],
                                                 in1=gbc[:, :], op0=ALU.mult, op1=ALU.mult),
         reads=[Xb[ti], st_b[k], gbc_b], writes=[xs_b[k]])
    pT = ps[pb][:, :].bitcast(BF16)
    for c in range(8):
        S.op("pe", lambda e: e.transpose(out=pT[:, c * 128:(c + 1) * 128], in_=xs[k][:, c * 128:(c + 1) * 128],
                                         identity=identb[:, :]),
             reads=[xs_b[k], C["cb"]], writes=[psb[pb]], signal=(c == 7))
    S.op("act", lambda e: e.activation(out=out_ap, in_=pT.rearrange("p (c t) -> p c t", c=8), func=AF.Copy),
         reads=[psb[pb]], writes=[out_b])


def even_mixer(S, nc, C, X, Xb, NT, P):
    ps, psb = C["ps"], C["psb"]
    ident, identb = C["ident"], C["identb"]
    T = NT * 128
    NQB = T // 512
    with ExitStack() as es:
        def sb(name, shape, dt):
            return es.enter_context(nc.sbuf_tensor(uname(name), shape, dt))
        yTa = sb("e_yTa", [128, 4, T], BF16); yT_b = [[S.buf("yT") for _ in range(NT)] for _ in range(8)]
        qT = sb("e_qT", [128, 4, T], BF16); qT_b = S.bufs("qT", NT)
        kT = sb("e_kT", [128, 4, T], BF16); kT_b = S.bufs("kT", NT)
        v = sb("e_v", [128, NT, 512], BF16); v_b = S.bufs("v", NT)
        woutv = P["w_out"].rearrange("(kc p) d -> p kc d", p=128)
        with ExitStack() as es1:
            def sb1(name, shape, dt):
                return es1.enter_context(nc.sbuf_tensor(uname(name), shape, dt))
            win = sb1("e_win", [128, 8, 2560], BF16); win_b = S.bufs("win", 5)
            winv = P["w_in"].rearrange("(kc p) f -> p kc f", p=128)
            for cb in range(5):
                S.dma("pool", win[:, :, cb * 512:(cb + 1) * 512], winv[:, :, cb * 512:(cb + 1) * 512], writes=[win_b[cb]])
            gbc = sb1("e_gbc", [128, 1024], F32); gbc_b = S.buf("gbc")
            lng = sb1("e_lng", [128, 512], F32); lng_b = S.buf("lng")
            lnb = sb1("e_lnb", [128, 512], F32); lnb_b = S.buf("lnb")
            qg = sb1("e_qg", [128, 64], F32); qg_b = S.buf("qg")
            kg = sb1("e_kg", [128, 64], F32); kg_b = S.buf("kg")
            sgb = sb1("e_sgb", [128, 8], F32); sgb_b = S.buf("sgb")
            sgw = sb1("e_sgw", [128, 8, 128], BF16); sgw_b = S.buf("sgw")
            wcT = sb1("e_wcT", [128, 8, 128], BF16); wcT_b = S.buf("wcT")
            S.dma("sp", gbc[:, :], P["g"].partition_broadcast(128), writes=[gbc_b])
            S.dma("sp", lng[:, :], P["ln_g"].partition_broadcast(128), writes=[lng_b])
            S.dma("sp", lnb[:, :], P["ln_b"].partition_broadcast(128), writes=[lnb_b])
            S.dma("sp", qg[:, :], P["qg"].partition_broadcast(128), writes=[qg_b])
            S.dma("sp", kg[:, :], P["kg"].partition_broadcast(128), writes=[kg_b])
            S.dma("sp", sgb[:, :], P["sg_b"].rearrange("g t -> t g"), writes=[sgb_b], allow_slow_non_contiguous=True)
            S.dma("pool", sgw[:, :, :], P["sg_w"].rearrange("g t s -> t g s"), writes=[sgw_b])
            S.op("dve", lambda e: e.tensor_scalar(out=qg[:, :], in0=qg[:, :], scalar1=0.125, scalar2=None, op0=ALU.mult),
                 reads=[qg_b], writes=[qg_b])
            S.op("pool", lambda e: e.affine_select(out=sgw[:, :, :], in_=sgw[:, :, :], pattern=[[0, 8], [-1, 128]],
                                                   compare_op=ALU.is_ge, fill=0.0, base=0, channel_multiplier=1),
                 reads=[sgw_b], writes=[sgw_b])
            for half in range(2):
                for gi in range(4):
                    g_ = half * 4 + gi
                    S.op("pe", lambda e: e.transpose(out=ps[half][:, :].bitcast(BF16)[:, gi * 128:(gi + 1) * 128], in_=sgw[:, g_, :],
                                                     identity=identb[:, :]),
                         reads=[sgw_b, C["cb"]], writes=[psb[half]], signal=(gi == 3))
                S.op("act", lambda e: e.activation(out=wcT[:, half * 4:(half + 1) * 4, :],
                                                   in_=ps[half][:, :].bitcast(BF16)[:, 0:512].rearrange("p (g t) -> p g t", g=4), func=AF.Copy),
                     reads=[psb[half]], writes=[wcT_b])
            junk = sb1("e_junk", [128, 1024], BF16); junk_b = S.buf("junk")
            xs = [sb1("e_xs%d" % i, [128, 1024], BF16) for i in range(1)]; xs_b = S.bufs("xs", 1)
            st = [sb1("e_st%d" % i, [128, 4], F32) for i in range(1)]; st_b = S.bufs("st", 1)
            tmpn = (junk, junk_b, xs, xs_b, st, st_b)
            hTt = [sb1("e_hTt%d" % i, [128, 8, 128], BF16) for i in range(1)]; hTt_b = S.bufs("hTt", 1)
            u = [sb1("e_u%d" % i, [128, 512], BF16) for i in range(1)]; u_b = S.bufs("u", 1)
            vg = [sb1("e_vg%d" % i, [128, 512], F32) for i in range(1)]; vg_b = S.bufs("vg", 1)
            vgn = [sb1("e_vgn%d" % i, [128, 512], BF16) for i in range(1)]; vgn_b = S.bufs("vgn", 1)
            lst = [sb1("e_lst%d" % i, [128, 8], F32) for i in range(1)]; lst_b = S.bufs("lst", 1)
            ya = [sb1("e_ya%d" % i, [128, 512], F32) for i in range(1)]; ya_b = S.bufs("ya", 1)
            yab = [sb1("e_yab%d" % i, [128, 512], BF16) for i in range(1)]; yab_b = S.bufs("yab", 1)
            sq = [sb1("e_sq%d" % i, [128, 512], F32) for i in range(1)]; sq_b = S.bufs("sq", 1)
            qst = [sb1("e_qst%d" % i, [128, 32], F32) for i in range(1)]; qst_b = S.bufs("qst", 1)
            qn = [sb1("e_qn%d" % i, [128, 512], BF16) for i in range(1)]; qn_b = S.bufs("qn", 1)
            for i in range(NT):
                k = 0
                norm_transpose(S, C, X, Xb, i, gbc, gbc_b, tmpn, 0, i % 2, hTt[k][:, :, :], hTt_b[k])
                for cb in range(5):
                    for kc in range(8):
                        S.op("pe", lambda e: e.matmul(ps[2 + cb][:, :], hTt[k][:, kc, :], win[:, kc, cb * 512:(cb + 1) * 512],
                                                      start=(kc == 0), stop=(kc == 7)),
                             reads=[hTt_b[k], win_b[cb]], writes=[psb[2 + cb]], signal=(kc == 7))
                S.op("act", lambda e: e.activation(out=u[k][:, :], in_=ps[2][:, :], func=AF.Gelu_apprx_tanh),
                     reads=[psb[2]], writes=[u_b[k]])
                S.op("act", lambda e: e.activation(out=vg[k][:, :], in_=ps[3][:, :], func=AF.Gelu_apprx_tanh,
                                                   accum_out=lst[k][:, 0:1]),
                     reads=[psb[3]], writes=[vg_b[k], lst_b[k]])
                S.op("act", lambda e: e.activation(out=junk[:, 0:512], in_=vg[k][:, :], func=AF.Square,
                                                   accum_out=lst[k][:, 1:2]),
                     reads=[vg_b[k], lst_b[k]], writes=[junk_b, lst_b[k]])
                L = lst[k]; Lb = lst_b[k]
                S.op("dve", lambda e: e.tensor_scalar(out=L[:, 2:4], in0=L[:, 0:2], scalar1=1.0 / 512, scalar2=None,
                                                      op0=ALU.mult), reads=[Lb], writes=[Lb])
                S.op("dve", lambda e: e.tensor_tensor(out=L[:, 4:5], in0=L[:, 2:3], in1=L[:, 2:3], op=ALU.mult),
                     reads=[Lb], writes=[Lb])
                S.op("dve", lambda e: e.scalar_tensor_tensor(out=L[:, 5:6], in0=L[:, 3:4], scalar=EPS, in1=L[:, 4:5],
                                                             op0=ALU.add, op1=ALU.subtract), reads=[Lb], writes=[Lb])
                S.op("act", lambda e: e.sqrt(out=L[:, 6:7], in_=L[:, 5:6]), reads=[Lb], writes=[Lb])
                S.op("dve", lambda e: e.reciprocal(out=L[:, 7:8], in_=L[:, 6:7]), reads=[Lb], writes=[Lb])
                S.op("dve", lambda e: e.scalar_tensor_tensor(out=L[:, 4:5], in0=L[:, 2:3], scalar=-1.0, in1=L[:, 7:8],
                                                             op0=ALU.mult, op1=ALU.mult), reads=[Lb], writes=[Lb])
                S.op("act", lambda e: e.activation(out=vg[k][:, :], in_=vg[k][:, :], func=AF.Identity,
                                                   bias=L[:, 4:5], scale=L[:, 7:8]),
                     reads=[vg_b[k], Lb], writes=[vg_b[k]])
                S.op("dve", lambda e: e.tensor_tensor(out=vg[k][:, :], in0=vg[k][:, :], in1=lng[:, :], op=ALU.mult),
                     reads=[vg_b[k], lng_b], writes=[vg_b[k]])
                S.op("dve", lambda e: e.tensor_tensor(out=vgn[k][:, :], in0=vg[k][:, :], in1=lnb[:, :], op=ALU.add),
                     reads=[vg_b[k], lnb_b], writes=[vgn_b[k]])
                for g_ in range(8):
                    S.op("pe", lambda e: e.matmul(ps[7][:, g_ * 64:(g_ + 1) * 64], wcT[:, g_, :], vgn[k][:, g_ * 64:(g_ + 1) * 64],
                                                  start=True, stop=True),
                         reads=[wcT_b, vgn_b[k]], writes=[psb[7]], signal=(g_ == 7))
                S.op("dve", lambda e: e.tensor_tensor(out=ya[k][:, :].rearrange("p (g c) -> p g c", g=8),
                                                      in0=ps[7][:, :].rearrange("p (g c) -> p g c", g=8),
                                                      in1=sgb[:, :].unsqueeze(2).to_broadcast([128, 8, 64]), op=ALU.add),
                     reads=[psb[7], sgb_b], writes=[ya_b[k]])
                S.op("dve", lambda e: e.tensor_tensor(out=yab[k][:, :], in0=ya[k][:, :], in1=u[k][:, :], op=ALU.mult),
                     reads=[ya_b[k], u_b[k]], writes=[yab_b[k]])
                pT = ps[7][:, :].bitcast(BF16)
                for c in range(4):
                    S.op("pe", lambda e: e.transpose(out=pT[:, c * 128:(c + 1) * 128], in_=yab[k][:, c * 128:(c + 1) * 128],
                                                     identity=identb[:, :]),
                         reads=[yab_b[k]], writes=[psb[7]], signal=(c == 3))
                S.op("act", lambda e: e.activation(out=yTa[:, 0:4, i * 128:(i + 1) * 128],
                                                   in_=pT[:, 0:512].rearrange("p (c t) -> p c t", c=4), func=AF.Copy),
                     reads=[psb[7]], writes=[yT_b[c][i] for c in range(4)])
                for which, bank, gt, gt_b, dstT, dstT_b in ((0, 4, qg, qg_b, qT, qT_b), (1, 5, kg, kg_b, kT, kT_b)):
                    kk = 0
                    Q = qst[kk]; Qb = qst_b[kk]
                    S.op("act", lambda e: e.activation(out=sq[kk][:, :], in_=ps[bank][:, :], func=AF.Square),
                         reads=[psb[bank]], writes=[sq_b[kk]])
                    S.op("dve", lambda e: e.tensor_reduce(out=Q[:, 0:8], in_=sq[kk][:, :].rearrange("p (h d) -> p h d", h=8),
                                                          axis=AX.X, op=ALU.add), reads=[sq_b[kk]], writes=[Qb])
                    rstd_ops(S, Q, Qb, 8, 1.0 / 64)
                    S.op("dve", lambda e: e.tensor_tensor(out=sq[kk][:, :].rearrange("p (h d) -> p h d", h=8),
                                                          in0=ps[bank][:, :].rearrange("p (h d) -> p h d", h=8),
                                                          in1=Q[:, 24:32].unsqueeze(2).to_broadcast([128, 8, 64]), op=ALU.mult),
                         reads=[psb[bank], Qb, sq_b[kk]], writes=[sq_b[kk]])
                    S.op("dve", lambda e: e.tensor_tensor(out=qn[kk][:, :].rearrange("p (h d) -> p h d", h=8),
                                                          in0=sq[kk][:, :].rearrange("p (h d) -> p h d", h=8),
                                                          in1=gt[:, :].unsqueeze(1).to_broadcast([128, 8, 64]), op=ALU.mult),
                         reads=[sq_b[kk], gt_b], writes=[qn_b[kk]])
                    pT2 = ps[bank][:, :].bitcast(BF16)
                    for c in range(4):
                        S.op("pe", lambda e: e.transpose(out=pT2[:, c * 128:(c + 1) * 128], in_=qn[kk][:, c * 128:(c + 1) * 128],
                                                         identity=identb[:, :]),
                             reads=[qn_b[kk]], writes=[psb[bank]], signal=(c == 3))
                    S.op("act", lambda e: e.activation(out=dstT[:, :, i * 128:(i + 1) * 128],
                                                       in_=pT2[:, 0:512].rearrange("p (c t) -> p c t", c=4), func=AF.Copy),
                         reads=[psb[bank]], writes=[dstT_b[i]])
                S.op("act", lambda e: e.activation(out=v[:, i, :], in_=ps[6][:, :], func=AF.Copy),
                     reads=[psb[6]], writes=[v_b[i]])
            S.fence()
        with ExitStack() as es2:
            def sb2(name, shape, dt):
                return es2.enter_context(nc.sbuf_tensor(uname(name), shape, dt))
            yTb = sb2("e_yTb", [128, 4, T], BF16)
            wout = sb2("e_wout", [128, 8, 1024], BF16); wout_b = S.bufs("wout", 2)
            for dh in range(2):
                S.dma("pool", wout[:, :, dh * 512:(dh + 1) * 512], woutv[:, :, dh * 512:(dh + 1) * 512], writes=[wout_b[dh]])
            e_sb = [sb2("a_e%d" % i, [128, 512], BF16) for i in range(2)]; e_b = S.bufs("e", 2)
            l_sb = [sb2("a_l%d" % i, [128, 512], BF16) for i in range(2)]; l_b = S.bufs("l", 2)
            x_sb = [sb2("a_x%d" % i, [128, 512], BF16) for i in range(2)]; x_b = S.bufs("x", 2)
            a_sb = [sb2("a_a%d" % i, [128, 512], BF16) for i in range(2)]; a_b = S.bufs("a", 2)
            negtri, negcmp, masklt = C["negtri"], C["negcmp"], C["masklt"]
            cbuf = C["cb"]
            pcount = 0
            for qb in range(NQB):
                for hp in range(4):
                    psO = 6 + (pcount % 2); pcount += 1
                    items = [(j, hh) for j in range(4 * qb + 3, -1, -1) for hh in range(2)]
                    jfirst = 4 * qb + 3

                    def stageA(idx):
                        j, hh = items[idx]
                        par = idx % 2
                        po = hh * 64
                        c0 = max(0, (j - 4 * qb)) * 128
                        zb = par
                        S.op("pe", lambda e: e.matmul(ps[zb][:, c0:512], kT[po:po + 64, hp, j * 128:(j + 1) * 128],
                                                      qT[po:po + 64, hp, qb * 512 + c0:(qb + 1) * 512], start=True, stop=True),
                             reads=[kT_b[j]] + qT_b[4 * qb:4 * qb + 4], writes=[psb[zb]])
                        S.op("act", lambda e: e.activation(out=e_sb[par][:, c0:512], in_=ps[zb][:, c0:512], func=AF.Exp),
                             reads=[psb[zb]], writes=[e_b[par]])
                        if j >= 4 * qb:
                            S.op("dve", lambda e: e.tensor_tensor(out=e_sb[par][:, c0:c0 + 128], in0=e_sb[par][:, c0:c0 + 128],
                                                                  in1=masklt[:, :], op=ALU.mult),
                                 reads=[e_b[par], cbuf], writes=[e_b[par]])
                        S.op("act", lambda e: e.activation(out=l_sb[par][:, c0:512], in_=e_sb[par][:, c0:512], func=AF.Ln,
                                                           bias=1.0),
                             reads=[e_b[par]], writes=[l_b[par]])

                    def stageB(idx):
                        j, hh = items[idx]
                        par = idx % 2
                        po = hh * 64
                        h = hp * 2 + hh
                        c0 = max(0, (j - 4 * qb)) * 128
                        sbk = 2 + hh
                        S.op("pe", lambda e: e.matmul(ps[sbk][:, c0:512], negtri[:, :], l_sb[par][:, c0:512],
                                                      start=(j == jfirst), stop=False, skip_group_check=True),
                             reads=[l_b[par], cbuf], writes=[psb[sbk]])
                        S.op("act", lambda e: e.activation(out=x_sb[par][:, c0:512], in_=ps[sbk][:, c0:512], func=AF.Exp),
                             reads=[psb[sbk]], writes=[x_b[par]])
                        if j > 0:
                            S.op("pe", lambda e: e.matmul(ps[sbk][:, c0:512], negcmp[:, :], l_sb[par][:, c0:512],
                                                          start=False, stop=(j == 0), skip_group_check=True),
                                 reads=[l_b[par], cbuf], writes=[psb[sbk]])
                        S.op("dve", lambda e: e.tensor_tensor(out=a_sb[par][:, c0:512], in0=e_sb[par][:, c0:512],
                                                              in1=x_sb[par][:, c0:512], op=ALU.mult),
                             reads=[e_b[par], x_b[par]], writes=[a_b[par]])
                        S.op("pe", lambda e: e.matmul(ps[psO][po:po + 64, c0:512], v[:, j, h * 64:(h + 1) * 64],
                                                      a_sb[par][:, c0:512], start=(j == jfirst), stop=(j == 0),
                                                      skip_group_check=True),
                             reads=[v_b[j], a_b[par]], writes=[psb[psO]])

                    stageA(0)
                    for idx in range(len(items)):
                        if idx + 1 < len(items):
                            stageA(idx + 1)
                        stageB(idx)
                    S.op("act", lambda e: e.activation(out=yTb[:, hp, qb * 512:(qb + 1) * 512], in_=ps[psO][:, :],
                                                       func=AF.Copy),
                         reads=[psb[psO]], writes=[yT_b[4 + hp][4 * qb + t] for t in range(4)])
            cnt = 0
            for i in range(NT):
                for dh in range(2):
                    pb = cnt % 4; cnt += 1
                    for kc in range(8):
                        lhs = yTa[:, kc, i * 128:(i + 1) * 128] if kc < 4 else yTb[:, kc - 4, i * 128:(i + 1) * 128]
                        S.op("pe", lambda e: e.matmul(ps[pb][:, :], lhs,
                                                      wout[:, kc, dh * 512:(dh + 1) * 512], start=(kc == 0), stop=(kc == 7)),
                             reads=[yT_b[kc][i], wout_b[dh]], writes=[psb[pb]], signal=(kc == 7))
                    S.op("dve", lambda e: e.tensor_tensor(out=X[:, i, dh * 512:(dh + 1) * 512], in0=ps[pb][:, :],
                                                          in1=X[:, i, dh * 512:(dh + 1) * 512], op=ALU.add),
                         reads=[psb[pb], Xb[i]], writes=[Xb[i]])
            S.fence()


def make_consts(S, nc, es, C):
    ident = es.enter_context(nc.sbuf_tensor("ident", [128, 128], F32))
    identb = es.enter_context(nc.sbuf_tensor("identb", [128, 128], BF16))
    tmp = es.enter_context(nc.sbuf_tensor("ctmp", [128, 128], F32))
    negtri = es.enter_context(nc.sbuf_tensor("negtri", [128, 128], BF16))
    negcmp = es.enter_context(nc.sbuf_tensor("negcmp", [128, 128], BF16))
    masklt = es.enter_context(nc.sbuf_tensor("masklt", [128, 128], BF16))
    maskle = es.enter_context(nc.sbuf_tensor("maskle", [128, 128], BF16))
    onesf = es.enter_context(nc.sbuf_tensor("onesf", [128, 128], F32))
    zerosf = es.enter_context(nc.sbuf_tensor("zerosf", [128, 128], F32))
    cb = S.buf("consts")
    S.op("pool", lambda e: e.memset(onesf[:, :], 1.0), writes=[cb])
    S.op("pool", lambda e: e.memset(zerosf[:, :], 0.0), writes=[cb])
    C.update(onesf=onesf, zerosf=zerosf)
    C.update(ident=ident, identb=identb, negtri=negtri, negcmp=negcmp, masklt=masklt, maskle=maskle, cb=cb)

    def sel(out, val, pattern, cm, op, base=0):
        S.op("pool", lambda e: e.memset(tmp[:, :], val), reads=[cb], writes=[cb])
        S.op("pool", lambda e: e.affine_select(out=tmp[:, :], in_=tmp[:, :], pattern=pattern, compare_op=op, fill=0.0,
                                               base=base, channel_multiplier=cm), reads=[cb], writes=[cb])
        S.op("pool", lambda e: e.tensor_copy(out=out[:, :], in_=tmp[:, :]), reads=[cb], writes=[cb])
    sel(ident, 1.0, [[-1, 128]], 1, ALU.is_equal)
    sel(identb, 1.0, [[-1, 128]], 1, ALU.is_equal)
    sel(negtri, -1.0, [[-1, 128]], 1, ALU.is_ge)
    sel(negcmp, -1.0, [[1, 128]], -1, ALU.is_gt)
    sel(masklt, 1.0, [[1, 128]], -1, ALU.is_gt)
    sel(maskle, 1.0, [[1, 128]], -1, ALU.is_ge)


def odd_mixer(S, nc, C, X, Xb, NT, P, gs_dram):
    ps, psb = C["ps"], C["psb"]
    ident, identb, onesf, zerosf, maskle, cbuf = C["ident"], C["identb"], C["onesf"], C["zerosf"], C["maskle"], C["cb"]
    T = NT * 128
    NQB = T // 512
    NP = 4 * NT
    KS = 128 ** -0.5
    with ExitStack() as es:
        def sb(name, shape, dt):
            return es.enter_context(nc.sbuf_tensor(uname(name), shape, dt))
        hT = sb("o_hT", [128, 8, T], BF16); hT_b = S.bufs("hT", NT)
        wout = sb("o_wout", [128, 8, 1024], BF16); wout_b = S.bufs("wout", 2)
        woutv = P["w_out"].rearrange("(kc p) d -> p kc d", p=128)
        winv = P["w_in"].rearrange("(kc p) f -> p kc f", p=128)
        ongb = sb("o_ong", [128, 1024], F32); ong_b = S.buf("ong")
        cw = sb("o_cw", [128, 8, 4], F32); cw_b = S.buf("cw")
        cbias = sb("o_cb", [128, 8], F32); cbias_b = S.buf("cbias")
        TS = sb("o_TS", [128, 4 * NP], F32); TS_b = S.buf("TS")
        DEC = sb("o_DEC", [128, NP], F32); DEC_b = S.buf("DEC")
        wg = sb("o_wg", [128, 8, 8], BF16); wg_b = S.buf("wg")
        S.dma("pool", wg[:, :, :], winv[:, :, 3072:3080], writes=[wg_b])
        for dh in range(2):
            S.dma("pool", wout[:, :, dh * 512:(dh + 1) * 512], woutv[:, :, dh * 512:(dh + 1) * 512], writes=[wout_b[dh]])
        S.dma("sp", ongb[:, :], P["on_g"].partition_broadcast(128), writes=[ong_b])
        for k_ in range(4):
            S.dma("sp", cw[:, :, k_], P["conv_w"][k_].rearrange("(i p) -> p i", p=128), writes=[cw_b], allow_slow_non_contiguous=True)
        S.dma("sp", cbias[:, :], P["conv_b"].rearrange("(i p) -> p i", p=128), writes=[cbias_b], allow_slow_non_contiguous=True)
        with ExitStack() as es1:
            def sb1(name, shape, dt):
                return es1.enter_context(nc.sbuf_tensor(uname(name), shape, dt))
            gbc = sb1("o_gbc", [128, 1024], F32); gbc_b = S.buf("gbc")
            S.dma("sp", gbc[:, :], P["g"].partition_broadcast(128), writes=[gbc_b])
            junk = sb1("o_junk", [128, 1024], BF16); junk_b = S.buf("junk")
            xs = [sb1("o_xs%d" % i, [128, 1024], BF16) for i in range(2)]; xs_b = S.bufs("xs", 2)
            st = [sb1("o_st%d" % i, [128, 4], F32) for i in range(2)]; st_b = S.bufs("st", 2)
            tmpn = (junk, junk_b, xs, xs_b, st, st_b)
            for i in range(NT):
                norm_transpose(S, C, X, Xb, i, gbc, gbc_b, tmpn, i % 2, i % 2, hT[:, :, i * 128:(i + 1) * 128], hT_b[i])
            gsb = sb1("o_gsb", [8, T], F32); gsb_b = S.buf("gsb")
            for blk in range(NQB):
                pb = 2 + blk % 2
                for kc in range(8):
                    S.op("pe", lambda e: e.matmul(ps[pb][0:8, :], wg[:, kc, :], hT[:, kc, blk * 512:(blk + 1) * 512],
                                                  start=(kc == 0), stop=(kc == 7)),
                         reads=[wg_b] + hT_b[blk * 4:(blk + 1) * 4], writes=[psb[pb]], signal=(kc == 7))
                S.op("act", lambda e: e.activation(out=gsb[:, blk * 512:(blk + 1) * 512], in_=ps[pb][0:8, :], func=AF.Copy),
                     reads=[psb[pb]], writes=[gsb_b])
            gsd_b = S.buf("gsd")
            S.dma("sp", gs_dram[:, :], gsb[:, :], reads=[gsb_b], writes=[gsd_b])
            G = {}
            for nm in ("I", "F", "e1", "sp", "bneg", "a", "cm", "Mx", "rowfac", "inter", "wkx", "en"):
                G[nm] = sb1("o_G" + nm, [NP, 128], F32)
            Gb = S.buf("G")
            S.dma("sp", G["I"][:, :], gs_dram[0:4, :].rearrange("h (c t) -> (h c) t", t=128), reads=[gsd_b], writes=[Gb])
            Fb = S.buf("GF")
            S.dma("sp", G["F"][:, :], gs_dram[4:8, :].rearrange("h (c t) -> (h c) t", t=128), reads=[gsd_b], writes=[Fb])
            bias = sb1("o_bias", [NP, 4], F32); bias_b = [S.buf("bias%d" % i) for i in range(8)]
            for h in range(4):
                S.dma("sp", bias[h * NT:(h + 1) * NT, 0:1], P["i_b"][h:h + 1].partition_broadcast(NT), writes=[bias_b[h]])
                S.dma("sp", bias[h * NT:(h + 1) * NT, 1:2], P["f_b"][h:h + 1].partition_broadcast(NT), writes=[bias_b[4 + h]])
            Kc = sb1("o_K", [NP, 4], F32)
            R = sb1("o_R", [1, 8 * NP], F32)
            allg = [Gb, Fb] + bias_b
            def gop(eng, fn):
                S.op(eng, fn, reads=allg + [cbuf], writes=[Gb])
            gop("dve", lambda e: e.tensor_scalar(out=bias[:, 2:3], in0=bias[:, 1:2], scalar1=-1.0, scalar2=None, op0=ALU.mult))
            gop("act", lambda e: e.activation(out=G["e1"][:, :], in_=G["F"][:, :], func=AF.Exp, bias=bias[:, 2:3], scale=-1.0))
            gop("act", lambda e: e.activation(out=G["sp"][:, :], in_=G["e1"][:, :], func=AF.Ln, bias=1.0))
            gop("dve", lambda e: e.tensor_tensor_scan(out=G["bneg"][:, :], data0=onesf[0:NP, :], data1=G["sp"][:, :],
                                                      initial=0.0, op0=ALU.mult, op1=ALU.add))
            gop("dve", lambda e: e.scalar_tensor_tensor(out=G["a"][:, :], in0=G["I"][:, :], scalar=bias[:, 0:1],
                                                        in1=G["bneg"][:, :], op0=ALU.add, op1=ALU.add))
            gop("dve", lambda e: e.tensor_tensor_scan(out=G["cm"][:, :], data0=zerosf[0:NP, :], data1=G["a"][:, :],
                                                      initial=-1e30, op0=ALU.add, op1=ALU.max))
            pr = 4
            S.op("pe", lambda e: e.transpose(out=ps[pr][0:1, 0:NP], in_=G["bneg"][:, 127:128], identity=ident[0:NP, 0:NP]),
                 reads=[Gb, cbuf], writes=[psb[pr]], signal=False)
            S.op("pe", lambda e: e.transpose(out=ps[pr][0:1, NP:2 * NP], in_=G["cm"][:, 127:128], identity=ident[0:NP, 0:NP]),
                 reads=[Gb, cbuf], writes=[psb[pr]])
            def rop(eng, fn, extra=()):
                S.op(eng, fn, reads=[Gb, cbuf] + list(extra), writes=[Gb])
            rop("act", lambda e: e.activation(out=R[:, 0:2 * NP], in_=ps[pr][0:1, 0:2 * NP], func=AF.Copy), [psb[pr]])
            rop("dve", lambda e: e.tensor_scalar(out=R[:, 2 * NP:3 * NP], in0=R[:, 0:NP], scalar1=-1.0, scalar2=None, op0=ALU.mult))
            for h in range(4):
                rop("dve", lambda e: e.tensor_tensor_scan(out=R[:, 3 * NP + h * NT:3 * NP + (h + 1) * NT],
                                                          data0=R[:, NP + h * NT:NP + (h + 1) * NT],
                                                          data1=R[:, 2 * NP + h * NT:2 * NP + (h + 1) * NT],
                                                          initial=0.0, op0=ALU.max, op1=ALU.add))
            rop("dve", lambda e: e.memset(R[:, 4 * NP:5 * NP], 0.0))
            rop("dve", lambda e: e.tensor_copy(out=R[:, 4 * NP:5 * NP].rearrange("o (h c) -> o h c", h=4)[:, :, 1:NT],
                                               in_=R[:, 3 * NP:4 * NP].rearrange("o (h c) -> o h c", h=4)[:, :, 0:NT - 1]))
            rop("dve", lambda e: e.tensor_tensor(out=R[:, 5 * NP:6 * NP], in0=R[:, 4 * NP:5 * NP], in1=R[:, NP:2 * NP], op=ALU.max))
            rop("dve", lambda e: e.tensor_tensor(out=R[:, 6 * NP:7 * NP], in0=R[:, 4 * NP:5 * NP], in1=R[:, 5 * NP:6 * NP],
                                                 op=ALU.subtract))
            rop("act", lambda e: e.activation(out=R[:, 7 * NP:8 * NP], in_=R[:, 6 * NP:7 * NP], func=AF.Exp))
            pk = 5
            S.op("pe", lambda e: e.matmul(ps[pk][0:NP, 0:1], R[0:1, 4 * NP:5 * NP], onesf[0:1, 0:1], start=True, stop=True),
                 reads=[Gb, cbuf], writes=[psb[pk]], signal=False)
            S.op("pe", lambda e: e.matmul(ps[pk][0:NP, 1:2], R[0:1, 5 * NP:6 * NP], onesf[0:1, 0:1], start=True, stop=True),
                 reads=[Gb, cbuf], writes=[psb[pk]])
            rop("act", lambda e: e.activation(out=Kc[:, 0:2], in_=ps[pk][0:NP, 0:2], func=AF.Copy), [psb[pk]])
            rop("dve", lambda e: e.tensor_scalar(out=Kc[:, 2:3], in0=Kc[:, 1:2], scalar1=-1.0, scalar2=None, op0=ALU.mult))
            rop("dve", lambda e: e.tensor_scalar(out=G["Mx"][:, :], in0=G["cm"][:, :], scalar1=Kc[:, 0:1], scalar2=None, op0=ALU.max))
            rop("act", lambda e: e.activation(out=G["rowfac"][:, :], in_=G["Mx"][:, :], func=AF.Exp, bias=Kc[:, 1:2], scale=-1.0))
            rop("act", lambda e: e.activation(out=G["inter"][:, :], in_=G["Mx"][:, :], func=AF.Exp, bias=Kc[:, 0:1], scale=-1.0))
            rop("act", lambda e: e.activation(out=G["wkx"][:, :], in_=G["a"][:, :], func=AF.Exp, bias=Kc[:, 2:3], scale=1.0))
            rop("dve", lambda e: e.tensor_tensor(out=G["e1"][:, :], in0=G["bneg"][:, :], in1=G["Mx"][:, :], op=ALU.subtract))
            rop("act", lambda e: e.activation(out=G["en"][:, :], in_=G["e1"][:, :], func=AF.Exp))
            pt = 6
            for qi, nm in enumerate(("rowfac", "inter", "wkx", "en")):
                S.op("pe", lambda e: e.transpose(out=ps[pt][:, qi * NP:(qi + 1) * NP], in_=G[nm][:, :], identity=ident[0:NP, 0:NP]),
                     reads=[Gb, cbuf], writes=[psb[pt]], signal=(qi == 3))
            S.op("act", lambda e: e.activation(out=TS[:, :], in_=ps[pt][:, 0:4 * NP], func=AF.Copy), reads=[psb[pt]], writes=[TS_b])
            pd = 7
            S.op("pe", lambda e: e.matmul(ps[pd][:, 0:NP], onesf[0:1, :], R[0:1, 7 * NP:8 * NP], start=True, stop=True),
                 reads=[Gb, cbuf], writes=[psb[pd]])
            S.op("act", lambda e: e.activation(out=DEC[:, :], in_=ps[pd][:, 0:NP], func=AF.Copy), reads=[psb[pd]], writes=[DEC_b])
            S.fence()
        whd = sb("o_whd", [128, 8, 768], BF16); whd_b = S.bufs("whd", 4)
        pre = sb("o_pre", [128, 2, 3 + T], F32); pre_b = S.bufs("pre", 2)
        acc = sb("o_acc", [128, T], F32); acc_b = S.buf("acc")
        qkT = sb("o_qkT", [128, 2, T], BF16); qkT_b = S.bufs("qkT", 2)
        vone = sb("o_vone", [128, NT, 257], BF16); vone_b = S.bufs("vone", NT)
        sog = sb("o_sog", [128, NT, 256], BF16); sog_b = S.bufs("sog", NT)
        C32 = sb("o_C32", [128, 257], F32); C32_b = S.buf("C32")
        Cx = sb("o_Cx", [128, 257], BF16); Cx_b = S.buf("Cx")
        vext = [sb("o_vext%d" % i, [128, 257], BF16) for i in range(2)]; vext_b = S.bufs("vext", 2)
        sm = [sb("o_sm%d" % i, [128, 128], BF16) for i in range(2)]; sm_b = S.bufs("sm", 2)
        ktok = [sb("o_ktok%d" % i, [128, 128], BF16) for i in range(2)]; ktok_b = S.bufs("ktok", 2)
        o1 = [sb("o_o1%d" % i, [128, 257], F32) for i in range(2)]; o1_b = S.bufs("o1", 2)
        o2 = [sb("o_o2%d" % i, [128, 257], F32) for i in range(2)]; o2_b = S.bufs("o2", 2)
        sc = [sb("o_sc%d" % i, [128, 8], F32) for i in range(2)]; sc_b = S.bufs("sc", 2)
        jk = sb("o_jk", [128, 256], BF16); jk_b = S.buf("jk")
        yf = [sb("o_yf%d" % i, [128, 256], F32) for i in range(2)]; yf_b = S.bufs("yf", 2)
        yb = [sb("o_yb%d" % i, [128, 256], BF16) for i in range(2)]; yb_b = S.bufs("yb", 2)
        yTh = [sb("o_yTh%d" % i, [128, 2, 128], BF16) for i in range(2)]; yTh_b = S.bufs("yTh", 2)
        S.op("dve", lambda e: e.memset(pre[:, :, 0:3], 0.0), writes=pre_b)
        S.op("pool", lambda e: e.memset(vone[:, :, 256:257], 1.0), writes=vone_b)
        for h in range(4):
            srcs = ((h * 128, 128), (512 + h * 128, 128), (1024 + h * 256, 256), (2048 + h * 256, 256))
            off = 0
            for si, (c0, n) in enumerate(srcs):
                S.dma("pool", whd[:, :, off:off + n], winv[:, :, c0:c0 + n], writes=[whd_b[si]])
                off += n
            cnt = 0
            for w in range(2):
                for blk in range(NQB):
                    pb = cnt % 2; cnt += 1
                    for kc in range(8):
                        S.op("pe", lambda e: e.matmul(ps[pb][:, :], whd[:, kc, w * 128:(w + 1) * 128],
                                                      hT[:, kc, blk * 512:(blk + 1) * 512], start=(kc == 0), stop=(kc == 7)),
                             reads=[whd_b[w]] + hT_b[blk * 4:(blk + 1) * 4], writes=[psb[pb]], signal=(kc == 7))
                    S.op("act", lambda e: e.activation(out=pre[:, w, 3 + blk * 512:3 + (blk + 1) * 512], in_=ps[pb][:, :],
                                                       func=AF.Copy), reads=[psb[pb]], writes=[pre_b[w]])
                idx = w * 4 + h
                S.op("dve", lambda e: e.tensor_scalar(out=acc[:, :], in0=pre[:, w, 0:T], scalar1=cw[:, idx, 0:1], scalar2=None,
                                                      op0=ALU.mult), reads=[pre_b[w], cw_b], writes=[acc_b])
                for k in range(1, 4):
                    S.op("dve", lambda e: e.scalar_tensor_tensor(out=acc[:, :], in0=pre[:, w, k:k + T], scalar=cw[:, idx, k:k + 1],
                                                                 in1=acc[:, :], op0=ALU.mult, op1=ALU.add),
                         reads=[pre_b[w], cw_b, acc_b], writes=[acc_b])
                if w == 0:
                    S.op("act", lambda e: e.activation(out=qkT[:, 0, :], in_=acc[:, :], func=AF.Silu, bias=cbias[:, idx:idx + 1]),
                         reads=[acc_b, cbias_b], writes=[qkT_b[0]])
                else:
                    S.op("act", lambda e: e.activation(out=acc[:, :], in_=acc[:, :], func=AF.Silu, bias=cbias[:, idx:idx + 1]),
                         reads=[acc_b, cbias_b], writes=[acc_b])
                    S.op("dve", lambda e: e.tensor_scalar(out=qkT[:, 1, :], in0=acc[:, :], scalar1=KS, scalar2=None, op0=ALU.mult),
                         reads=[acc_b], writes=[qkT_b[1]])
            for i in range(NT):
                pb = 2 + i % 2
                for kc in range(8):
                    S.op("pe", lambda e: e.matmul(ps[pb][:, :], hT[:, kc, i * 128:(i + 1) * 128], whd[:, kc, 256:768],
                                                  start=(kc == 0), stop=(kc == 7)),
                         reads=[hT_b[i], whd_b[2], whd_b[3]], writes=[psb[pb]], signal=(kc == 7))
                S.op("act", lambda e: e.activation(out=vone[:, i, 0:256], in_=ps[pb][:, 0:256], func=AF.Copy),
                     reads=[psb[pb]], writes=[vone_b[i]])
                S.op("act", lambda e: e.activation(out=sog[:, i, :], in_=ps[pb][:, 256:512], func=AF.Sigmoid),
                     reads=[psb[pb]], writes=[sog_b[i]])
            S.op("dve", lambda e: e.memset(C32[:, :], 0.0), writes=[C32_b])
            S.op("pool", lambda e: e.memset(Cx[:, :], 0.0), writes=[Cx_b])
            for c in range(NT):
                p = h * NT + c
                k2 = c % 2
                cs = slice(c * 128, (c + 1) * 128)
                S.op("dve", lambda e: e.tensor_scalar(out=vext[k2][:, :], in0=vone[:, c, :], scalar1=TS[:, 2 * NP + p:2 * NP + p + 1],
                                                      scalar2=None, op0=ALU.mult), reads=[vone_b[c], TS_b], writes=[vext_b[k2]])
                S.op("pe", lambda e: e.matmul(ps[4][:, 0:128], qkT[:, 1, cs], qkT[:, 0, cs], start=True, stop=True),
                     reads=[qkT_b[0], qkT_b[1]], writes=[psb[4]])
                S.op("dve", lambda e: e.tensor_tensor(out=sm[k2][:, :], in0=ps[4][:, 0:128], in1=maskle[:, :], op=ALU.mult),
                     reads=[psb[4], cbuf], writes=[sm_b[k2]])
                S.op("pe", lambda e: e.matmul(ps[5][:, 0:257], sm[k2][:, :], vext[k2][:, :], start=True, stop=True),
                     reads=[sm_b[k2], vext_b[k2]], writes=[psb[5]])
                S.op("pe", lambda e: e.matmul(ps[6][:, 0:257], qkT[:, 0, cs], Cx[:, :], start=True, stop=True),
                     reads=[qkT_b[0], Cx_b], writes=[psb[6]])
                S.op("act", lambda e: e.activation(out=o1[k2][:, :], in_=ps[5][:, 0:257], func=AF.Identity,
                                                   scale=TS[:, p:p + 1]), reads=[psb[5], TS_b], writes=[o1_b[k2]])
                S.op("dve", lambda e: e.scalar_tensor_tensor(out=o2[k2][:, :], in0=ps[6][:, 0:257], scalar=TS[:, NP + p:NP + p + 1],
                                                             in1=o1[k2][:, :], op0=ALU.mult, op1=ALU.add),
                     reads=[psb[6], TS_b, o1_b[k2]], writes=[o2_b[k2]])
                pTk = ps[7][:, :].bitcast(BF16)
                S.op("pe", lambda e: e.transpose(out=pTk[:, 0:128], in_=qkT[:, 1, cs], identity=identb[:, :]),
                     reads=[qkT_b[1], cbuf], writes=[psb[7]])
                S.op("act", lambda e: e.activation(out=ktok[k2][:, :], in_=pTk[:, 0:128], func=AF.Copy),
                     reads=[psb[7]], writes=[ktok_b[k2]])
                S.op("pe", lambda e: e.matmul(ps[7][:, 128:385], ktok[k2][:, :], vext[k2][:, :], start=True, stop=True),
                     reads=[ktok_b[k2], vext_b[k2]], writes=[psb[7]])
                S.op("dve", lambda e: e.scalar_tensor_tensor(out=C32[:, :], in0=C32[:, :], scalar=DEC[:, p:p + 1],
                                                             in1=ps[7][:, 128:385], op0=ALU.mult, op1=ALU.add),
                     reads=[C32_b, DEC_b, psb[7]], writes=[C32_b])
                S.op("act", lambda e: e.activation(out=Cx[:, :], in_=C32[:, :], func=AF.Copy), reads=[C32_b], writes=[Cx_b])
                s_ = sc[k2]; s_b = sc_b[k2]
                S.op("dve", lambda e: e.tensor_scalar(out=s_[:, 7:8], in0=o2[k2][:, 256:257], scalar1=-1.0, scalar2=None,
                                                      op0=ALU.mult), reads=[o2_b[k2]], writes=[s_b])
                S.op("dve", lambda e: e.tensor_tensor(out=s_[:, 0:1], in0=o2[k2][:, 256:257], in1=s_[:, 7:8], op=ALU.max),
                     reads=[o2_b[k2], s_b], writes=[s_b])
                S.op("dve", lambda e: e.tensor_tensor(out=s_[:, 0:1], in0=s_[:, 0:1], in1=TS[:, 3 * NP + p:3 * NP + p + 1],
                                                      op=ALU.max), reads=[s_b, TS_b], writes=[s_b])
                S.op("dve", lambda e: e.reciprocal(out=s_[:, 1:2], in_=s_[:, 0:1]), reads=[s_b], writes=[s_b])
                S.op("act", lambda e: e.activation(out=jk[:, :], in_=o2[k2][:, 0:256], func=AF.Square, scale=s_[:, 1:2],
                                                   accum_out=s_[:, 2:3]), reads=[o2_b[k2], s_b], writes=[jk_b, s_b])
                S.op("dve", lambda e: e.tensor_scalar(out=s_[:, 3:4], in0=s_[:, 2:3], scalar1=1.0 / 256, scalar2=EPS,
                                                      op0=ALU.mult, op1=ALU.add), reads=[s_b], writes=[s_b])
                S.op("act", lambda e: e.sqrt(out=s_[:, 4:5], in_=s_[:, 3:4]), reads=[s_b], writes=[s_b])
                S.op("dve", lambda e: e.reciprocal(out=s_[:, 5:6], in_=s_[:, 4:5]), reads=[s_b], writes=[s_b])
                S.op("dve", lambda e: e.tensor_tensor(out=s_[:, 6:7], in0=s_[:, 5:6], in1=s_[:, 1:2], op=ALU.mult),
                     reads=[s_b], writes=[s_b])
                S.op("dve", lambda e: e.scalar_tensor_tensor(out=yf[k2][:, :], in0=o2[k2][:, 0:256], scalar=s_[:, 6:7],
                                                             in1=ongb[:, h * 256:(h + 1) * 256], op0=ALU.mult, op1=ALU.mult),
                     reads=[o2_b[k2], s_b, ong_b], writes=[yf_b[k2]])
                S.op("dve", lambda e: e.tensor_tensor(out=yb[k2][:, :], in0=yf[k2][:, :], in1=sog[:, c, :], op=ALU.mult),
                     reads=[yf_b[k2], sog_b[c]], writes=[yb_b[k2]])
                pTy = ps[4][:, :].bitcast(BF16)
                for kk in range(2):
                    S.op("pe", lambda e: e.transpose(out=pTy[:, 512 + kk * 128:512 + (kk + 1) * 128],
                                                     in_=yb[k2][:, kk * 128:(kk + 1) * 128], identity=identb[:, :]),
                         reads=[yb_b[k2], cbuf], writes=[psb[4]], signal=(kk == 1))
                S.op("act", lambda e: e.activation(out=yTh[k2][:, :, :], in_=pTy[:, 512:768].rearrange("p (k t) -> p k t", k=2),
                                                   func=AF.Copy), reads=[psb[4]], writes=[yTh_b[k2]])
                for dh in range(2):
                    pb = dh
                    for kk in range(2):
                        S.op("pe", lambda e: e.matmul(ps[pb][:, :], yTh[k2][:, kk, :], wout[:, 2 * h + kk, dh * 512:(dh + 1) * 512],
                                                      start=(kk == 0), stop=(kk == 1)),
                             reads=[yTh_b[k2], wout_b[dh]], writes=[psb[pb]], signal=(kk == 1))
                    S.op("dve", lambda e: e.tensor_tensor(out=X[:, c, dh * 512:(dh + 1) * 512], in0=ps[pb][:, :],
                                                          in1=X[:, c, dh * 512:(dh + 1) * 512], op=ALU.add),
                         reads=[psb[pb], Xb[c]], writes=[Xb[c]])
        S.fence()


def load_bcast(S, q, dst_tile, dst_buf, src_row_ap, n):
    S.dma(q, dst_tile[:, :], src_row_ap.partition_broadcast(128), writes=[dst_buf])

def mlp_block(S, nc, C, X, Xb, tiles, g_row, w1, w2):
    ps, psb = C["ps"], C["psb"]
    ident, identb = C["ident"], C["identb"]
    NTL = len(tiles)
    TB = NTL * 128
    NH = TB // 512
    with ExitStack() as es:
        def sb(name, shape, dt):
            return es.enter_context(nc.sbuf_tensor(uname(name), shape, dt))
        gbc = sb("m_gbc", [128, 1024], F32); gbc_b = S.buf("gbc")
        hT = sb("m_hT", [128, 8, TB], BF16); hT_b = S.bufs("hT", NTL)
        h1T = sb("m_h1T", [128, 32, TB], BF16); h1T_b = [[S.buf("h1T") for _ in range(NH)] for _ in range(32)]
        junk = sb("m_junk", [128, 1024], BF16); junk_b = S.buf("junk")
        xs = [sb("m_xs%d" % i, [128, 1024], BF16) for i in range(2)]; xs_b = S.bufs("xs", 2)
        st = [sb("m_st%d" % i, [128, 4], F32) for i in range(2)]; st_b = S.bufs("st", 2)
        rl = [sb("m_rl%d" % i, [128, 512], BF16) for i in range(2)]; rl_b = S.bufs("rl", 2)
        w1t = [sb("m_w1_%d" % i, [128, 8, 512], BF16) for i in range(2)]; w1_b = S.bufs("w1t", 2)
        w2t = [sb("m_w2_%d" % i, [128, 4, 512], BF16) for i in range(3)]; w2_b = S.bufs("w2t", 3)

        S.dma("sp", gbc[:, :], g_row.partition_broadcast(128), writes=[gbc_b])
        w1v = w1.rearrange("(kc p) f -> p kc f", p=128)
        def load_w1(blk):
            S.dma("pool", w1t[blk % 2][:, :, :], w1v[:, :, blk * 512:(blk + 1) * 512], writes=[w1_b[blk % 2]])
        load_w1(0); load_w1(1)
        for j, ti in enumerate(tiles):
            k = j % 2
            S.op("act", lambda e: e.activation(out=junk[:, :], in_=X[:, ti, :], func=AF.Square,
                                               accum_out=st[k][:, 0:1]),
                 reads=[Xb[ti]], writes=[junk_b, st_b[k]])
            S.op("dve", lambda e: e.tensor_scalar(out=st[k][:, 1:2], in0=st[k][:, 0:1], scalar1=1.0 / 1024,
                                                  scalar2=EPS, op0=ALU.mult, op1=ALU.add),
                 reads=[st_b[k]], writes=[st_b[k]])
            S.op("act", lambda e: e.sqrt(out=st[k][:, 2:3], in_=st[k][:, 1:2]), reads=[st_b[k]], writes=[st_b[k]])
            S.op("dve", lambda e: e.reciprocal(out=st[k][:, 3:4], in_=st[k][:, 2:3]), reads=[st_b[k]], writes=[st_b[k]])
            S.op("dve", lambda e: e.scalar_tensor_tensor(out=xs[k][:, :], in0=X[:, ti, :], scalar=st[k][:, 3:4],
                                                         in1=gbc[:, :], op0=ALU.mult, op1=ALU.mult),
                 reads=[Xb[ti], st_b[k], gbc_b], writes=[xs_b[k]])
            pb = j % 2
            pT = ps[pb][:, :].bitcast(BF16)
            for c in range(8):
                S.op("pe", lambda e: e.transpose(out=pT[:, c * 128:(c + 1) * 128], in_=xs[k][:, c * 128:(c + 1) * 128],
                                                 identity=identb[:, :]),
                     reads=[xs_b[k]], writes=[psb[pb]], signal=(c == 7))
            S.op("act", lambda e: e.activation(out=hT[:, :, j * 128:(j + 1) * 128],
                                               in_=pT.rearrange("p (c t) -> p c t", c=8), func=AF.Copy),
                 reads=[psb[pb]], writes=[hT_b[j]])
        cnt = 0
        for blk in range(8):
            wt = w1t[blk % 2]; wb = w1_b[blk % 2]
            for fi in range(4):
                fc = blk * 4 + fi
                for th in range(NH):
                    pb = 2 + (cnt % 3); cnt += 1
                    for kc in range(8):
                        S.op("pe", lambda e: e.matmul(ps[pb][:, :], wt[:, kc, fi * 128:(fi + 1) * 128],
                                                      hT[:, kc, th * 512:(th + 1) * 512], start=(kc == 0), stop=(kc == 7)),
                             reads=[wb] + hT_b[th * 4:(th + 1) * 4], writes=[psb[pb]], signal=(kc == 7))
                    k = cnt % 2
                    S.op("act", lambda e: e.activation(out=rl[k][:, :], in_=ps[pb][:, :], func=AF.Relu),
                         reads=[psb[pb]], writes=[rl_b[k]])
                    S.op("dve", lambda e: e.tensor_tensor(out=h1T[:, fc, th * 512:(th + 1) * 512], in0=rl[k][:, :],
                                                          in1=rl[k][:, :], op=ALU.mult),
                         reads=[rl_b[k]], writes=[h1T_b[fc][th]])
            if blk + 2 < 8:
                load_w1(blk + 2)
        w2v = w2.rearrange("(g fi p) d -> g p fi d", p=128, fi=4)
        nload = [0]
        def load_w2(idx):
            dh, g = divmod(idx, 8)
            S.dma("pool", w2t[idx % 3][:, :, :], w2v[g, :, :, dh * 512:(dh + 1) * 512], writes=[w2_b[idx % 3]])
        for i in range(3):
            load_w2(i)
        for dh in range(2):
            for g in range(8):
                idx = dh * 8 + g
                wt = w2t[idx % 3]; wb = w2_b[idx % 3]
                for fi in range(4):
                    fc = g * 4 + fi
                    for j in range(NTL):
                        th = j // 4
                        S.op("pe", lambda e: e.matmul(ps[j][:, :], h1T[:, fc, j * 128:(j + 1) * 128], wt[:, fi, :],
                                                      start=(fc == 0), stop=(fc == 31)),
                             reads=[wb, h1T_b[fc][th]], writes=[psb[j]], signal=(fc == 31 or fi == 3))
                if idx + 3 < 16:
                    load_w2(idx + 3)
            for j, ti in enumerate(tiles):
                S.op("dve", lambda e: e.tensor_tensor(out=X[:, ti, dh * 512:(dh + 1) * 512], in0=ps[j][:, :],
                                                      in1=X[:, ti, dh * 512:(dh + 1) * 512], op=ALU.add),
                     reads=[psb[j], Xb[ti]], writes=[Xb[ti]])
        S.fence()

T_SEQ = 2048
NSEQ_CORE = 2
N_CORES = 8
IN_SHAPES = {
    'mix_norm_g': [4, 1024], 'mlp_norm_g': [4, 1024], 'mlp_w1': [4, 1024, 4096], 'mlp_w2': [4, 4096, 1024],
    'ev_w_in': [2, 1024, 2560], 'ev_w_out': [2, 1024, 1024], 'sg_ln_g': [2, 512], 'sg_ln_b': [2, 512],
    'sg_w': [2, 8, 128, 128], 'sg_b': [2, 8, 128], 'sb_q_norm_g': [2, 64], 'sb_k_norm_g': [2, 64],
    'od_w_in': [2, 1024, 3080], 'od_conv_w': [2, 4, 1024], 'od_conv_b': [2, 1024], 'od_i_b': [2, 4], 'od_f_b': [2, 4],
    'od_out_norm_g': [2, 1024], 'od_w_out': [2, 1024, 1024],
}


def build_program(T=T_SEQ, nseq=NSEQ_CORE, layers=(0, 1, 2, 3)):
    NT = T // 128
    nc = bass.Bass("TRN2", target_bir_lowering=False)
    x = nc.dram_tensor("x", [nseq, T, 1024], F32, kind="ExternalInput").ap()
    W = {k: nc.dram_tensor(k, shp, F32, kind="ExternalInput").ap() for k, shp in IN_SHAPES.items()}
    y = nc.dram_tensor("y", [nseq, T, 1024], F32, kind="ExternalOutput").ap()
    gs = nc.dram_tensor("gs_scratch", [8, T], F32, kind="Internal").ap()
    with ExitStack() as es:
        S = Sched(nc, es)
        C = {}
        C["ps"] = [es.enter_context(nc.psum_tensor("ps%d" % i, [128, 512], F32)) for i in range(8)]
        C["psb"] = S.bufs("ps", 8)
        make_consts(S, nc, es, C)
        X = es.enter_context(nc.sbuf_tensor("X", [128, NT, 1024], F32))
        Xb = S.bufs("X", NT, persist=True)
        for s in range(nseq):
            xv = x[s].rearrange("(n p) d -> p n d", p=128)
            yv = y[s].rearrange("(n p) d -> p n d", p=128)
            for i in range(NT):
                S.dma("sp", X[:, i, :], xv[:, i, :], writes=[Xb[i]])
            for l in layers:
                j = l // 2
                if l % 2 == 0:
                    P = dict(g=W['mix_norm_g'][l], w_in=W['ev_w_in'][j], w_out=W['ev_w_out'][j], ln_g=W['sg_ln_g'][j],
                             ln_b=W['sg_ln_b'][j], sg_w=W['sg_w'][j], sg_b=W['sg_b'][j], qg=W['sb_q_norm_g'][j],
                             kg=W['sb_k_norm_g'][j])
                    even_mixer(S, nc, C, X, Xb, NT, P)
                else:
                    P = dict(g=W['mix_norm_g'][l], w_in=W['od_w_in'][j], w_out=W['od_w_out'][j], conv_w=W['od_conv_w'][j],
                             conv_b=W['od_conv_b'][j], i_b=W['od_i_b'][j], f_b=W['od_f_b'][j], on_g=W['od_out_norm_g'][j])
                    odd_mixer(S, nc, C, X, Xb, NT, P, gs)
                for t0 in range(0, NT, 8):
                    tiles = list(range(t0, min(t0 + 8, NT)))
                    mlp_block(S, nc, C, X, Xb, tiles, W['mlp_norm_g'][l], W['mlp_w1'][l], W['mlp_w2'][l])
            for i in range(NT):
                S.dma("sp", yv[:, i, :], X[:, i, :], reads=[Xb[i]])
        S.finish()
    return nc


def kernel(**inputs):
    x = np.ascontiguousarray(np.asarray(inputs['x'], dtype=np.float32))
    B = x.shape[0]
    per = B // N_CORES
    nc = build_program(T=x.shape[1], nseq=per)
    shared = {k: np.ascontiguousarray(np.asarray(inputs[k], dtype=np.float32)) for k in IN_SHAPES}
    in_maps = []
    for c in range(N_CORES):
        m = dict(shared)
        m['x'] = np.ascontiguousarray(x[c * per:(c + 1) * per])
        in_maps.append(m)
    res = run_bass_kernel_spmd(nc, in_maps, core_ids=list(range(N_CORES)))
    return np.concatenate([np.asarray(r['y']) for r in res.results], axis=0).astype(np.float32)
```

```python
from contextlib import ExitStack
from concourse.bass_utils import run_bass_kernel_spmd
import numpy as np
import concourse.bass as bass
import concourse.mybir as mybir

F32 = mybir.dt.float32
BF16 = mybir.dt.bfloat16
AF = mybir.ActivationFunctionType
ALU = mybir.AluOpType
AX = mybir.AxisListType


_UID = [0]


def uname(name):
    _UID[0] += 1
    return "%s_%d" % (name, _UID[0])


class Buf:
    __slots__ = ("name", "w", "r", "sem", "semval", "persist")

    def __init__(self, name, fence, persist=False):
        self.persist = persist
        self.name = name
        self.w = None
        self.r = list(fence)
        self.sem = None
        self.semval = 0


class EngState:
    def __init__(self, name, obj, sem):
        self.name = name
        self.obj = obj
        self.sem = sem
        self.count = 0
        self.know = {}
        self.last = None


class Sched:
    def __init__(self, nc, stack):
        self.nc = nc
        self.stack = stack
        self.E = {}
        for name, obj in (("pe", nc.tensor), ("act", nc.scalar), ("dve", nc.vector),
                          ("pool", nc.gpsimd), ("sp", nc.sync)):
            sem = stack.enter_context(nc.semaphore("sem_" + name))
            self.E[name] = EngState(name, obj, sem)
        self.fence_recs = []
        self.dma_recs = []
        self.pending_pe = []
        self.sem_pool = {}
        self.phase_bufs = []
        self.nsem = 0
        self.nwaits = 0
        self.nops = 0

    def buf(self, name, persist=False):
        return Buf(name, self.fence_recs, persist)

    def bufs(self, name, n, persist=False):
        return [Buf("%s%d" % (name, i), self.fence_recs, persist) for i in range(n)]

    def _waits(self, eng, reads, writes):
        st = self.E[eng]
        deps = []
        for b in reads:
            if b.w is not None:
                deps.append(b.w)
        for b in writes:
            if b.w is not None:
                deps.append(b.w)
            deps.extend(b.r)
        pesem = self.E["pe"].sem
        seen = set()
        for rec in deps:
            if id(rec) in seen:
                continue
            seen.add(id(rec))
            s, v, clk = rec
            if eng == "pe" and s is pesem:
                continue
            assert v is not None, "dependency on unsignaled PE op"
            if st.know.get(s, 0) >= v:
                continue
            st.obj.wait_ge(s, v)
            self.nwaits += 1
            for ks, kv in clk.items():
                if st.know.get(ks, 0) < kv:
                    st.know[ks] = kv

    def op(self, eng, fn, reads=(), writes=(), signal=True):
        st = self.E[eng]
        self._waits(eng, reads, writes)
        ins = fn(st.obj)
        self.nops += 1
        if signal:
            st.count += 1
            ins.then_inc(st.sem, 1)
            clk = dict(st.know)
            clk[st.sem] = st.count
            rec = [st.sem, st.count, clk]
            if eng == "pe":
                for p in self.pending_pe:
                    p[1] = st.count
                    p[2] = clk
                self.pending_pe = []
            st.last = rec
        else:
            assert eng == "pe"
            rec = [st.sem, None, None]
            self.pending_pe.append(rec)
        for b in reads:
            b.r.append(rec)
        for b in writes:
            b.w = rec
            b.r = []
        return ins

    def dma(self, q, out, in_, reads=(), writes=(), **kw):
        st = self.E[q]
        self._waits(q, reads, writes)
        owner = writes[0] if writes else reads[0]
        if owner.sem is None:
            pool = self.sem_pool.setdefault(q, [])
            if pool and not owner.persist:
                owner.sem, owner.semval = pool.pop()
            else:
                self.nsem += 1
                owner.sem = self.stack.enter_context(self.nc.semaphore("dsem%d" % self.nsem))
            if not owner.persist:
                self.phase_bufs.append((owner, q))
        owner.semval += 16
        ins = st.obj.dma_start(out=out, in_=in_, **kw)
        ins.then_inc(owner.sem, 16)
        clk = dict(st.know)
        clk[owner.sem] = owner.semval
        rec = [owner.sem, owner.semval, clk]
        self.dma_recs.append(rec)
        for b in reads:
            b.r.append(rec)
        for b in writes:
            b.w = rec
            b.r = []
        return ins

    def fence(self):
        sp = self.E["sp"]
        for rec in self.dma_recs:
            s, v, clk = rec
            if sp.know.get(s, 0) >= v:
                continue
            sp.obj.wait_ge(s, v)
            for ks, kv in clk.items():
                if sp.know.get(ks, 0) < kv:
                    sp.know[ks] = kv
        self.dma_recs = []
        for b, bq in self.phase_bufs:
            self.sem_pool.setdefault(bq, []).append((b.sem, b.semval))
            b.sem = None
        self.phase_bufs = []
        ins = sp.obj.nop()
        sp.count += 1
        ins.then_inc(sp.sem, 1)
        clk = dict(sp.know)
        clk[sp.sem] = sp.count
        sp.last = [sp.sem, sp.count, clk]
        assert not self.pending_pe
        recs = []
        for name, st in self.E.items():
            if st.last is not None:
                recs.append(st.last)
        self.fence_recs = recs

    def finish(self):
        self.fence()


EPS = 1e-6

def rstd_ops(S, st, st_b, n, inv_n):
    S.op("dve", lambda e: e.tensor_scalar(out=st[:, n:2 * n], in0=st[:, 0:n], scalar1=inv_n, scalar2=EPS,
                                          op0=ALU.mult, op1=ALU.add), reads=[st_b], writes=[st_b])
    S.op("act", lambda e: e.sqrt(out=st[:, 2 * n:3 * n], in_=st[:, n:2 * n]), reads=[st_b], writes=[st_b])
    S.op("dve", lambda e: e.reciprocal(out=st[:, 3 * n:4 * n], in_=st[:, 2 * n:3 * n]), reads=[st_b], writes=[st_b])


def norm_transpose(S, C, X, Xb, ti, gbc, gbc_b, tmp, k, pb, out_ap, out_b):
    ps, psb, identb = C["ps"], C["psb"], C["identb"]
    junk, junk_b, xs, xs_b, st, st_b = tmp
    S.op("act", lambda e: e.activation(out=junk[:, :], in_=X[:, ti, :], func=AF.Square, accum_out=st[k][:, 0:1]),
         reads=[Xb[ti]], writes=[junk_b, st_b[k]])
    rstd_ops(S, st[k], st_b[k], 1, 1.0 / 1024)
    S.op("dve", lambda e: e.scalar_tensor_tensor(out=xs[k][:, :], in0=X[:, ti, :], scalar=st[k][:, 3:4],
                                                 in1=gbc[:, :], op0=ALU.mult, op1=ALU.mult),
         reads=[Xb[ti], st_b[k], gbc_b], writes=[xs_b[k]])
    pT = ps[pb][:, :].bitcast(BF16)
    for c in range(8):
        S.op("pe", lambda e: e.transpose(out=pT[:, c * 128:(c + 1) * 128], in_=xs[k][:, c * 128:(c + 1) * 128],
                                         identity=identb[:, :]),
             reads=[xs_b[k], C["cb"]], writes=[psb[pb]], signal=(c == 7))
    S.op("act", lambda e: e.activation(out=out_ap, in_=pT.rearrange("p (c t) -> p c t", c=8), func=AF.Copy),
         reads=[psb[pb]], writes=[out_b])


def even_mixer(S, nc, C, X, Xb, NT, P):
    ps, psb = C["ps"], C["psb"]
    ident, identb = C["ident"], C["identb"]
    T = NT * 128
    NQB = T // 512
    with ExitStack() as es:
        def sb(name, shape, dt):
            return es.enter_context(nc.sbuf_tensor(uname(name), shape, dt))
        yTa = sb("e_yTa", [128, 4, T], BF16); yT_b = [[S.buf("yT") for _ in range(NT)] for _ in range(8)]
        qT = sb("e_qT", [128, 4, T], BF16); qT_b = S.bufs("qT", NT)
        kT = sb("e_kT", [128, 4, T], BF16); kT_b = S.bufs("kT", NT)
        v = sb("e_v", [128, NT, 512], BF16); v_b = S.bufs("v", NT)
        woutv = P["w_out"].rearrange("(kc p) d -> p kc d", p=128)
        with ExitStack() as es1:
            def sb1(name, shape, dt):
                return es1.enter_context(nc.sbuf_tensor(uname(name), shape, dt))
            win = sb1("e_win", [128, 8, 2560], BF16); win_b = S.bufs("win", 5)
            winv = P["w_in"].rearrange("(kc p) f -> p kc f", p=128)
            for cb in range(5):
                S.dma("pool", win[:, :, cb * 512:(cb + 1) * 512], winv[:, :, cb * 512:(cb + 1) * 512], writes=[win_b[cb]])
            gbc = sb1("e_gbc", [128, 1024], F32); gbc_b = S.buf("gbc")
            lng = sb1("e_lng", [128, 512], F32); lng_b = S.buf("lng")
            lnb = sb1("e_lnb", [128, 512], F32); lnb_b = S.buf("lnb")
            qg = sb1("e_qg", [128, 64], F32); qg_b = S.buf("qg")
            kg = sb1("e_kg", [128, 64], F32); kg_b = S.buf("kg")
            sgb = sb1("e_sgb", [128, 8], F32); sgb_b = S.buf("sgb")
            sgw = sb1("e_sgw", [128, 8, 128], BF16); sgw_b = S.buf("sgw")
            wcT = sb1("e_wcT", [128, 8, 128], BF16); wcT_b = S.buf("wcT")
            S.dma("sp", gbc[:, :], P["g"].partition_broadcast(128), writes=[gbc_b])
            S.dma("sp", lng[:, :], P["ln_g"].partition_broadcast(128), writes=[lng_b])
            S.dma("sp", lnb[:, :], P["ln_b"].partition_broadcast(128), writes=[lnb_b])
            S.dma("sp", qg[:, :], P["qg"].partition_broadcast(128), writes=[qg_b])
            S.dma("sp", kg[:, :], P["kg"].partition_broadcast(128), writes=[kg_b])
            S.dma("sp", sgb[:, :], P["sg_b"].rearrange("g t -> t g"), writes=[sgb_b], allow_slow_non_contiguous=True)
            S.dma("pool", sgw[:, :, :], P["sg_w"].rearrange("g t s -> t g s"), writes=[sgw_b])
            S.op("dve", lambda e: e.tensor_scalar(out=qg[:, :], in0=qg[:, :], scalar1=0.125, scalar2=None, op0=ALU.mult),
                 reads=[qg_b], writes=[qg_b])
            S.op("pool", lambda e: e.affine_select(out=sgw[:, :, :], in_=sgw[:, :, :], pattern=[[0, 8], [-1, 128]],
                                                   compare_op=ALU.is_ge, fill=0.0, base=0, channel_multiplier=1),
                 reads=[sgw_b], writes=[sgw_b])
            for half in range(2):
                for gi in range(4):
                    g_ = half * 4 + gi
                    S.op("pe", lambda e: e.transpose(out=ps[half][:, :].bitcast(BF16)[:, gi * 128:(gi + 1) * 128], in_=sgw[:, g_, :],
                                                     identity=identb[:, :]),
                         reads=[sgw_b, C["cb"]], writes=[psb[half]], signal=(gi == 3))
                S.op("act", lambda e: e.activation(out=wcT[:, half * 4:(half + 1) * 4, :],
                                                   in_=ps[half][:, :].bitcast(BF16)[:, 0:512].rearrange("p (g t) -> p g t", g=4), func=AF.Copy),
                     reads=[psb[half]], writes=[wcT_b])
            junk = sb1("e_junk", [128, 1024], BF16); junk_b = S.buf("junk")
            xs = [sb1("e_xs%d" % i, [128, 1024], BF16) for i in range(2)]; xs_b = S.bufs("xs", 2)
            st = [sb1("e_st%d" % i, [128, 4], F32) for i in range(2)]; st_b = S.bufs("st", 2)
            tmpn = (junk, junk_b, xs, xs_b, st, st_b)
            hTt = [sb1("e_hTt%d" % i, [128, 8, 128], BF16) for i in range(2)]; hTt_b = S.bufs("hTt", 2)
            u = [sb1("e_u%d" % i, [128, 512], BF16) for i in range(1)]; u_b = S.bufs("u", 1)
            vg = [sb1("e_vg%d" % i, [128, 512], F32) for i in range(1)]; vg_b = S.bufs("vg", 1)
            vgn = [sb1("e_vgn%d" % i, [128, 512], BF16) for i in range(1)]; vgn_b = S.bufs("vgn", 1)
            lst = [sb1("e_lst%d" % i, [128, 8], F32) for i in range(1)]; lst_b = S.bufs("lst", 1)
            ya = [sb1("e_ya%d" % i, [128, 512], F32) for i in range(1)]; ya_b = S.bufs("ya", 1)
            yab = [sb1("e_yab%d" % i, [128, 512], BF16) for i in range(1)]; yab_b = S.bufs("yab", 1)
            sq = [sb1("e_sq%d" % i, [128, 512], F32) for i in range(1)]; sq_b = S.bufs("sq", 1)
            qst = [sb1("e_qst%d" % i, [128, 32], F32) for i in range(1)]; qst_b = S.bufs("qst", 1)
            qn = [sb1("e_qn%d" % i, [128, 512], BF16) for i in range(1)]; qn_b = S.bufs("qn", 1)
            for i in range(NT):
                k = 0
                kn = i % 2
                norm_transpose(S, C, X, Xb, i, gbc, gbc_b, tmpn, kn, i % 2, hTt[kn][:, :, :], hTt_b[kn])
                for cb in range(5):
                    for kc in range(8):
                        S.op("pe", lambda e: e.matmul(ps[2 + cb][:, :], hTt[kn][:, kc, :], win[:, kc, cb * 512:(cb + 1) * 512],
                                                      start=(kc == 0), stop=(kc == 7)),
                             reads=[hTt_b[kn], win_b[cb]], writes=[psb[2 + cb]], signal=(kc == 7))
                S.op("act", lambda e: e.activation(out=u[k][:, :], in_=ps[2][:, :], func=AF.Gelu_apprx_tanh),
                     reads=[psb[2]], writes=[u_b[k]])
                S.op("act", lambda e: e.activation(out=vg[k][:, :], in_=ps[3][:, :], func=AF.Gelu_apprx_tanh,
                                                   accum_out=lst[k][:, 0:1]),
                     reads=[psb[3]], writes=[vg_b[k], lst_b[k]])
                S.op("act", lambda e: e.activation(out=junk[:, 0:512], in_=vg[k][:, :], func=AF.Square,
                                                   accum_out=lst[k][:, 1:2]),
                     reads=[vg_b[k], lst_b[k]], writes=[junk_b, lst_b[k]])
                L = lst[k]; Lb = lst_b[k]
                S.op("dve", lambda e: e.tensor_scalar(out=L[:, 2:4], in0=L[:, 0:2], scalar1=1.0 / 512, scalar2=None,
                                                      op0=ALU.mult), reads=[Lb], writes=[Lb])
                S.op("dve", lambda e: e.tensor_tensor(out=L[:, 4:5], in0=L[:, 2:3], in1=L[:, 2:3], op=ALU.mult),
                     reads=[Lb], writes=[Lb])
                S.op("dve", lambda e: e.scalar_tensor_tensor(out=L[:, 5:6], in0=L[:, 3:4], scalar=EPS, in1=L[:, 4:5],
                                                             op0=ALU.add, op1=ALU.subtract), reads=[Lb], writes=[Lb])
                S.op("act", lambda e: e.sqrt(out=L[:, 6:7], in_=L[:, 5:6]), reads=[Lb], writes=[Lb])
                S.op("dve", lambda e: e.reciprocal(out=L[:, 7:8], in_=L[:, 6:7]), reads=[Lb], writes=[Lb])
                S.op("dve", lambda e: e.scalar_tensor_tensor(out=L[:, 4:5], in0=L[:, 2:3], scalar=-1.0, in1=L[:, 7:8],
                                                             op0=ALU.mult, op1=ALU.mult), reads=[Lb], writes=[Lb])
                S.op("act", lambda e: e.activation(out=vg[k][:, :], in_=vg[k][:, :], func=AF.Identity,
                                                   bias=L[:, 4:5], scale=L[:, 7:8]),
                     reads=[vg_b[k], Lb], writes=[vg_b[k]])
                S.op("dve", lambda e: e.tensor_tensor(out=vg[k][:, :], in0=vg[k][:, :], in1=lng[:, :], op=ALU.mult),
                     reads=[vg_b[k], lng_b], writes=[vg_b[k]])
                S.op("dve", lambda e: e.tensor_tensor(out=vgn[k][:, :], in0=vg[k][:, :], in1=lnb[:, :], op=ALU.add),
                     reads=[vg_b[k], lnb_b], writes=[vgn_b[k]])
                for g_ in range(8):
                    S.op("pe", lambda e: e.matmul(ps[7][:, g_ * 64:(g_ + 1) * 64], wcT[:, g_, :], vgn[k][:, g_ * 64:(g_ + 1) * 64],
                                                  start=True, stop=True),
                         reads=[wcT_b, vgn_b[k]], writes=[psb[7]], signal=(g_ == 7))
                S.op("dve", lambda e: e.tensor_tensor(out=ya[k][:, :].rearrange("p (g c) -> p g c", g=8),
                                                      in0=ps[7][:, :].rearrange("p (g c) -> p g c", g=8),
                                                      in1=sgb[:, :].unsqueeze(2).to_broadcast([128, 8, 64]), op=ALU.add),
                     reads=[psb[7], sgb_b], writes=[ya_b[k]])
                S.op("dve", lambda e: e.tensor_tensor(out=yab[k][:, :], in0=ya[k][:, :], in1=u[k][:, :], op=ALU.mult),
                     reads=[ya_b[k], u_b[k]], writes=[yab_b[k]])
                pT = ps[7][:, :].bitcast(BF16)
                for c in range(4):
                    S.op("pe", lambda e: e.transpose(out=pT[:, c * 128:(c + 1) * 128], in_=yab[k][:, c * 128:(c + 1) * 128],
                                                     identity=identb[:, :]),
                         reads=[yab_b[k]], writes=[psb[7]], signal=(c == 3))
                S.op("act", lambda e: e.activation(out=yTa[:, 0:4, i * 128:(i + 1) * 128],
                                                   in_=pT[:, 0:512].rearrange("p (c t) -> p c t", c=4), func=AF.Copy),
                     reads=[psb[7]], writes=[yT_b[c][i] for c in range(4)])
                for which, bank, gt, gt_b, dstT, dstT_b in ((0, 4, qg, qg_b, qT, qT_b), (1, 5, kg, kg_b, kT, kT_b)):
                    kk = 0
                    Q = qst[kk]; Qb = qst_b[kk]
                    S.op("act", lambda e: e.activation(out=sq[kk][:, :], in_=ps[bank][:, :], func=AF.Square),
                         reads=[psb[bank]], writes=[sq_b[kk]])
                    S.op("dve", lambda e: e.tensor_reduce(out=Q[:, 0:8], in_=sq[kk][:, :].rearrange("p (h d) -> p h d", h=8),
                                                          axis=AX.X, op=ALU.add), reads=[sq_b[kk]], writes=[Qb])
                    rstd_ops(S, Q, Qb, 8, 1.0 / 64)
                    S.op("dve", lambda e: e.tensor_tensor(out=sq[kk][:, :].rearrange("p (h d) -> p h d", h=8),
                                                          in0=ps[bank][:, :].rearrange("p (h d) -> p h d", h=8),
                                                          in1=Q[:, 24:32].unsqueeze(2).to_broadcast([128, 8, 64]), op=ALU.mult),
                         reads=[psb[bank], Qb, sq_b[kk]], writes=[sq_b[kk]])
                    S.op("dve", lambda e: e.tensor_tensor(out=qn[kk][:, :].rearrange("p (h d) -> p h d", h=8),
                                                          in0=sq[kk][:, :].rearrange("p (h d) -> p h d", h=8),
                                                          in1=gt[:, :].unsqueeze(1).to_broadcast([128, 8, 64]), op=ALU.mult),
                         reads=[sq_b[kk], gt_b], writes=[qn_b[kk]])
                    pT2 = ps[bank][:, :].bitcast(BF16)
                    for c in range(4):
                        S.op("pe", lambda e: e.transpose(out=pT2[:, c * 128:(c + 1) * 128], in_=qn[kk][:, c * 128:(c + 1) * 128],
                                                         identity=identb[:, :]),
                             reads=[qn_b[kk]], writes=[psb[bank]], signal=(c == 3))
                    S.op("act", lambda e: e.activation(out=dstT[:, :, i * 128:(i + 1) * 128],
                                                       in_=pT2[:, 0:512].rearrange("p (c t) -> p c t", c=4), func=AF.Copy),
                         reads=[psb[bank]], writes=[dstT_b[i]])
                S.op("act", lambda e: e.activation(out=v[:, i, :], in_=ps[6][:, :], func=AF.Copy),
                     reads=[psb[6]], writes=[v_b[i]])
            S.fence()
        with ExitStack() as es2:
            def sb2(name, shape, dt):
                return es2.enter_context(nc.sbuf_tensor(uname(name), shape, dt))
            yTb = sb2("e_yTb", [128, 4, T], BF16)
            wout = sb2("e_wout", [128, 8, 1024], BF16); wout_b = S.bufs("wout", 2)
            for dh in range(2):
                S.dma("pool", wout[:, :, dh * 512:(dh + 1) * 512], woutv[:, :, dh * 512:(dh + 1) * 512], writes=[wout_b[dh]])
            e_sb = [sb2("a_e%d" % i, [128, 512], BF16) for i in range(4)]; e_b = S.bufs("e", 4)
            l_sb = [sb2("a_l%d" % i, [128, 512], BF16) for i in range(4)]; l_b = S.bufs("l", 4)
            x_sb = [sb2("a_x%d" % i, [128, 512], BF16) for i in range(4)]; x_b = S.bufs("x", 4)
            a_sb = [sb2("a_a%d" % i, [128, 512], BF16) for i in range(4)]; a_b = S.bufs("a", 4)
            negtri, negcmp, masklt = C["negtri"], C["negcmp"], C["masklt"]
            cbuf = C["cb"]
            LA = 3
            qz = [sb2("a_qz%d" % i, [128, 2, 512], BF16) for i in range(2)]; qz_b = S.bufs("qz", 2)
            for i in range(2):
                S.op("pool", lambda e: e.memset(qz[i][:, :, :], 0.0), writes=[qz_b[i]])
            pcount = 0
            for qb in range(NQB):
                for hp in range(4):
                    pq = pcount % 2; pcount += 1
                    obase = 4 + 2 * pq
                    S.op("dve", lambda e: e.tensor_copy(out=qz[pq][0:64, 0, :], in_=qT[0:64, hp, qb * 512:(qb + 1) * 512]),
                         reads=qT_b[4 * qb:4 * qb + 4], writes=[qz_b[pq]])
                    S.op("dve", lambda e: e.tensor_copy(out=qz[pq][64:128, 1, :], in_=qT[64:128, hp, qb * 512:(qb + 1) * 512]),
                         reads=qT_b[4 * qb:4 * qb + 4], writes=[qz_b[pq]])
                    items = [(j, hh) for j in range(4 * qb + 3, -1, -1) for hh in range(2)]
                    jfirst = 4 * qb + 3

                    n = len(items)

                    def geo(i):
                        j, hh = items[i]
                        return j, hh, i % 4, max(0, (j - 4 * qb)) * 128

                    def pe_z(i):
                        j, hh, par, c0 = geo(i)
                        zb = i % 2
                        S.op("pe", lambda e: e.matmul(ps[zb][:, c0:512], kT[:, hp, j * 128:(j + 1) * 128],
                                                      qz[pq][:, hh, c0:512], start=True, stop=True),
                             reads=[kT_b[j], qz_b[pq]], writes=[psb[zb]])

                    def pe_tri(i):
                        j, hh, par, c0 = geo(i)
                        S.op("pe", lambda e: e.matmul(ps[2 + hh][:, c0:512], negtri[:, :], l_sb[par][:, c0:512],
                                                      start=(j == jfirst), stop=False, skip_group_check=True),
                             reads=[l_b[par], cbuf], writes=[psb[2 + hh]])

                    def pe_cmp_av(i):
                        j, hh, par, c0 = geo(i)
                        if j > 0:
                            S.op("pe", lambda e: e.matmul(ps[2 + hh][:, c0:512], negcmp[:, :], l_sb[par][:, c0:512],
                                                          start=False, stop=False, skip_group_check=True),
                                 reads=[l_b[par], cbuf], writes=[psb[2 + hh]])
                        S.op("pe", lambda e: e.matmul(ps[obase + hh][:, c0:512], v[:, j, hp * 128:(hp + 1) * 128],
                                                      a_sb[par][:, c0:512], start=(j == jfirst), stop=(j == 0),
                                                      skip_group_check=True),
                             reads=[v_b[j], a_b[par]], writes=[psb[obase + hh]])

                    def act_a(i):
                        j, hh, par, c0 = geo(i)
                        zb = i % 2
                        S.op("act", lambda e: e.activation(out=e_sb[par][:, c0:512], in_=ps[zb][:, c0:512], func=AF.Exp),
                             reads=[psb[zb]], writes=[e_b[par]])
                        if j >= 4 * qb:
                            S.op("dve", lambda e: e.tensor_tensor(out=e_sb[par][:, c0:c0 + 128], in0=e_sb[par][:, c0:c0 + 128],
                                                                  in1=masklt[:, :], op=ALU.mult),
                                 reads=[e_b[par], cbuf], writes=[e_b[par]])
                        S.op("act", lambda e: e.activation(out=l_sb[par][:, c0:512], in_=e_sb[par][:, c0:512], func=AF.Ln,
                                                           bias=1.0),
                             reads=[e_b[par]], writes=[l_b[par]])

                    def act_b(i):
                        j, hh, par, c0 = geo(i)
                        S.op("act", lambda e: e.activation(out=x_sb[par][:, c0:512], in_=ps[2 + hh][:, c0:512], func=AF.Exp),
                             reads=[psb[2 + hh]], writes=[x_b[par]])
                        S.op("dve", lambda e: e.tensor_tensor(out=a_sb[par][:, c0:512], in0=e_sb[par][:, c0:512],
                                                              in1=x_sb[par][:, c0:512], op=ALU.mult),
                             reads=[e_b[par], x_b[par]], writes=[a_b[par]])

                    for i in range(-1, n + 2):
                        if 0 <= i + 1 < n:
                            pe_z(i + 1)
                        if 0 <= i - 1 < n:
                            pe_tri(i - 1)
                        if 0 <= i - 2 < n:
                            pe_cmp_av(i - 2)
                        if 0 <= i < n:
                            act_a(i)
                        if 0 <= i - 1 < n:
                            act_b(i - 1)
                    for hh in range(2):
                        S.op("act", lambda e: e.activation(out=yTb[hh * 64:(hh + 1) * 64, hp, qb * 512:(qb + 1) * 512],
                                                           in_=ps[obase + hh][hh * 64:(hh + 1) * 64, :], func=AF.Copy),
                             reads=[psb[obase + hh]], writes=[yT_b[4 + hp][4 * qb + t] for t in range(4)])
            cnt = 0
            for i in range(NT):
                for dh in range(2):
                    pb = cnt % 4; cnt += 1
                    for kc in range(8):
                        lhs = yTa[:, kc, i * 128:(i + 1) * 128] if kc < 4 else yTb[:, kc - 4, i * 128:(i + 1) * 128]
                        S.op("pe", lambda e: e.matmul(ps[pb][:, :], lhs,
                                                      wout[:, kc, dh * 512:(dh + 1) * 512], start=(kc == 0), stop=(kc == 7)),
                             reads=[yT_b[kc][i], wout_b[dh]], writes=[psb[pb]], signal=(kc == 7))
                    S.op("dve", lambda e: e.tensor_tensor(out=X[:, i, dh * 512:(dh + 1) * 512], in0=ps[pb][:, :],
                                                          in1=X[:, i, dh * 512:(dh + 1) * 512], op=ALU.add),
                         reads=[psb[pb], Xb[i]], writes=[Xb[i]])
            S.fence()


def make_consts(S, nc, es, C):
    ident = es.enter_context(nc.sbuf_tensor("ident", [128, 128], F32))
    identb = es.enter_context(nc.sbuf_tensor("identb", [128, 128], BF16))
    tmp = es.enter_context(nc.sbuf_tensor("ctmp", [128, 128], F32))
    negtri = es.enter_context(nc.sbuf_tensor("negtri", [128, 128], BF16))
    negcmp = es.enter_context(nc.sbuf_tensor("negcmp", [128, 128], BF16))
    masklt = es.enter_context(nc.sbuf_tensor("masklt", [128, 128], BF16))
    maskle = es.enter_context(nc.sbuf_tensor("maskle", [128, 128], BF16))
    onesf = es.enter_context(nc.sbuf_tensor("onesf", [128, 128], F32))
    zerosf = es.enter_context(nc.sbuf_tensor("zerosf", [128, 128], F32))
    cb = S.buf("consts")
    S.op("pool", lambda e: e.memset(onesf[:, :], 1.0), writes=[cb])
    S.op("pool", lambda e: e.memset(zerosf[:, :], 0.0), writes=[cb])
    C.update(onesf=onesf, zerosf=zerosf)
    C.update(ident=ident, identb=identb, negtri=negtri, negcmp=negcmp, masklt=masklt, maskle=maskle, cb=cb)

    def sel(out, val, pattern, cm, op, base=0):
        S.op("pool", lambda e: e.memset(tmp[:, :], val), reads=[cb], writes=[cb])
        S.op("pool", lambda e: e.affine_select(out=tmp[:, :], in_=tmp[:, :], pattern=pattern, compare_op=op, fill=0.0,
                                               base=base, channel_multiplier=cm), reads=[cb], writes=[cb])
        S.op("pool", lambda e: e.tensor_copy(out=out[:, :], in_=tmp[:, :]), reads=[cb], writes=[cb])
    sel(ident, 1.0, [[-1, 128]], 1, ALU.is_equal)
    sel(identb, 1.0, [[-1, 128]], 1, ALU.is_equal)
    sel(negtri, -1.0, [[-1, 128]], 1, ALU.is_ge)
    sel(negcmp, -1.0, [[1, 128]], -1, ALU.is_gt)
    sel(masklt, 1.0, [[1, 128]], -1, ALU.is_gt)
    sel(maskle, 1.0, [[1, 128]], -1, ALU.is_ge)


def odd_mixer(S, nc, C, X, Xb, NT, P, gs_dram):
    ps, psb = C["ps"], C["psb"]
    ident, identb, onesf, zerosf, maskle, cbuf = C["ident"], C["identb"], C["onesf"], C["zerosf"], C["maskle"], C["cb"]
    T = NT * 128
    NQB = T // 512
    NP = 4 * NT
    KS = 128 ** -0.5
    with ExitStack() as es:
        def sb(name, shape, dt):
            return es.enter_context(nc.sbuf_tensor(uname(name), shape, dt))
        hT = sb("o_hT", [128, 8, T], BF16); hT_b = S.bufs("hT", NT)
        wout = sb("o_wout", [128, 8, 1024], BF16); wout_b = S.bufs("wout", 2)
        woutv = P["w_out"].rearrange("(kc p) d -> p kc d", p=128)
        winv = P["w_in"].rearrange("(kc p) f -> p kc f", p=128)
        ongb = sb("o_ong", [128, 1024], F32); ong_b = S.buf("ong")
        cw = sb("o_cw", [128, 8, 4], F32); cw_b = S.buf("cw")
        cbias = sb("o_cb", [128, 8], F32); cbias_b = S.buf("cbias")
        TS = sb("o_TS", [128, 4 * NP], F32); TS_b = S.buf("TS")
        DEC = sb("o_DEC", [128, NP], F32); DEC_b = S.buf("DEC")
        wg = sb("o_wg", [128, 8, 8], BF16); wg_b = S.buf("wg")
        S.dma("pool", wg[:, :, :], winv[:, :, 3072:3080], writes=[wg_b])
        for dh in range(2):
            S.dma("pool", wout[:, :, dh * 512:(dh + 1) * 512], woutv[:, :, dh * 512:(dh + 1) * 512], writes=[wout_b[dh]])
        S.dma("sp", ongb[:, :], P["on_g"].partition_broadcast(128), writes=[ong_b])
        for k_ in range(4):
            S.dma("sp", cw[:, :, k_], P["conv_w"][k_].rearrange("(i p) -> p i", p=128), writes=[cw_b], allow_slow_non_contiguous=True)
        S.dma("sp", cbias[:, :], P["conv_b"].rearrange("(i p) -> p i", p=128), writes=[cbias_b], allow_slow_non_contiguous=True)
        with ExitStack() as es1:
            def sb1(name, shape, dt):
                return es1.enter_context(nc.sbuf_tensor(uname(name), shape, dt))
            gbc = sb1("o_gbc", [128, 1024], F32); gbc_b = S.buf("gbc")
            S.dma("sp", gbc[:, :], P["g"].partition_broadcast(128), writes=[gbc_b])
            junk = sb1("o_junk", [128, 1024], BF16); junk_b = S.buf("junk")
            xs = [sb1("o_xs%d" % i, [128, 1024], BF16) for i in range(2)]; xs_b = S.bufs("xs", 2)
            st = [sb1("o_st%d" % i, [128, 4], F32) for i in range(2)]; st_b = S.bufs("st", 2)
            tmpn = (junk, junk_b, xs, xs_b, st, st_b)
            for i in range(NT):
                norm_transpose(S, C, X, Xb, i, gbc, gbc_b, tmpn, i % 2, i % 2, hT[:, :, i * 128:(i + 1) * 128], hT_b[i])
            gsb = sb1("o_gsb", [8, T], F32); gsb_b = S.buf("gsb")
            for blk in range(NQB):
                pb = 2 + blk % 2
                for kc in range(8):
                    S.op("pe", lambda e: e.matmul(ps[pb][0:8, :], wg[:, kc, :], hT[:, kc, blk * 512:(blk + 1) * 512],
                                                  start=(kc == 0), stop=(kc == 7)),
                         reads=[wg_b] + hT_b[blk * 4:(blk + 1) * 4], writes=[psb[pb]], signal=(kc == 7))
                S.op("act", lambda e: e.activation(out=gsb[:, blk * 512:(blk + 1) * 512], in_=ps[pb][0:8, :], func=AF.Copy),
                     reads=[psb[pb]], writes=[gsb_b])
            gsd_b = S.buf("gsd")
            S.dma("sp", gs_dram[:, :], gsb[:, :], reads=[gsb_b], writes=[gsd_b])
            G = {}
            for nm in ("I", "F", "e1", "sp", "bneg", "a", "cm", "Mx", "rowfac", "inter", "wkx", "en"):
                G[nm] = sb1("o_G" + nm, [NP, 128], F32)
            Gb = S.buf("G")
            S.dma("sp", G["I"][:, :], gs_dram[0:4, :].rearrange("h (c t) -> (h c) t", t=128), reads=[gsd_b], writes=[Gb])
            Fb = S.buf("GF")
            S.dma("sp", G["F"][:, :], gs_dram[4:8, :].rearrange("h (c t) -> (h c) t", t=128), reads=[gsd_b], writes=[Fb])
            bias = sb1("o_bias", [NP, 4], F32); bias_b = [S.buf("bias%d" % i) for i in range(8)]
            for h in range(4):
                S.dma("sp", bias[h * NT:(h + 1) * NT, 0:1], P["i_b"][h:h + 1].partition_broadcast(NT), writes=[bias_b[h]])
                S.dma("sp", bias[h * NT:(h + 1) * NT, 1:2], P["f_b"][h:h + 1].partition_broadcast(NT), writes=[bias_b[4 + h]])
            Kc = sb1("o_K", [NP, 4], F32)
            R = sb1("o_R", [1, 8 * NP], F32)
            allg = [Gb, Fb] + bias_b
            def gop(eng, fn):
                S.op(eng, fn, reads=allg + [cbuf], writes=[Gb])
            gop("dve", lambda e: e.tensor_scalar(out=bias[:, 2:3], in0=bias[:, 1:2], scalar1=-1.0, scalar2=None, op0=ALU.mult))
            gop("act", lambda e: e.activation(out=G["e1"][:, :], in_=G["F"][:, :], func=AF.Exp, bias=bias[:, 2:3], scale=-1.0))
            gop("act", lambda e: e.activation(out=G["sp"][:, :], in_=G["e1"][:, :], func=AF.Ln, bias=1.0))
            gop("dve", lambda e: e.tensor_tensor_scan(out=G["bneg"][:, :], data0=onesf[0:NP, :], data1=G["sp"][:, :],
                                                      initial=0.0, op0=ALU.mult, op1=ALU.add))
            gop("dve", lambda e: e.scalar_tensor_tensor(out=G["a"][:, :], in0=G["I"][:, :], scalar=bias[:, 0:1],
                                                        in1=G["bneg"][:, :], op0=ALU.add, op1=ALU.add))
            gop("dve", lambda e: e.tensor_tensor_scan(out=G["cm"][:, :], data0=zerosf[0:NP, :], data1=G["a"][:, :],
                                                      initial=-1e30, op0=ALU.add, op1=ALU.max))
            pr = 4
            S.op("pe", lambda e: e.transpose(out=ps[pr][0:1, 0:NP], in_=G["bneg"][:, 127:128], identity=ident[0:NP, 0:NP]),
                 reads=[Gb, cbuf], writes=[psb[pr]], signal=False)
            S.op("pe", lambda e: e.transpose(out=ps[pr][0:1, NP:2 * NP], in_=G["cm"][:, 127:128], identity=ident[0:NP, 0:NP]),
                 reads=[Gb, cbuf], writes=[psb[pr]])
            def rop(eng, fn, extra=()):
                S.op(eng, fn, reads=[Gb, cbuf] + list(extra), writes=[Gb])
            rop("act", lambda e: e.activation(out=R[:, 0:2 * NP], in_=ps[pr][0:1, 0:2 * NP], func=AF.Copy), [psb[pr]])
            rop("dve", lambda e: e.tensor_scalar(out=R[:, 2 * NP:3 * NP], in0=R[:, 0:NP], scalar1=-1.0, scalar2=None, op0=ALU.mult))
            for h in range(4):
                rop("dve", lambda e: e.tensor_tensor_scan(out=R[:, 3 * NP + h * NT:3 * NP + (h + 1) * NT],
                                                          data0=R[:, NP + h * NT:NP + (h + 1) * NT],
                                                          data1=R[:, 2 * NP + h * NT:2 * NP + (h + 1) * NT],
                                                          initial=0.0, op0=ALU.max, op1=ALU.add))
            rop("dve", lambda e: e.memset(R[:, 4 * NP:5 * NP], 0.0))
            rop("dve", lambda e: e.tensor_copy(out=R[:, 4 * NP:5 * NP].rearrange("o (h c) -> o h c", h=4)[:, :, 1:NT],
                                               in_=R[:, 3 * NP:4 * NP].rearrange("o (h c) -> o h c", h=4)[:, :, 0:NT - 1]))
            rop("dve", lambda e: e.tensor_tensor(out=R[:, 5 * NP:6 * NP], in0=R[:, 4 * NP:5 * NP], in1=R[:, NP:2 * NP], op=ALU.max))
            rop("dve", lambda e: e.tensor_tensor(out=R[:, 6 * NP:7 * NP], in0=R[:, 4 * NP:5 * NP], in1=R[:, 5 * NP:6 * NP],
                                                 op=ALU.subtract))
            rop("act", lambda e: e.activation(out=R[:, 7 * NP:8 * NP], in_=R[:, 6 * NP:7 * NP], func=AF.Exp))
            pk = 5
            S.op("pe", lambda e: e.matmul(ps[pk][0:NP, 0:1], R[0:1, 4 * NP:5 * NP], onesf[0:1, 0:1], start=True, stop=True),
                 reads=[Gb, cbuf], writes=[psb[pk]], signal=False)
            S.op("pe", lambda e: e.matmul(ps[pk][0:NP, 1:2], R[0:1, 5 * NP:6 * NP], onesf[0:1, 0:1], start=True, stop=True),
                 reads=[Gb, cbuf], writes=[psb[pk]])
            rop("act", lambda e: e.activation(out=Kc[:, 0:2], in_=ps[pk][0:NP, 0:2], func=AF.Copy), [psb[pk]])
            rop("dve", lambda e: e.tensor_scalar(out=Kc[:, 2:3], in0=Kc[:, 1:2], scalar1=-1.0, scalar2=None, op0=ALU.mult))
            rop("dve", lambda e: e.tensor_scalar(out=G["Mx"][:, :], in0=G["cm"][:, :], scalar1=Kc[:, 0:1], scalar2=None, op0=ALU.max))
            rop("act", lambda e: e.activation(out=G["rowfac"][:, :], in_=G["Mx"][:, :], func=AF.Exp, bias=Kc[:, 1:2], scale=-1.0))
            rop("act", lambda e: e.activation(out=G["inter"][:, :], in_=G["Mx"][:, :], func=AF.Exp, bias=Kc[:, 0:1], scale=-1.0))
            rop("act", lambda e: e.activation(out=G["wkx"][:, :], in_=G["a"][:, :], func=AF.Exp, bias=Kc[:, 2:3], scale=1.0))
            rop("dve", lambda e: e.tensor_tensor(out=G["e1"][:, :], in0=G["bneg"][:, :], in1=G["Mx"][:, :], op=ALU.subtract))
            rop("act", lambda e: e.activation(out=G["en"][:, :], in_=G["e1"][:, :], func=AF.Exp))
            pt = 6
            for qi, nm in enumerate(("rowfac", "inter", "wkx", "en")):
                S.op("pe", lambda e: e.transpose(out=ps[pt][:, qi * NP:(qi + 1) * NP], in_=G[nm][:, :], identity=ident[0:NP, 0:NP]),
                     reads=[Gb, cbuf], writes=[psb[pt]], signal=(qi == 3))
            S.op("act", lambda e: e.activation(out=TS[:, :], in_=ps[pt][:, 0:4 * NP], func=AF.Copy), reads=[psb[pt]], writes=[TS_b])
            pd = 7
            S.op("pe", lambda e: e.matmul(ps[pd][:, 0:NP], onesf[0:1, :], R[0:1, 7 * NP:8 * NP], start=True, stop=True),
                 reads=[Gb, cbuf], writes=[psb[pd]])
            S.op("act", lambda e: e.activation(out=DEC[:, :], in_=ps[pd][:, 0:NP], func=AF.Copy), reads=[psb[pd]], writes=[DEC_b])
            S.fence()
        whd = sb("o_whd", [128, 8, 768], BF16); whd_b = S.bufs("whd", 4)
        pre = sb("o_pre", [128, 2, 3 + T], F32); pre_b = S.bufs("pre", 2)
        acc = sb("o_acc", [128, T], F32); acc_b = S.buf("acc")
        qkT = sb("o_qkT", [128, 2, T], BF16); qkT_b = S.bufs("qkT", 2)
        vone = sb("o_vone", [128, NT, 257], BF16); vone_b = S.bufs("vone", NT)
        sog = sb("o_sog", [128, NT, 256], BF16); sog_b = S.bufs("sog", NT)
        C32 = sb("o_C32", [128, 257], F32); C32_b = S.buf("C32")
        Cx = sb("o_Cx", [128, 257], BF16); Cx_b = S.buf("Cx")
        vext = [sb("o_vext%d" % i, [128, 257], BF16) for i in range(2)]; vext_b = S.bufs("vext", 2)
        sm = [sb("o_sm%d" % i, [128, 128], BF16) for i in range(2)]; sm_b = S.bufs("sm", 2)
        ktok = [sb("o_ktok%d" % i, [128, 128], BF16) for i in range(2)]; ktok_b = S.bufs("ktok", 2)
        o1 = [sb("o_o1%d" % i, [128, 257], F32) for i in range(2)]; o1_b = S.bufs("o1", 2)
        o2 = [sb("o_o2%d" % i, [128, 257], F32) for i in range(2)]; o2_b = S.bufs("o2", 2)
        sc = [sb("o_sc%d" % i, [128, 8], F32) for i in range(2)]; sc_b = S.bufs("sc", 2)
        jk = sb("o_jk", [128, 256], BF16); jk_b = S.buf("jk")
        yf = [sb("o_yf%d" % i, [128, 256], F32) for i in range(2)]; yf_b = S.bufs("yf", 2)
        yb = [sb("o_yb%d" % i, [128, 256], BF16) for i in range(2)]; yb_b = S.bufs("yb", 2)
        yTh = [sb("o_yTh%d" % i, [128, 2, 128], BF16) for i in range(2)]; yTh_b = S.bufs("yTh", 2)
        S.op("dve", lambda e: e.memset(pre[:, :, 0:3], 0.0), writes=pre_b)
        S.op("pool", lambda e: e.memset(vone[:, :, 256:257], 1.0), writes=vone_b)
        for h in range(4):
            srcs = ((h * 128, 128), (512 + h * 128, 128), (1024 + h * 256, 256), (2048 + h * 256, 256))
            off = 0
            for si, (c0, n) in enumerate(srcs):
                S.dma("pool", whd[:, :, off:off + n], winv[:, :, c0:c0 + n], writes=[whd_b[si]])
                off += n
            cnt = 0
            for w in range(2):
                for blk in range(NQB):
                    pb = cnt % 2; cnt += 1
                    for kc in range(8):
                        S.op("pe", lambda e: e.matmul(ps[pb][:, :], whd[:, kc, w * 128:(w + 1) * 128],
                                                      hT[:, kc, blk * 512:(blk + 1) * 512], start=(kc == 0), stop=(kc == 7)),
                             reads=[whd_b[w]] + hT_b[blk * 4:(blk + 1) * 4], writes=[psb[pb]], signal=(kc == 7))
                    S.op("act", lambda e: e.activation(out=pre[:, w, 3 + blk * 512:3 + (blk + 1) * 512], in_=ps[pb][:, :],
                                                       func=AF.Copy), reads=[psb[pb]], writes=[pre_b[w]])
                idx = w * 4 + h
                S.op("dve", lambda e: e.tensor_scalar(out=acc[:, :], in0=pre[:, w, 0:T], scalar1=cw[:, idx, 0:1], scalar2=None,
                                                      op0=ALU.mult), reads=[pre_b[w], cw_b], writes=[acc_b])
                for k in range(1, 4):
                    S.op("dve", lambda e: e.scalar_tensor_tensor(out=acc[:, :], in0=pre[:, w, k:k + T], scalar=cw[:, idx, k:k + 1],
                                                                 in1=acc[:, :], op0=ALU.mult, op1=ALU.add),
                         reads=[pre_b[w], cw_b, acc_b], writes=[acc_b])
                if w == 0:
                    S.op("act", lambda e: e.activation(out=qkT[:, 0, :], in_=acc[:, :], func=AF.Silu, bias=cbias[:, idx:idx + 1]),
                         reads=[acc_b, cbias_b], writes=[qkT_b[0]])
                else:
                    S.op("act", lambda e: e.activation(out=acc[:, :], in_=acc[:, :], func=AF.Silu, bias=cbias[:, idx:idx + 1]),
                         reads=[acc_b, cbias_b], writes=[acc_b])
                    S.op("dve", lambda e: e.tensor_scalar(out=qkT[:, 1, :], in0=acc[:, :], scalar1=KS, scalar2=None, op0=ALU.mult),
                         reads=[acc_b], writes=[qkT_b[1]])
            for i in range(NT):
                pb = 2 + i % 2
                for kc in range(8):
                    S.op("pe", lambda e: e.matmul(ps[pb][:, :], hT[:, kc, i * 128:(i + 1) * 128], whd[:, kc, 256:768],
                                                  start=(kc == 0), stop=(kc == 7)),
                         reads=[hT_b[i], whd_b[2], whd_b[3]], writes=[psb[pb]], signal=(kc == 7))
                S.op("act", lambda e: e.activation(out=vone[:, i, 0:256], in_=ps[pb][:, 0:256], func=AF.Copy),
                     reads=[psb[pb]], writes=[vone_b[i]])
                S.op("act", lambda e: e.activation(out=sog[:, i, :], in_=ps[pb][:, 256:512], func=AF.Sigmoid),
                     reads=[psb[pb]], writes=[sog_b[i]])
            S.op("dve", lambda e: e.memset(C32[:, :], 0.0), writes=[C32_b])
            S.op("pool", lambda e: e.memset(Cx[:, :], 0.0), writes=[Cx_b])
            for c in range(NT):
                p = h * NT + c
                k2 = c % 2
                cs = slice(c * 128, (c + 1) * 128)
                S.op("dve", lambda e: e.tensor_scalar(out=vext[k2][:, :], in0=vone[:, c, :], scalar1=TS[:, 2 * NP + p:2 * NP + p + 1],
                                                      scalar2=None, op0=ALU.mult), reads=[vone_b[c], TS_b], writes=[vext_b[k2]])
                S.op("pe", lambda e: e.matmul(ps[4][:, 0:128], qkT[:, 1, cs], qkT[:, 0, cs], start=True, stop=True),
                     reads=[qkT_b[0], qkT_b[1]], writes=[psb[4]])
                S.op("dve", lambda e: e.tensor_tensor(out=sm[k2][:, :], in0=ps[4][:, 0:128], in1=maskle[:, :], op=ALU.mult),
                     reads=[psb[4], cbuf], writes=[sm_b[k2]])
                S.op("pe", lambda e: e.matmul(ps[5][:, 0:257], sm[k2][:, :], vext[k2][:, :], start=True, stop=True),
                     reads=[sm_b[k2], vext_b[k2]], writes=[psb[5]])
                S.op("pe", lambda e: e.matmul(ps[6][:, 0:257], qkT[:, 0, cs], Cx[:, :], start=True, stop=True),
                     reads=[qkT_b[0], Cx_b], writes=[psb[6]])
                S.op("act", lambda e: e.activation(out=o1[k2][:, :], in_=ps[5][:, 0:257], func=AF.Identity,
                                                   scale=TS[:, p:p + 1]), reads=[psb[5], TS_b], writes=[o1_b[k2]])
                S.op("dve", lambda e: e.scalar_tensor_tensor(out=o2[k2][:, :], in0=ps[6][:, 0:257], scalar=TS[:, NP + p:NP + p + 1],
                                                             in1=o1[k2][:, :], op0=ALU.mult, op1=ALU.add),
                     reads=[psb[6], TS_b, o1_b[k2]], writes=[o2_b[k2]])
                pTk = ps[7][:, :].bitcast(BF16)
                S.op("pe", lambda e: e.transpose(out=pTk[:, 0:128], in_=qkT[:, 1, cs], identity=identb[:, :]),
                     reads=[qkT_b[1], cbuf], writes=[psb[7]])
                S.op("act", lambda e: e.activation(out=ktok[k2][:, :], in_=pTk[:, 0:128], func=AF.Copy),
                     reads=[psb[7]], writes=[ktok_b[k2]])
                S.op("pe", lambda e: e.matmul(ps[7][:, 128:385], ktok[k2][:, :], vext[k2][:, :], start=True, stop=True),
                     reads=[ktok_b[k2], vext_b[k2]], writes=[psb[7]])
                S.op("dve", lambda e: e.scalar_tensor_tensor(out=C32[:, :], in0=C32[:, :], scalar=DEC[:, p:p + 1],
                                                             in1=ps[7][:, 128:385], op0=ALU.mult, op1=ALU.add),
                     reads=[C32_b, DEC_b, psb[7]], writes=[C32_b])
                S.op("act", lambda e: e.activation(out=Cx[:, :], in_=C32[:, :], func=AF.Copy), reads=[C32_b], writes=[Cx_b])
                s_ = sc[k2]; s_b = sc_b[k2]
                S.op("dve", lambda e: e.tensor_scalar(out=s_[:, 7:8], in0=o2[k2][:, 256:257], scalar1=-1.0, scalar2=None,
                                                      op0=ALU.mult), reads=[o2_b[k2]], writes=[s_b])
                S.op("dve", lambda e: e.tensor_tensor(out=s_[:, 0:1], in0=o2[k2][:, 256:257], in1=s_[:, 7:8], op=ALU.max),
                     reads=[o2_b[k2], s_b], writes=[s_b])
                S.op("dve", lambda e: e.tensor_tensor(out=s_[:, 0:1], in0=s_[:, 0:1], in1=TS[:, 3 * NP + p:3 * NP + p + 1],
                                                      op=ALU.max), reads=[s_b, TS_b], writes=[s_b])
                S.op("dve", lambda e: e.reciprocal(out=s_[:, 1:2], in_=s_[:, 0:1]), reads=[s_b], writes=[s_b])
                S.op("act", lambda e: e.activation(out=jk[:, :], in_=o2[k2][:, 0:256], func=AF.Square, scale=s_[:, 1:2],
                                                   accum_out=s_[:, 2:3]), reads=[o2_b[k2], s_b], writes=[jk_b, s_b])
                S.op("dve", lambda e: e.tensor_scalar(out=s_[:, 3:4], in0=s_[:, 2:3], scalar1=1.0 / 256, scalar2=EPS,
                                                      op0=ALU.mult, op1=ALU.add), reads=[s_b], writes=[s_b])
                S.op("act", lambda e: e.sqrt(out=s_[:, 4:5], in_=s_[:, 3:4]), reads=[s_b], writes=[s_b])
                S.op("dve", lambda e: e.reciprocal(out=s_[:, 5:6], in_=s_[:, 4:5]), reads=[s_b], writes=[s_b])
                S.op("dve", lambda e: e.tensor_tensor(out=s_[:, 6:7], in0=s_[:, 5:6], in1=s_[:, 1:2], op=ALU.mult),
                     reads=[s_b], writes=[s_b])
                S.op("dve", lambda e: e.scalar_tensor_tensor(out=yf[k2][:, :], in0=o2[k2][:, 0:256], scalar=s_[:, 6:7],
                                                             in1=ongb[:, h * 256:(h + 1) * 256], op0=ALU.mult, op1=ALU.mult),
                     reads=[o2_b[k2], s_b, ong_b], writes=[yf_b[k2]])
                S.op("dve", lambda e: e.tensor_tensor(out=yb[k2][:, :], in0=yf[k2][:, :], in1=sog[:, c, :], op=ALU.mult),
                     reads=[yf_b[k2], sog_b[c]], writes=[yb_b[k2]])
                pTy = ps[4][:, :].bitcast(BF16)
                for kk in range(2):
                    S.op("pe", lambda e: e.transpose(out=pTy[:, 512 + kk * 128:512 + (kk + 1) * 128],
                                                     in_=yb[k2][:, kk * 128:(kk + 1) * 128], identity=identb[:, :]),
                         reads=[yb_b[k2], cbuf], writes=[psb[4]], signal=(kk == 1))
                S.op("act", lambda e: e.activation(out=yTh[k2][:, :, :], in_=pTy[:, 512:768].rearrange("p (k t) -> p k t", k=2),
                                                   func=AF.Copy), reads=[psb[4]], writes=[yTh_b[k2]])
                for dh in range(2):
                    pb = dh
                    for kk in range(2):
                        S.op("pe", lambda e: e.matmul(ps[pb][:, :], yTh[k2][:, kk, :], wout[:, 2 * h + kk, dh * 512:(dh + 1) * 512],
                                                      start=(kk == 0), stop=(kk == 1)),
                             reads=[yTh_b[k2], wout_b[dh]], writes=[psb[pb]], signal=(kk == 1))
                    S.op("dve", lambda e: e.tensor_tensor(out=X[:, c, dh * 512:(dh + 1) * 512], in0=ps[pb][:, :],
                                                          in1=X[:, c, dh * 512:(dh + 1) * 512], op=ALU.add),
                         reads=[psb[pb], Xb[c]], writes=[Xb[c]])
        S.fence()


def load_bcast(S, q, dst_tile, dst_buf, src_row_ap, n):
    S.dma(q, dst_tile[:, :], src_row_ap.partition_broadcast(128), writes=[dst_buf])

def mlp_block(S, nc, C, X, Xb, tiles, g_row, w1, w2):
    ps, psb = C["ps"], C["psb"]
    ident, identb = C["ident"], C["identb"]
    NTL = len(tiles)
    TB = NTL * 128
    NH = TB // 512
    with ExitStack() as es:
        def sb(name, shape, dt):
            return es.enter_context(nc.sbuf_tensor(uname(name), shape, dt))
        gbc = sb("m_gbc", [128, 1024], F32); gbc_b = S.buf("gbc")
        hT = sb("m_hT", [128, 8, TB], BF16); hT_b = S.bufs("hT", NTL)
        h1T = sb("m_h1T", [128, 32, TB], BF16); h1T_b = [[S.buf("h1T") for _ in range(NH)] for _ in range(32)]
        junk = sb("m_junk", [128, 1024], BF16); junk_b = S.buf("junk")
        xs = [sb("m_xs%d" % i, [128, 1024], BF16) for i in range(2)]; xs_b = S.bufs("xs", 2)
        st = [sb("m_st%d" % i, [128, 4], F32) for i in range(2)]; st_b = S.bufs("st", 2)
        rl = [sb("m_rl%d" % i, [128, 512], BF16) for i in range(2)]; rl_b = S.bufs("rl", 2)
        w1t = [sb("m_w1_%d" % i, [128, 8, 512], BF16) for i in range(2)]; w1_b = S.bufs("w1t", 2)
        w2t = [sb("m_w2_%d" % i, [128, 4, 512], BF16) for i in range(3)]; w2_b = S.bufs("w2t", 3)

        S.dma("sp", gbc[:, :], g_row.partition_broadcast(128), writes=[gbc_b])
        w1v = w1.rearrange("(kc p) f -> p kc f", p=128)
        def load_w1(blk):
            S.dma("pool", w1t[blk % 2][:, :, :], w1v[:, :, blk * 512:(blk + 1) * 512], writes=[w1_b[blk % 2]])
        load_w1(0); load_w1(1)
        for j, ti in enumerate(tiles):
            k = j % 2
            S.op("act", lambda e: e.activation(out=junk[:, :], in_=X[:, ti, :], func=AF.Square,
                                               accum_out=st[k][:, 0:1]),
                 reads=[Xb[ti]], writes=[junk_b, st_b[k]])
            S.op("dve", lambda e: e.tensor_scalar(out=st[k][:, 1:2], in0=st[k][:, 0:1], scalar1=1.0 / 1024,
                                                  scalar2=EPS, op0=ALU.mult, op1=ALU.add),
                 reads=[st_b[k]], writes=[st_b[k]])
            S.op("act", lambda e: e.sqrt(out=st[k][:, 2:3], in_=st[k][:, 1:2]), reads=[st_b[k]], writes=[st_b[k]])
            S.op("dve", lambda e: e.reciprocal(out=st[k][:, 3:4], in_=st[k][:, 2:3]), reads=[st_b[k]], writes=[st_b[k]])
            S.op("dve", lambda e: e.scalar_tensor_tensor(out=xs[k][:, :], in0=X[:, ti, :], scalar=st[k][:, 3:4],
                                                         in1=gbc[:, :], op0=ALU.mult, op1=ALU.mult),
                 reads=[Xb[ti], st_b[k], gbc_b], writes=[xs_b[k]])
            pb = j % 2
            pT = ps[pb][:, :].bitcast(BF16)
            for c in range(8):
                S.op("pe", lambda e: e.transpose(out=pT[:, c * 128:(c + 1) * 128], in_=xs[k][:, c * 128:(c + 1) * 128],
                                                 identity=identb[:, :]),
                     reads=[xs_b[k]], writes=[psb[pb]], signal=(c == 7))
            S.op("act", lambda e: e.activation(out=hT[:, :, j * 128:(j + 1) * 128],
                                               in_=pT.rearrange("p (c t) -> p c t", c=8), func=AF.Copy),
                 reads=[psb[pb]], writes=[hT_b[j]])
        cnt = 0
        for blk in range(8):
            wt = w1t[blk % 2]; wb = w1_b[blk % 2]
            for fi in range(4):
                fc = blk * 4 + fi
                for th in range(NH):
                    pb = 2 + (cnt % 3); cnt += 1
                    for kc in range(8):
                        S.op("pe", lambda e: e.matmul(ps[pb][:, :], wt[:, kc, fi * 128:(fi + 1) * 128],
                                                      hT[:, kc, th * 512:(th + 1) * 512], start=(kc == 0), stop=(kc == 7)),
                             reads=[wb] + hT_b[th * 4:(th + 1) * 4], writes=[psb[pb]], signal=(kc == 7))
                    k = cnt % 2
                    S.op("act", lambda e: e.activation(out=rl[k][:, :], in_=ps[pb][:, :], func=AF.Relu),
                         reads=[psb[pb]], writes=[rl_b[k]])
                    S.op("dve", lambda e: e.tensor_tensor(out=h1T[:, fc, th * 512:(th + 1) * 512], in0=rl[k][:, :],
                                                          in1=rl[k][:, :], op=ALU.mult),
                         reads=[rl_b[k]], writes=[h1T_b[fc][th]])
            if blk + 2 < 8:
                load_w1(blk + 2)
        w2v = w2.rearrange("(g fi p) d -> g p fi d", p=128, fi=4)
        nload = [0]
        def load_w2(idx):
            dh, g = divmod(idx, 8)
            S.dma("pool", w2t[idx % 3][:, :, :], w2v[g, :, :, dh * 512:(dh + 1) * 512], writes=[w2_b[idx % 3]])
        for i in range(3):
            load_w2(i)
        for dh in range(2):
            for g in range(8):
                idx = dh * 8 + g
                wt = w2t[idx % 3]; wb = w2_b[idx % 3]
                for fi in range(4):
                    fc = g * 4 + fi
                    for j in range(NTL):
                        th = j // 4
                        S.op("pe", lambda e: e.matmul(ps[j][:, :], h1T[:, fc, j * 128:(j + 1) * 128], wt[:, fi, :],
                                                      start=(fc == 0), stop=(fc == 31)),
                             reads=[wb, h1T_b[fc][th]], writes=[psb[j]], signal=(fc == 31 or fi == 3))
                if idx + 3 < 16:
                    load_w2(idx + 3)
            for j, ti in enumerate(tiles):
                S.op("dve", lambda e: e.tensor_tensor(out=X[:, ti, dh * 512:(dh + 1) * 512], in0=ps[j][:, :],
                                                      in1=X[:, ti, dh * 512:(dh + 1) * 512], op=ALU.add),
                     reads=[psb[j], Xb[ti]], writes=[Xb[ti]])
        S.fence()

T_SEQ = 2048
NSEQ_CORE = 2
N_CORES = 8
IN_SHAPES = {
    'mix_norm_g': [4, 1024], 'mlp_norm_g': [4, 1024], 'mlp_w1': [4, 1024, 4096], 'mlp_w2': [4, 4096, 1024],
    'ev_w_in': [2, 1024, 2560], 'ev_w_out': [2, 1024, 1024], 'sg_ln_g': [2, 512], 'sg_ln_b': [2, 512],
    'sg_w': [2, 8, 128, 128], 'sg_b': [2, 8, 128], 'sb_q_norm_g': [2, 64], 'sb_k_norm_g': [2, 64],
    'od_w_in': [2, 1024, 3080], 'od_conv_w': [2, 4, 1024], 'od_conv_b': [2, 1024], 'od_i_b': [2, 4], 'od_f_b': [2, 4],
    'od_out_norm_g': [2, 1024], 'od_w_out': [2, 1024, 1024],
}


def build_program(T=T_SEQ, nseq=NSEQ_CORE, layers=(0, 1, 2, 3)):
    NT = T // 128
    nc = bass.Bass("TRN2", target_bir_lowering=False)
    x = nc.dram_tensor("x", [nseq, T, 1024], F32, kind="ExternalInput").ap()
    W = {k: nc.dram_tensor(k, shp, F32, kind="ExternalInput").ap() for k, shp in IN_SHAPES.items()}
    y = nc.dram_tensor("y", [nseq, T, 1024], F32, kind="ExternalOutput").ap()
    gs = nc.dram_tensor("gs_scratch", [8, T], F32, kind="Internal").ap()
    with ExitStack() as es:
        S = Sched(nc, es)
        C = {}
        C["ps"] = [es.enter_context(nc.psum_tensor("ps%d" % i, [128, 512], F32)) for i in range(8)]
        C["psb"] = S.bufs("ps", 8)
        make_consts(S, nc, es, C)
        X = es.enter_context(nc.sbuf_tensor("X", [128, NT, 1024], F32))
        Xb = S.bufs("X", NT, persist=True)
        for s in range(nseq):
            xv = x[s].rearrange("(n p) d -> p n d", p=128)
            yv = y[s].rearrange("(n p) d -> p n d", p=128)
            for i in range(NT):
                S.dma("sp", X[:, i, :], xv[:, i, :], writes=[Xb[i]])
            for l in layers:
                j = l // 2
                if l % 2 == 0:
                    P = dict(g=W['mix_norm_g'][l], w_in=W['ev_w_in'][j], w_out=W['ev_w_out'][j], ln_g=W['sg_ln_g'][j],
                             ln_b=W['sg_ln_b'][j], sg_w=W['sg_w'][j], sg_b=W['sg_b'][j], qg=W['sb_q_norm_g'][j],
                             kg=W['sb_k_norm_g'][j])
                    even_mixer(S, nc, C, X, Xb, NT, P)
                else:
                    P = dict(g=W['mix_norm_g'][l], w_in=W['od_w_in'][j], w_out=W['od_w_out'][j], conv_w=W['od_conv_w'][j],
                             conv_b=W['od_conv_b'][j], i_b=W['od_i_b'][j], f_b=W['od_f_b'][j], on_g=W['od_out_norm_g'][j])
                    odd_mixer(S, nc, C, X, Xb, NT, P, gs)
                for t0 in range(0, NT, 8):
                    tiles = list(range(t0, min(t0 + 8, NT)))
                    mlp_block(S, nc, C, X, Xb, tiles, W['mlp_norm_g'][l], W['mlp_w1'][l], W['mlp_w2'][l])
            for i in range(NT):
                S.dma("sp", yv[:, i, :], X[:, i, :], reads=[Xb[i]])
        S.finish()
    return nc


def kernel(**inputs):
    x = np.ascontiguousarray(np.asarray(inputs['x'], dtype=np.float32))
    B = x.shape[0]
    per = B // N_CORES
    nc = build_program(T=x.shape[1], nseq=per)
    shared = {k: np.ascontiguousarray(np.asarray(inputs[k], dtype=np.float32)) for k in IN_SHAPES}
    in_maps = []
    for c in range(N_CORES):
        m = dict(shared)
        m['x'] = np.ascontiguousarray(x[c * per:(c + 1) * per])
        in_maps.append(m)
    res = run_bass_kernel_spmd(nc, in_maps, core_ids=list(range(N_CORES)))
    return np.concatenate([np.asarray(r['y']) for r in res.results], axis=0).astype(np.float32)
```

```python
from contextlib import ExitStack
from concourse.bass_utils import run_bass_kernel_spmd
import numpy as np
import concourse.bass as bass
import concourse.mybir as mybir

F32 = mybir.dt.float32
BF16 = mybir.dt.bfloat16
AF = mybir.ActivationFunctionType
ALU = mybir.AluOpType
AX = mybir.AxisListType


_UID = [0]


def uname(name):
    _UID[0] += 1
    return "%s_%d" % (name, _UID[0])


class Buf:
    __slots__ = ("name", "w", "r", "sem", "semval", "persist")

    def __init__(self, name, fence, persist=False):
        self.persist = persist
        self.name = name
        self.w = None
        self.r = list(fence)
        self.sem = None
        self.semval = 0


class EngState:
    def __init__(self, name, obj, sem):
        self.name = name
        self.obj = obj
        self.sem = sem
        self.count = 0
        self.know = {}
        self.last = None


class Sched:
    def __init__(self, nc, stack):
        self.nc = nc
        self.stack = stack
        self.E = {}
        for name, obj in (("pe", nc.tensor), ("act", nc.scalar), ("dve", nc.vector),
                          ("pool", nc.gpsimd), ("sp", nc.sync)):
            sem = stack.enter_context(nc.semaphore("sem_" + name))
            self.E[name] = EngState(name, obj, sem)
        self.fence_recs = []
        self.dma_recs = []
        self.pending_pe = []
        self.sem_pool = {}
        self.phase_bufs = []
        self.nsem = 0
        self.nwaits = 0
        self.nops = 0

    def buf(self, name, persist=False):
        return Buf(name, self.fence_recs, persist)

    def bufs(self, name, n, persist=False):
        return [Buf("%s%d" % (name, i), self.fence_recs, persist) for i in range(n)]

    def _waits(self, eng, reads, writes):
        st = self.E[eng]
        deps = []
        for b in reads:
            if b.w is not None:
                deps.append(b.w)
        for b in writes:
            if b.w is not None:
                deps.append(b.w)
            deps.extend(b.r)
        pesem = self.E["pe"].sem
        seen = set()
        for rec in deps:
            if id(rec) in seen:
                continue
            seen.add(id(rec))
            s, v, clk = rec
            if eng == "pe" and s is pesem:
                continue
            assert v is not None, "dependency on unsignaled PE op"
            if st.know.get(s, 0) >= v:
                continue
            st.obj.wait_ge(s, v)
            self.nwaits += 1
            for ks, kv in clk.items():
                if st.know.get(ks, 0) < kv:
                    st.know[ks] = kv

    def op(self, eng, fn, reads=(), writes=(), signal=True):
        st = self.E[eng]
        self._waits(eng, reads, writes)
        ins = fn(st.obj)
        self.nops += 1
        if signal:
            st.count += 1
            ins.then_inc(st.sem, 1)
            clk = dict(st.know)
            clk[st.sem] = st.count
            rec = [st.sem, st.count, clk]
            if eng == "pe":
                for p in self.pending_pe:
                    p[1] = st.count
                    p[2] = clk
                self.pending_pe = []
            st.last = rec
        else:
            assert eng == "pe"
            rec = [st.sem, None, None]
            self.pending_pe.append(rec)
        for b in reads:
            b.r.append(rec)
        for b in writes:
            b.w = rec
            b.r = []
        return ins

    def dma(self, q, out, in_, reads=(), writes=(), **kw):
        st = self.E[q]
        self._waits(q, reads, writes)
        owner = writes[0] if writes else reads[0]
        if owner.sem is None:
            pool = self.sem_pool.setdefault(q, [])
            if pool and not owner.persist:
                owner.sem, owner.semval = pool.pop()
            else:
                self.nsem += 1
                owner.sem = self.stack.enter_context(self.nc.semaphore("dsem%d" % self.nsem))
            if not owner.persist:
                self.phase_bufs.append((owner, q))
        owner.semval += 16
        ins = st.obj.dma_start(out=out, in_=in_, **kw)
        ins.then_inc(owner.sem, 16)
        clk = dict(st.know)
        clk[owner.sem] = owner.semval
        rec = [owner.sem, owner.semval, clk]
        self.dma_recs.append(rec)
        for b in reads:
            b.r.append(rec)
        for b in writes:
            b.w = rec
            b.r = []
        return ins

    def fence(self):
        sp = self.E["sp"]
        for rec in self.dma_recs:
            s, v, clk = rec
            if sp.know.get(s, 0) >= v:
                continue
            sp.obj.wait_ge(s, v)
            for ks, kv in clk.items():
                if sp.know.get(ks, 0) < kv:
                    sp.know[ks] = kv
        self.dma_recs = []
        for b, bq in self.phase_bufs:
            self.sem_pool.setdefault(bq, []).append((b.sem, b.semval))
            b.sem = None
        self.phase_bufs = []
        ins = sp.obj.nop()
        sp.count += 1
        ins.then_inc(sp.sem, 1)
        clk = dict(sp.know)
        clk[sp.sem] = sp.count
        sp.last = [sp.sem, sp.count, clk]
        assert not self.pending_pe
        recs = []
        for name, st in self.E.items():
            if st.last is not None:
                recs.append(st.last)
        self.fence_recs = recs

    def finish(self):
        self.fence()


class Recorder:
    def __init__(self):
        self.calls = []

    def op(self, eng, fn, reads=(), writes=(), signal=True):
        self.calls.append((eng, fn, list(reads), list(writes), signal))


def interleave(S, recs):
    lists = [r.calls for r in recs if r.calls]
    pos = [0] * len(lists)
    total = sum(len(l) for l in lists)
    done = 0
    while done < total:
        best, bf = None, None
        for t, l in enumerate(lists):
            if pos[t] >= len(l):
                continue
            frac = pos[t] / len(l)
            if bf is None or frac < bf:
                best, bf = t, frac
        l = lists[best]
        while True:
            eng, fn, reads, writes, signal = l[pos[best]]
            S.op(eng, fn, reads, writes, signal)
            pos[best] += 1
            done += 1
            if signal or pos[best] >= len(l):
                break


EPS = 1e-6

def rstd_ops(S, st, st_b, n, inv_n):
    S.op("dve", lambda e: e.tensor_scalar(out=st[:, n:2 * n], in0=st[:, 0:n], scalar1=inv_n, scalar2=EPS,
                                          op0=ALU.mult, op1=ALU.add), reads=[st_b], writes=[st_b])
    S.op("act", lambda e: e.sqrt(out=st[:, 2 * n:3 * n], in_=st[:, n:2 * n]), reads=[st_b], writes=[st_b])
    S.op("dve", lambda e: e.reciprocal(out=st[:, 3 * n:4 * n], in_=st[:, 2 * n:3 * n]), reads=[st_b], writes=[st_b])


def norm_transpose(S, C, X, Xb, ti, gbc, gbc_b, tmp, k, pb, out_ap, out_b):
    ps, psb, identb = C["ps"], C["psb"], C["identb"]
    junk, junk_b, xs, xs_b, st, st_b = tmp
    S.op("act", lambda e: e.activation(out=junk[:, :], in_=X[:, ti, :], func=AF.Square, accum_out=st[k][:, 0:1]),
         reads=[Xb[ti]], writes=[junk_b, st_b[k]])
    rstd_ops(S, st[k], st_b[k], 1, 1.0 / 1024)
    S.op("dve", lambda e: e.scalar_tensor_tensor(out=xs[k][:, :], in0=X[:, ti, :], scalar=st[k][:, 3:4],
                                                 in1=gbc[:, :], op0=ALU.mult, op1=ALU.mult),
         reads=[Xb[ti], st_b[k], gbc_b], writes=[xs_b[k]])
    pT = ps[pb][:, :].bitcast(BF16)
    for c in range(8):
        S.op("pe", lambda e: e.transpose(out=pT[:, c * 128:(c + 1) * 128], in_=xs[k][:, c * 128:(c + 1) * 128],
                                         identity=identb[:, :]),
             reads=[xs_b[k], C["cb"]], writes=[psb[pb]], signal=(c == 7))
    S.op("act", lambda e: e.activation(out=out_ap, in_=pT.rearrange("p (c t) -> p c t", c=8), func=AF.Copy),
         reads=[psb[pb]], writes=[out_b])


def even_mixer(S, nc, C, X, Xb, NT, P):
    ps, psb = C["ps"], C["psb"]
    ident, identb = C["ident"], C["identb"]
    T = NT * 128
    NQB = T // 512
    with ExitStack() as es:
        def sb(name, shape, dt):
            return es.enter_context(nc.sbuf_tensor(uname(name), shape, dt))
        yTa = sb("e_yTa", [128, 4, T], BF16); yT_b = [[S.buf("yT") for _ in range(NT)] for _ in range(8)]
        qT = sb("e_qT", [128, 4, T], BF16); qT_b = S.bufs("qT", NT)
        kT = sb("e_kT", [128, 4, T], BF16); kT_b = S.bufs("kT", NT)
        v = sb("e_v", [128, NT, 512], BF16); v_b = S.bufs("v", NT)
        woutv = P["w_out"].rearrange("(kc p) d -> p kc d", p=128)
        with ExitStack() as es1:
            def sb1(name, shape, dt):
                return es1.enter_context(nc.sbuf_tensor(uname(name), shape, dt))
            win = sb1("e_win", [128, 8, 2560], BF16); win_b = S.bufs("win", 5)
            winv = P["w_in"].rearrange("(kc p) f -> p kc f", p=128)
            for cb in range(5):
                S.dma("pool", win[:, :, cb * 512:(cb + 1) * 512], winv[:, :, cb * 512:(cb + 1) * 512], writes=[win_b[cb]])
            gbc = sb1("e_gbc", [128, 1024], F32); gbc_b = S.buf("gbc")
            lng = sb1("e_lng", [128, 512], F32); lng_b = S.buf("lng")
            lnb = sb1("e_lnb", [128, 512], F32); lnb_b = S.buf("lnb")
            qg = sb1("e_qg", [128, 64], F32); qg_b = S.buf("qg")
            kg = sb1("e_kg", [128, 64], F32); kg_b = S.buf("kg")
            sgb = sb1("e_sgb", [128, 8], F32); sgb_b = S.buf("sgb")
            sgw = sb1("e_sgw", [128, 8, 128], BF16); sgw_b = S.buf("sgw")
            wcT = sb1("e_wcT", [128, 8, 128], BF16); wcT_b = S.buf("wcT")
            S.dma("sp", gbc[:, :], P["g"].partition_broadcast(128), writes=[gbc_b])
            S.dma("sp", lng[:, :], P["ln_g"].partition_broadcast(128), writes=[lng_b])
            S.dma("sp", lnb[:, :], P["ln_b"].partition_broadcast(128), writes=[lnb_b])
            S.dma("sp", qg[:, :], P["qg"].partition_broadcast(128), writes=[qg_b])
            S.dma("sp", kg[:, :], P["kg"].partition_broadcast(128), writes=[kg_b])
            S.dma("sp", sgb[:, :], P["sg_b"].rearrange("g t -> t g"), writes=[sgb_b], allow_slow_non_contiguous=True)
            S.dma("pool", sgw[:, :, :], P["sg_w"].rearrange("g t s -> t g s"), writes=[sgw_b])
            S.op("dve", lambda e: e.tensor_scalar(out=qg[:, :], in0=qg[:, :], scalar1=0.125, scalar2=None, op0=ALU.mult),
                 reads=[qg_b], writes=[qg_b])
            S.op("pool", lambda e: e.affine_select(out=sgw[:, :, :], in_=sgw[:, :, :], pattern=[[0, 8], [-1, 128]],
                                                   compare_op=ALU.is_ge, fill=0.0, base=0, channel_multiplier=1),
                 reads=[sgw_b], writes=[sgw_b])
            for half in range(2):
                for gi in range(4):
                    g_ = half * 4 + gi
                    S.op("pe", lambda e: e.transpose(out=ps[half][:, :].bitcast(BF16)[:, gi * 128:(gi + 1) * 128], in_=sgw[:, g_, :],
                                                     identity=identb[:, :]),
                         reads=[sgw_b, C["cb"]], writes=[psb[half]], signal=(gi == 3))
                S.op("act", lambda e: e.activation(out=wcT[:, half * 4:(half + 1) * 4, :],
                                                   in_=ps[half][:, :].bitcast(BF16)[:, 0:512].rearrange("p (g t) -> p g t", g=4), func=AF.Copy),
                     reads=[psb[half]], writes=[wcT_b])
            junk = sb1("e_junk", [128, 1024], BF16); junk_b = S.buf("junk")
            xs = [sb1("e_xs%d" % i, [128, 1024], BF16) for i in range(2)]; xs_b = S.bufs("xs", 2)
            st = [sb1("e_st%d" % i, [128, 4], F32) for i in range(2)]; st_b = S.bufs("st", 2)
            tmpn = (junk, junk_b, xs, xs_b, st, st_b)
            hTt = [sb1("e_hTt%d" % i, [128, 8, 128], BF16) for i in range(2)]; hTt_b = S.bufs("hTt", 2)
            u = [sb1("e_u%d" % i, [128, 512], BF16) for i in range(1)]; u_b = S.bufs("u", 1)
            vg = [sb1("e_vg%d" % i, [128, 512], F32) for i in range(1)]; vg_b = S.bufs("vg", 1)
            vgn = [sb1("e_vgn%d" % i, [128, 512], BF16) for i in range(1)]; vgn_b = S.bufs("vgn", 1)
            lst = [sb1("e_lst%d" % i, [128, 8], F32) for i in range(1)]; lst_b = S.bufs("lst", 1)
            ya = [sb1("e_ya%d" % i, [128, 512], F32) for i in range(1)]; ya_b = S.bufs("ya", 1)
            yab = [sb1("e_yab%d" % i, [128, 512], BF16) for i in range(1)]; yab_b = S.bufs("yab", 1)
            sq = [sb1("e_sq%d" % i, [128, 512], F32) for i in range(1)]; sq_b = S.bufs("sq", 1)
            qst = [sb1("e_qst%d" % i, [128, 32], F32) for i in range(1)]; qst_b = S.bufs("qst", 1)
            qn = [sb1("e_qn%d" % i, [128, 512], BF16) for i in range(1)]; qn_b = S.bufs("qn", 1)
            for i in range(NT):
                k = 0
                kn = i % 2
                norm_transpose(S, C, X, Xb, i, gbc, gbc_b, tmpn, kn, i % 2, hTt[kn][:, :, :], hTt_b[kn])
                for cb in range(5):
                    for kc in range(8):
                        S.op("pe", lambda e: e.matmul(ps[2 + cb][:, :], hTt[kn][:, kc, :], win[:, kc, cb * 512:(cb + 1) * 512],
                                                      start=(kc == 0), stop=(kc == 7)),
                             reads=[hTt_b[kn], win_b[cb]], writes=[psb[2 + cb]], signal=(kc == 7))
                S.op("act", lambda e: e.activation(out=u[k][:, :], in_=ps[2][:, :], func=AF.Gelu_apprx_tanh),
                     reads=[psb[2]], writes=[u_b[k]])
                S.op("act", lambda e: e.activation(out=vg[k][:, :], in_=ps[3][:, :], func=AF.Gelu_apprx_tanh,
                                                   accum_out=lst[k][:, 0:1]),
                     reads=[psb[3]], writes=[vg_b[k], lst_b[k]])
                S.op("act", lambda e: e.activation(out=junk[:, 0:512], in_=vg[k][:, :], func=AF.Square,
                                                   accum_out=lst[k][:, 1:2]),
                     reads=[vg_b[k], lst_b[k]], writes=[junk_b, lst_b[k]])
                L = lst[k]; Lb = lst_b[k]
                S.op("dve", lambda e: e.tensor_scalar(out=L[:, 2:4], in0=L[:, 0:2], scalar1=1.0 / 512, scalar2=None,
                                                      op0=ALU.mult), reads=[Lb], writes=[Lb])
                S.op("dve", lambda e: e.tensor_tensor(out=L[:, 4:5], in0=L[:, 2:3], in1=L[:, 2:3], op=ALU.mult),
                     reads=[Lb], writes=[Lb])
                S.op("dve", lambda e: e.scalar_tensor_tensor(out=L[:, 5:6], in0=L[:, 3:4], scalar=EPS, in1=L[:, 4:5],
                                                             op0=ALU.add, op1=ALU.subtract), reads=[Lb], writes=[Lb])
                S.op("act", lambda e: e.sqrt(out=L[:, 6:7], in_=L[:, 5:6]), reads=[Lb], writes=[Lb])
                S.op("dve", lambda e: e.reciprocal(out=L[:, 7:8], in_=L[:, 6:7]), reads=[Lb], writes=[Lb])
                S.op("dve", lambda e: e.scalar_tensor_tensor(out=L[:, 4:5], in0=L[:, 2:3], scalar=-1.0, in1=L[:, 7:8],
                                                             op0=ALU.mult, op1=ALU.mult), reads=[Lb], writes=[Lb])
                S.op("act", lambda e: e.activation(out=vg[k][:, :], in_=vg[k][:, :], func=AF.Identity,
                                                   bias=L[:, 4:5], scale=L[:, 7:8]),
                     reads=[vg_b[k], Lb], writes=[vg_b[k]])
                S.op("dve", lambda e: e.tensor_tensor(out=vg[k][:, :], in0=vg[k][:, :], in1=lng[:, :], op=ALU.mult),
                     reads=[vg_b[k], lng_b], writes=[vg_b[k]])
                S.op("dve", lambda e: e.tensor_tensor(out=vgn[k][:, :], in0=vg[k][:, :], in1=lnb[:, :], op=ALU.add),
                     reads=[vg_b[k], lnb_b], writes=[vgn_b[k]])
                for g_ in range(8):
                    S.op("pe", lambda e: e.matmul(ps[7][:, g_ * 64:(g_ + 1) * 64], wcT[:, g_, :], vgn[k][:, g_ * 64:(g_ + 1) * 64],
                                                  start=True, stop=True),
                         reads=[wcT_b, vgn_b[k]], writes=[psb[7]], signal=(g_ == 7))
                S.op("dve", lambda e: e.tensor_tensor(out=ya[k][:, :].rearrange("p (g c) -> p g c", g=8),
                                                      in0=ps[7][:, :].rearrange("p (g c) -> p g c", g=8),
                                                      in1=sgb[:, :].unsqueeze(2).to_broadcast([128, 8, 64]), op=ALU.add),
                     reads=[psb[7], sgb_b], writes=[ya_b[k]])
                S.op("dve", lambda e: e.tensor_tensor(out=yab[k][:, :], in0=ya[k][:, :], in1=u[k][:, :], op=ALU.mult),
                     reads=[ya_b[k], u_b[k]], writes=[yab_b[k]])
                pT = ps[7][:, :].bitcast(BF16)
                for c in range(4):
                    S.op("pe", lambda e: e.transpose(out=pT[:, c * 128:(c + 1) * 128], in_=yab[k][:, c * 128:(c + 1) * 128],
                                                     identity=identb[:, :]),
                         reads=[yab_b[k]], writes=[psb[7]], signal=(c == 3))
                S.op("act", lambda e: e.activation(out=yTa[:, 0:4, i * 128:(i + 1) * 128],
                                                   in_=pT[:, 0:512].rearrange("p (c t) -> p c t", c=4), func=AF.Copy),
                     reads=[psb[7]], writes=[yT_b[c][i] for c in range(4)])
                for which, bank, gt, gt_b, dstT, dstT_b in ((0, 4, qg, qg_b, qT, qT_b), (1, 5, kg, kg_b, kT, kT_b)):
                    kk = 0
                    Q = qst[kk]; Qb = qst_b[kk]
                    S.op("act", lambda e: e.activation(out=sq[kk][:, :], in_=ps[bank][:, :], func=AF.Square),
                         reads=[psb[bank]], writes=[sq_b[kk]])
                    S.op("dve", lambda e: e.tensor_reduce(out=Q[:, 0:8], in_=sq[kk][:, :].rearrange("p (h d) -> p h d", h=8),
                                                          axis=AX.X, op=ALU.add), reads=[sq_b[kk]], writes=[Qb])
                    rstd_ops(S, Q, Qb, 8, 1.0 / 64)
                    S.op("dve", lambda e: e.tensor_tensor(out=sq[kk][:, :].rearrange("p (h d) -> p h d", h=8),
                                                          in0=ps[bank][:, :].rearrange("p (h d) -> p h d", h=8),
                                                          in1=Q[:, 24:32].unsqueeze(2).to_broadcast([128, 8, 64]), op=ALU.mult),
                         reads=[psb[bank], Qb, sq_b[kk]], writes=[sq_b[kk]])
                    S.op("dve", lambda e: e.tensor_tensor(out=qn[kk][:, :].rearrange("p (h d) -> p h d", h=8),
                                                          in0=sq[kk][:, :].rearrange("p (h d) -> p h d", h=8),
                                                          in1=gt[:, :].unsqueeze(1).to_broadcast([128, 8, 64]), op=ALU.mult),
                         reads=[sq_b[kk], gt_b], writes=[qn_b[kk]])
                    pT2 = ps[bank][:, :].bitcast(BF16)
                    for c in range(4):
                        S.op("pe", lambda e: e.transpose(out=pT2[:, c * 128:(c + 1) * 128], in_=qn[kk][:, c * 128:(c + 1) * 128],
                                                         identity=identb[:, :]),
                             reads=[qn_b[kk]], writes=[psb[bank]], signal=(c == 3))
                    S.op("act", lambda e: e.activation(out=dstT[:, :, i * 128:(i + 1) * 128],
                                                       in_=pT2[:, 0:512].rearrange("p (c t) -> p c t", c=4), func=AF.Copy),
                         reads=[psb[bank]], writes=[dstT_b[i]])
                S.op("act", lambda e: e.activation(out=v[:, i, :], in_=ps[6][:, :], func=AF.Copy),
                     reads=[psb[6]], writes=[v_b[i]])
            S.fence()
        with ExitStack() as es2:
            def sb2(name, shape, dt):
                return es2.enter_context(nc.sbuf_tensor(uname(name), shape, dt))
            yTb = sb2("e_yTb", [128, 4, T], BF16)
            wout = sb2("e_wout", [128, 8, 1024], BF16); wout_b = S.bufs("wout", 2)
            for dh in range(2):
                S.dma("pool", wout[:, :, dh * 512:(dh + 1) * 512], woutv[:, :, dh * 512:(dh + 1) * 512], writes=[wout_b[dh]])
            e_sb = [sb2("a_e%d" % i, [128, 512], BF16) for i in range(4)]; e_b = S.bufs("e", 4)
            l_sb = [sb2("a_l%d" % i, [128, 512], BF16) for i in range(4)]; l_b = S.bufs("l", 4)
            x_sb = [sb2("a_x%d" % i, [128, 512], BF16) for i in range(4)]; x_b = S.bufs("x", 4)
            a_sb = [sb2("a_a%d" % i, [128, 512], BF16) for i in range(4)]; a_b = S.bufs("a", 4)
            negtri, negcmp, masklt = C["negtri"], C["negcmp"], C["masklt"]
            cbuf = C["cb"]
            LA = 3
            qz = [sb2("a_qz%d" % i, [128, 2, 512], BF16) for i in range(2)]; qz_b = S.bufs("qz", 2)
            for i in range(2):
                S.op("pool", lambda e: e.memset(qz[i][:, :, :], 0.0), writes=[qz_b[i]])
            pcount = 0
            for qb in range(NQB):
                for hp in range(4):
                    pq = pcount % 2; pcount += 1
                    obase = 4 + 2 * pq
                    S.op("dve", lambda e: e.tensor_copy(out=qz[pq][0:64, 0, :], in_=qT[0:64, hp, qb * 512:(qb + 1) * 512]),
                         reads=qT_b[4 * qb:4 * qb + 4], writes=[qz_b[pq]])
                    S.op("dve", lambda e: e.tensor_copy(out=qz[pq][64:128, 1, :], in_=qT[64:128, hp, qb * 512:(qb + 1) * 512]),
                         reads=qT_b[4 * qb:4 * qb + 4], writes=[qz_b[pq]])
                    items = [(j, hh) for j in range(4 * qb + 3, -1, -1) for hh in range(2)]
                    jfirst = 4 * qb + 3

                    n = len(items)

                    def geo(i):
                        j, hh = items[i]
                        return j, hh, i % 4, max(0, (j - 4 * qb)) * 128

                    def pe_z(i):
                        j, hh, par, c0 = geo(i)
                        zb = i % 2
                        S.op("pe", lambda e: e.matmul(ps[zb][:, c0:512], kT[:, hp, j * 128:(j + 1) * 128],
                                                      qz[pq][:, hh, c0:512], start=True, stop=True),
                             reads=[kT_b[j], qz_b[pq]], writes=[psb[zb]])

                    def pe_tri(i):
                        j, hh, par, c0 = geo(i)
                        S.op("pe", lambda e: e.matmul(ps[2 + hh][:, c0:512], negtri[:, :], l_sb[par][:, c0:512],
                                                      start=(j == jfirst), stop=False, skip_group_check=True),
                             reads=[l_b[par], cbuf], writes=[psb[2 + hh]])

                    def pe_cmp_av(i):
                        j, hh, par, c0 = geo(i)
                        if j > 0:
                            S.op("pe", lambda e: e.matmul(ps[2 + hh][:, c0:512], negcmp[:, :], l_sb[par][:, c0:512],
                                                          start=False, stop=False, skip_group_check=True),
                                 reads=[l_b[par], cbuf], writes=[psb[2 + hh]])
                        S.op("pe", lambda e: e.matmul(ps[obase + hh][:, c0:512], v[:, j, hp * 128:(hp + 1) * 128],
                                                      a_sb[par][:, c0:512], start=(j == jfirst), stop=(j == 0),
                                                      skip_group_check=True),
                             reads=[v_b[j], a_b[par]], writes=[psb[obase + hh]])

                    def act_a(i):
                        j, hh, par, c0 = geo(i)
                        zb = i % 2
                        S.op("act", lambda e: e.activation(out=e_sb[par][:, c0:512], in_=ps[zb][:, c0:512], func=AF.Exp),
                             reads=[psb[zb]], writes=[e_b[par]])
                        if j >= 4 * qb:
                            S.op("dve", lambda e: e.tensor_tensor(out=e_sb[par][:, c0:c0 + 128], in0=e_sb[par][:, c0:c0 + 128],
                                                                  in1=masklt[:, :], op=ALU.mult),
                                 reads=[e_b[par], cbuf], writes=[e_b[par]])
                        S.op("act", lambda e: e.activation(out=l_sb[par][:, c0:512], in_=e_sb[par][:, c0:512], func=AF.Ln,
                                                           bias=1.0),
                             reads=[e_b[par]], writes=[l_b[par]])

                    def act_b(i):
                        j, hh, par, c0 = geo(i)
                        S.op("act", lambda e: e.activation(out=x_sb[par][:, c0:512], in_=ps[2 + hh][:, c0:512], func=AF.Exp),
                             reads=[psb[2 + hh]], writes=[x_b[par]])
                        S.op("dve", lambda e: e.tensor_tensor(out=a_sb[par][:, c0:512], in0=e_sb[par][:, c0:512],
                                                              in1=x_sb[par][:, c0:512], op=ALU.mult),
                             reads=[e_b[par], x_b[par]], writes=[a_b[par]])

                    for i in range(-1, n + 2):
                        if 0 <= i + 1 < n:
                            pe_z(i + 1)
                        if 0 <= i - 1 < n:
                            pe_tri(i - 1)
                        if 0 <= i - 2 < n:
                            pe_cmp_av(i - 2)
                        if 0 <= i < n:
                            act_a(i)
                        if 0 <= i - 1 < n:
                            act_b(i - 1)
                    for hh in range(2):
                        S.op("act", lambda e: e.activation(out=yTb[hh * 64:(hh + 1) * 64, hp, qb * 512:(qb + 1) * 512],
                                                           in_=ps[obase + hh][hh * 64:(hh + 1) * 64, :], func=AF.Copy),
                             reads=[psb[obase + hh]], writes=[yT_b[4 + hp][4 * qb + t] for t in range(4)])
            cnt = 0
            for i in range(NT):
                for dh in range(2):
                    pb = cnt % 4; cnt += 1
                    for kc in range(8):
                        lhs = yTa[:, kc, i * 128:(i + 1) * 128] if kc < 4 else yTb[:, kc - 4, i * 128:(i + 1) * 128]
                        S.op("pe", lambda e: e.matmul(ps[pb][:, :], lhs,
                                                      wout[:, kc, dh * 512:(dh + 1) * 512], start=(kc == 0), stop=(kc == 7)),
                             reads=[yT_b[kc][i], wout_b[dh]], writes=[psb[pb]], signal=(kc == 7))
                    S.op("dve", lambda e: e.tensor_tensor(out=X[:, i, dh * 512:(dh + 1) * 512], in0=ps[pb][:, :],
                                                          in1=X[:, i, dh * 512:(dh + 1) * 512], op=ALU.add),
                         reads=[psb[pb], Xb[i]], writes=[Xb[i]])
            S.fence()


def make_consts(S, nc, es, C):
    ident = es.enter_context(nc.sbuf_tensor("ident", [128, 128], F32))
    identb = es.enter_context(nc.sbuf_tensor("identb", [128, 128], BF16))
    tmp = es.enter_context(nc.sbuf_tensor("ctmp", [128, 128], F32))
    negtri = es.enter_context(nc.sbuf_tensor("negtri", [128, 128], BF16))
    negcmp = es.enter_context(nc.sbuf_tensor("negcmp", [128, 128], BF16))
    masklt = es.enter_context(nc.sbuf_tensor("masklt", [128, 128], BF16))
    maskle = es.enter_context(nc.sbuf_tensor("maskle", [128, 128], BF16))
    onesf = es.enter_context(nc.sbuf_tensor("onesf", [128, 128], F32))
    zerosf = es.enter_context(nc.sbuf_tensor("zerosf", [128, 128], F32))
    cb = S.buf("consts")
    S.op("pool", lambda e: e.memset(onesf[:, :], 1.0), writes=[cb])
    S.op("pool", lambda e: e.memset(zerosf[:, :], 0.0), writes=[cb])
    C.update(onesf=onesf, zerosf=zerosf, psb4a=S.buf("ps4a", persist=True), psb4b=S.buf("ps4b", persist=True))
    C.update(ident=ident, identb=identb, negtri=negtri, negcmp=negcmp, masklt=masklt, maskle=maskle, cb=cb)

    def sel(out, val, pattern, cm, op, base=0):
        S.op("pool", lambda e: e.memset(tmp[:, :], val), reads=[cb], writes=[cb])
        S.op("pool", lambda e: e.affine_select(out=tmp[:, :], in_=tmp[:, :], pattern=pattern, compare_op=op, fill=0.0,
                                               base=base, channel_multiplier=cm), reads=[cb], writes=[cb])
        S.op("pool", lambda e: e.tensor_copy(out=out[:, :], in_=tmp[:, :]), reads=[cb], writes=[cb])
    sel(ident, 1.0, [[-1, 128]], 1, ALU.is_equal)
    sel(identb, 1.0, [[-1, 128]], 1, ALU.is_equal)
    sel(negtri, -1.0, [[-1, 128]], 1, ALU.is_ge)
    sel(negcmp, -1.0, [[1, 128]], -1, ALU.is_gt)
    sel(masklt, 1.0, [[1, 128]], -1, ALU.is_gt)
    sel(maskle, 1.0, [[1, 128]], -1, ALU.is_ge)


def odd_mixer(S, nc, C, X, Xb, NT, P, gs_dram):
    ps, psb = C["ps"], C["psb"]
    ident, identb, onesf, zerosf, maskle, cbuf = C["ident"], C["identb"], C["onesf"], C["zerosf"], C["maskle"], C["cb"]
    T = NT * 128
    NQB = T // 512
    NP = 4 * NT
    KS = 128 ** -0.5
    with ExitStack() as es:
        def sb(name, shape, dt):
            return es.enter_context(nc.sbuf_tensor(uname(name), shape, dt))
        hT = sb("o_hT", [128, 8, T], BF16); hT_b = S.bufs("hT", NT)
        wout = sb("o_wout", [128, 8, 1024], BF16); wout_b = S.bufs("wout", 2)
        woutv = P["w_out"].rearrange("(kc p) d -> p kc d", p=128)
        winv = P["w_in"].rearrange("(kc p) f -> p kc f", p=128)
        ongb = sb("o_ong", [128, 1024], F32); ong_b = S.buf("ong")
        cw = sb("o_cw", [128, 8, 4], F32); cw_b = S.buf("cw")
        cbias = sb("o_cb", [128, 8], F32); cbias_b = S.buf("cbias")
        TS = sb("o_TS", [128, 4 * NP], F32); TS_b = S.buf("TS")
        DEC = sb("o_DEC", [128, NP], F32); DEC_b = S.buf("DEC")
        wg = sb("o_wg", [128, 8, 8], BF16); wg_b = S.buf("wg")
        S.dma("pool", wg[:, :, :], winv[:, :, 3072:3080], writes=[wg_b])
        for dh in range(2):
            S.dma("pool", wout[:, :, dh * 512:(dh + 1) * 512], woutv[:, :, dh * 512:(dh + 1) * 512], writes=[wout_b[dh]])
        S.dma("sp", ongb[:, :], P["on_g"].partition_broadcast(128), writes=[ong_b])
        for k_ in range(4):
            S.dma("sp", cw[:, :, k_], P["conv_w"][k_].rearrange("(i p) -> p i", p=128), writes=[cw_b], allow_slow_non_contiguous=True)
        S.dma("sp", cbias[:, :], P["conv_b"].rearrange("(i p) -> p i", p=128), writes=[cbias_b], allow_slow_non_contiguous=True)
        with ExitStack() as es1:
            def sb1(name, shape, dt):
                return es1.enter_context(nc.sbuf_tensor(uname(name), shape, dt))
            gbc = sb1("o_gbc", [128, 1024], F32); gbc_b = S.buf("gbc")
            S.dma("sp", gbc[:, :], P["g"].partition_broadcast(128), writes=[gbc_b])
            junk = sb1("o_junk", [128, 1024], BF16); junk_b = S.buf("junk")
            xs = [sb1("o_xs%d" % i, [128, 1024], BF16) for i in range(2)]; xs_b = S.bufs("xs", 2)
            st = [sb1("o_st%d" % i, [128, 4], F32) for i in range(2)]; st_b = S.bufs("st", 2)
            tmpn = (junk, junk_b, xs, xs_b, st, st_b)
            for i in range(NT):
                norm_transpose(S, C, X, Xb, i, gbc, gbc_b, tmpn, i % 2, i % 2, hT[:, :, i * 128:(i + 1) * 128], hT_b[i])
            gsb = sb1("o_gsb", [8, T], F32); gsb_b = S.buf("gsb")
            for blk in range(NQB):
                pb = 2 + blk % 2
                for kc in range(8):
                    S.op("pe", lambda e: e.matmul(ps[pb][0:8, :], wg[:, kc, :], hT[:, kc, blk * 512:(blk + 1) * 512],
                                                  start=(kc == 0), stop=(kc == 7)),
                         reads=[wg_b] + hT_b[blk * 4:(blk + 1) * 4], writes=[psb[pb]], signal=(kc == 7))
                S.op("act", lambda e: e.activation(out=gsb[:, blk * 512:(blk + 1) * 512], in_=ps[pb][0:8, :], func=AF.Copy),
                     reads=[psb[pb]], writes=[gsb_b])
            gsd_b = S.buf("gsd")
            S.dma("sp", gs_dram[:, :], gsb[:, :], reads=[gsb_b], writes=[gsd_b])
            G = {}
            for nm in ("I", "F", "e1", "sp", "bneg", "a", "cm", "Mx", "rowfac", "inter", "wkx", "en"):
                G[nm] = sb1("o_G" + nm, [NP, 128], F32)
            Gb = S.buf("G")
            S.dma("sp", G["I"][:, :], gs_dram[0:4, :].rearrange("h (c t) -> (h c) t", t=128), reads=[gsd_b], writes=[Gb])
            Fb = S.buf("GF")
            S.dma("sp", G["F"][:, :], gs_dram[4:8, :].rearrange("h (c t) -> (h c) t", t=128), reads=[gsd_b], writes=[Fb])
            bias = sb1("o_bias", [NP, 4], F32); bias_b = [S.buf("bias%d" % i) for i in range(8)]
            for h in range(4):
                S.dma("sp", bias[h * NT:(h + 1) * NT, 0:1], P["i_b"][h:h + 1].partition_broadcast(NT), writes=[bias_b[h]])
                S.dma("sp", bias[h * NT:(h + 1) * NT, 1:2], P["f_b"][h:h + 1].partition_broadcast(NT), writes=[bias_b[4 + h]])
            Kc = sb1("o_K", [NP, 4], F32)
            R = sb1("o_R", [1, 8 * NP], F32)
            allg = [Gb, Fb] + bias_b
            def gop(eng, fn):
                S.op(eng, fn, reads=allg + [cbuf], writes=[Gb])
            gop("dve", lambda e: e.tensor_scalar(out=bias[:, 2:3], in0=bias[:, 1:2], scalar1=-1.0, scalar2=None, op0=ALU.mult))
            gop("act", lambda e: e.activation(out=G["e1"][:, :], in_=G["F"][:, :], func=AF.Exp, bias=bias[:, 2:3], scale=-1.0))
            gop("act", lambda e: e.activation(out=G["sp"][:, :], in_=G["e1"][:, :], func=AF.Ln, bias=1.0))
            gop("dve", lambda e: e.tensor_tensor_scan(out=G["bneg"][:, :], data0=onesf[0:NP, :], data1=G["sp"][:, :],
                                                      initial=0.0, op0=ALU.mult, op1=ALU.add))
            gop("dve", lambda e: e.scalar_tensor_tensor(out=G["a"][:, :], in0=G["I"][:, :], scalar=bias[:, 0:1],
                                                        in1=G["bneg"][:, :], op0=ALU.add, op1=ALU.add))
            gop("dve", lambda e: e.tensor_tensor_scan(out=G["cm"][:, :], data0=zerosf[0:NP, :], data1=G["a"][:, :],
                                                      initial=-1e30, op0=ALU.add, op1=ALU.max))
            pr = 4
            S.op("pe", lambda e: e.transpose(out=ps[pr][0:1, 0:NP], in_=G["bneg"][:, 127:128], identity=ident[0:NP, 0:NP]),
                 reads=[Gb, cbuf], writes=[psb[pr]], signal=False)
            S.op("pe", lambda e: e.transpose(out=ps[pr][0:1, NP:2 * NP], in_=G["cm"][:, 127:128], identity=ident[0:NP, 0:NP]),
                 reads=[Gb, cbuf], writes=[psb[pr]])
            def rop(eng, fn, extra=()):
                S.op(eng, fn, reads=[Gb, cbuf] + list(extra), writes=[Gb])
            rop("act", lambda e: e.activation(out=R[:, 0:2 * NP], in_=ps[pr][0:1, 0:2 * NP], func=AF.Copy), [psb[pr]])
            rop("dve", lambda e: e.tensor_scalar(out=R[:, 2 * NP:3 * NP], in0=R[:, 0:NP], scalar1=-1.0, scalar2=None, op0=ALU.mult))
            for h in range(4):
                rop("dve", lambda e: e.tensor_tensor_scan(out=R[:, 3 * NP + h * NT:3 * NP + (h + 1) * NT],
                                                          data0=R[:, NP + h * NT:NP + (h + 1) * NT],
                                                          data1=R[:, 2 * NP + h * NT:2 * NP + (h + 1) * NT],
                                                          initial=0.0, op0=ALU.max, op1=ALU.add))
            rop("dve", lambda e: e.memset(R[:, 4 * NP:5 * NP], 0.0))
            rop("dve", lambda e: e.tensor_copy(out=R[:, 4 * NP:5 * NP].rearrange("o (h c) -> o h c", h=4)[:, :, 1:NT],
                                               in_=R[:, 3 * NP:4 * NP].rearrange("o (h c) -> o h c", h=4)[:, :, 0:NT - 1]))
            rop("dve", lambda e: e.tensor_tensor(out=R[:, 5 * NP:6 * NP], in0=R[:, 4 * NP:5 * NP], in1=R[:, NP:2 * NP], op=ALU.max))
            rop("dve", lambda e: e.tensor_tensor(out=R[:, 6 * NP:7 * NP], in0=R[:, 4 * NP:5 * NP], in1=R[:, 5 * NP:6 * NP],
                                                 op=ALU.subtract))
            rop("act", lambda e: e.activation(out=R[:, 7 * NP:8 * NP], in_=R[:, 6 * NP:7 * NP], func=AF.Exp))
            pk = 5
            S.op("pe", lambda e: e.matmul(ps[pk][0:NP, 0:1], R[0:1, 4 * NP:5 * NP], onesf[0:1, 0:1], start=True, stop=True),
                 reads=[Gb, cbuf], writes=[psb[pk]], signal=False)
            S.op("pe", lambda e: e.matmul(ps[pk][0:NP, 1:2], R[0:1, 5 * NP:6 * NP], onesf[0:1, 0:1], start=True, stop=True),
                 reads=[Gb, cbuf], writes=[psb[pk]])
            rop("act", lambda e: e.activation(out=Kc[:, 0:2], in_=ps[pk][0:NP, 0:2], func=AF.Copy), [psb[pk]])
            rop("dve", lambda e: e.tensor_scalar(out=Kc[:, 2:3], in0=Kc[:, 1:2], scalar1=-1.0, scalar2=None, op0=ALU.mult))
            rop("dve", lambda e: e.tensor_scalar(out=G["Mx"][:, :], in0=G["cm"][:, :], scalar1=Kc[:, 0:1], scalar2=None, op0=ALU.max))
            rop("act", lambda e: e.activation(out=G["rowfac"][:, :], in_=G["Mx"][:, :], func=AF.Exp, bias=Kc[:, 1:2], scale=-1.0))
            rop("act", lambda e: e.activation(out=G["inter"][:, :], in_=G["Mx"][:, :], func=AF.Exp, bias=Kc[:, 0:1], scale=-1.0))
            rop("act", lambda e: e.activation(out=G["wkx"][:, :], in_=G["a"][:, :], func=AF.Exp, bias=Kc[:, 2:3], scale=1.0))
            rop("dve", lambda e: e.tensor_tensor(out=G["e1"][:, :], in0=G["bneg"][:, :], in1=G["Mx"][:, :], op=ALU.subtract))
            rop("act", lambda e: e.activation(out=G["en"][:, :], in_=G["e1"][:, :], func=AF.Exp))
            pt = 6
            for qi, nm in enumerate(("rowfac", "inter", "wkx", "en")):
                S.op("pe", lambda e: e.transpose(out=ps[pt][:, qi * NP:(qi + 1) * NP], in_=G[nm][:, :], identity=ident[0:NP, 0:NP]),
                     reads=[Gb, cbuf], writes=[psb[pt]], signal=(qi == 3))
            S.op("act", lambda e: e.activation(out=TS[:, :], in_=ps[pt][:, 0:4 * NP], func=AF.Copy), reads=[psb[pt]], writes=[TS_b])
            pd = 7
            S.op("pe", lambda e: e.matmul(ps[pd][:, 0:NP], onesf[0:1, :], R[0:1, 7 * NP:8 * NP], start=True, stop=True),
                 reads=[Gb, cbuf], writes=[psb[pd]])
            S.op("act", lambda e: e.activation(out=DEC[:, :], in_=ps[pd][:, 0:NP], func=AF.Copy), reads=[psb[pd]], writes=[DEC_b])
            S.fence()
        whd = sb("o_whd", [128, 8, 768], BF16); whd_b = S.bufs("whd", 4)
        pre = sb("o_pre", [128, 2, 3 + T], F32); pre_b = S.bufs("pre", 2)
        acc = sb("o_acc", [128, T], F32); acc_b = S.buf("acc")
        qkT = sb("o_qkT", [128, 2, T], BF16); qkT_b = S.bufs("qkT", 2)
        vone = sb("o_vone", [128, NT, 257], BF16); vone_b = S.bufs("vone", NT)
        sog = sb("o_sog", [128, NT, 256], BF16); sog_b = S.bufs("sog", NT)
        C32 = sb("o_C32", [128, 257], F32); C32_b = S.buf("C32")
        Cx = sb("o_Cx", [128, 257], BF16); Cx_b = S.buf("Cx")
        vext = [sb("o_vext%d" % i, [128, 257], BF16) for i in range(2)]; vext_b = S.bufs("vext", 2)
        sm = [sb("o_sm%d" % i, [128, 128], BF16) for i in range(2)]; sm_b = S.bufs("sm", 2)
        ktok = [sb("o_ktok%d" % i, [128, 128], BF16) for i in range(2)]; ktok_b = S.bufs("ktok", 2)
        o1 = [sb("o_o1%d" % i, [128, 257], F32) for i in range(2)]; o1_b = S.bufs("o1", 2)
        o2 = [sb("o_o2%d" % i, [128, 257], F32) for i in range(2)]; o2_b = S.bufs("o2", 2)
        sc = [sb("o_sc%d" % i, [128, 8], F32) for i in range(2)]; sc_b = S.bufs("sc", 2)
        jk = sb("o_jk", [128, 256], BF16); jk_b = S.buf("jk")
        yf = [sb("o_yf%d" % i, [128, 256], F32) for i in range(2)]; yf_b = S.bufs("yf", 2)
        yb = [sb("o_yb%d" % i, [128, 256], BF16) for i in range(2)]; yb_b = S.bufs("yb", 2)
        yTh = [sb("o_yTh%d" % i, [128, 2, 128], BF16) for i in range(2)]; yTh_b = S.bufs("yTh", 2)
        S.op("dve", lambda e: e.memset(pre[:, :, 0:3], 0.0), writes=pre_b)
        S.op("pool", lambda e: e.memset(vone[:, :, 256:257], 1.0), writes=vone_b)
        for h in range(4):
            srcs = ((h * 128, 128), (512 + h * 128, 128), (1024 + h * 256, 256), (2048 + h * 256, 256))
            off = 0
            for si, (c0, n) in enumerate(srcs):
                S.dma("pool", whd[:, :, off:off + n], winv[:, :, c0:c0 + n], writes=[whd_b[si]])
                off += n
            cnt = 0
            for w in range(2):
                for blk in range(NQB):
                    pb = cnt % 2; cnt += 1
                    for kc in range(8):
                        S.op("pe", lambda e: e.matmul(ps[pb][:, :], whd[:, kc, w * 128:(w + 1) * 128],
                                                      hT[:, kc, blk * 512:(blk + 1) * 512], start=(kc == 0), stop=(kc == 7)),
                             reads=[whd_b[w]] + hT_b[blk * 4:(blk + 1) * 4], writes=[psb[pb]], signal=(kc == 7))
                    S.op("act", lambda e: e.activation(out=pre[:, w, 3 + blk * 512:3 + (blk + 1) * 512], in_=ps[pb][:, :],
                                                       func=AF.Copy), reads=[psb[pb]], writes=[pre_b[w]])
                idx = w * 4 + h
                S.op("dve", lambda e: e.tensor_scalar(out=acc[:, :], in0=pre[:, w, 0:T], scalar1=cw[:, idx, 0:1], scalar2=None,
                                                      op0=ALU.mult), reads=[pre_b[w], cw_b], writes=[acc_b])
                for k in range(1, 4):
                    S.op("dve", lambda e: e.scalar_tensor_tensor(out=acc[:, :], in0=pre[:, w, k:k + T], scalar=cw[:, idx, k:k + 1],
                                                                 in1=acc[:, :], op0=ALU.mult, op1=ALU.add),
                         reads=[pre_b[w], cw_b, acc_b], writes=[acc_b])
                if w == 0:
                    S.op("act", lambda e: e.activation(out=qkT[:, 0, :], in_=acc[:, :], func=AF.Silu, bias=cbias[:, idx:idx + 1]),
                         reads=[acc_b, cbias_b], writes=[qkT_b[0]])
                else:
                    S.op("act", lambda e: e.activation(out=acc[:, :], in_=acc[:, :], func=AF.Silu, bias=cbias[:, idx:idx + 1]),
                         reads=[acc_b, cbias_b], writes=[acc_b])
                    S.op("dve", lambda e: e.tensor_scalar(out=qkT[:, 1, :], in0=acc[:, :], scalar1=KS, scalar2=None, op0=ALU.mult),
                         reads=[acc_b], writes=[qkT_b[1]])
            for i in range(NT):
                pb = 2 + i % 2
                for kc in range(8):
                    S.op("pe", lambda e: e.matmul(ps[pb][:, :], hT[:, kc, i * 128:(i + 1) * 128], whd[:, kc, 256:768],
                                                  start=(kc == 0), stop=(kc == 7)),
                         reads=[hT_b[i], whd_b[2], whd_b[3]], writes=[psb[pb]], signal=(kc == 7))
                S.op("act", lambda e: e.activation(out=vone[:, i, 0:256], in_=ps[pb][:, 0:256], func=AF.Copy),
                     reads=[psb[pb]], writes=[vone_b[i]])
                S.op("act", lambda e: e.activation(out=sog[:, i, :], in_=ps[pb][:, 256:512], func=AF.Sigmoid),
                     reads=[psb[pb]], writes=[sog_b[i]])
            S.op("dve", lambda e: e.memset(C32[:, :], 0.0), writes=[C32_b])
            S.op("pool", lambda e: e.memset(Cx[:, :], 0.0), writes=[Cx_b])
            psb4a, psb4b = C["psb4a"], C["psb4b"]

            def stageF(c, S=S):
                p = h * NT + c
                k2 = c % 2
                cs = slice(c * 128, (c + 1) * 128)
                S.op("dve", lambda e: e.tensor_scalar(out=vext[k2][:, :], in0=vone[:, c, :], scalar1=TS[:, 2 * NP + p:2 * NP + p + 1],
                                                      scalar2=None, op0=ALU.mult), reads=[vone_b[c], TS_b], writes=[vext_b[k2]])
                S.op("pe", lambda e: e.matmul(ps[4][:, 0:128], qkT[:, 1, cs], qkT[:, 0, cs], start=True, stop=True),
                     reads=[qkT_b[0], qkT_b[1]], writes=[psb4a])
                S.op("dve", lambda e: e.tensor_tensor(out=sm[k2][:, :], in0=ps[4][:, 0:128], in1=maskle[:, :], op=ALU.mult),
                     reads=[psb4a, cbuf], writes=[sm_b[k2]])
                S.op("pe", lambda e: e.matmul(ps[5][:, 0:257], sm[k2][:, :], vext[k2][:, :], start=True, stop=True),
                     reads=[sm_b[k2], vext_b[k2]], writes=[psb[5]])
                S.op("act", lambda e: e.activation(out=o1[k2][:, :], in_=ps[5][:, 0:257], func=AF.Identity,
                                                   scale=TS[:, p:p + 1]), reads=[psb[5], TS_b], writes=[o1_b[k2]])
                pTk = ps[7][:, :].bitcast(BF16)
                S.op("pe", lambda e: e.transpose(out=pTk[:, k2 * 128:(k2 + 1) * 128], in_=qkT[:, 1, cs], identity=identb[:, :]),
                     reads=[qkT_b[1], cbuf], writes=[psb[7]])
                S.op("act", lambda e: e.activation(out=ktok[k2][:, :], in_=pTk[:, k2 * 128:(k2 + 1) * 128], func=AF.Copy),
                     reads=[psb[7]], writes=[ktok_b[k2]])
                S.op("pe", lambda e: e.matmul(ps[2 + k2][:, 0:257], ktok[k2][:, :], vext[k2][:, :], start=True, stop=True),
                     reads=[ktok_b[k2], vext_b[k2]], writes=[psb[2 + k2]])

            def stageG(c, S=S):
                p = h * NT + c
                k2 = c % 2
                cs = slice(c * 128, (c + 1) * 128)
                S.op("pe", lambda e: e.matmul(ps[6][:, 0:257], qkT[:, 0, cs], Cx[:, :], start=True, stop=True),
                     reads=[qkT_b[0], Cx_b], writes=[psb[6]])
                S.op("dve", lambda e: e.scalar_tensor_tensor(out=o2[k2][:, :], in0=ps[6][:, 0:257], scalar=TS[:, NP + p:NP + p + 1],
                                                             in1=o1[k2][:, :], op0=ALU.mult, op1=ALU.add),
                     reads=[psb[6], TS_b, o1_b[k2]], writes=[o2_b[k2]])
                S.op("dve", lambda e: e.scalar_tensor_tensor(out=C32[:, :], in0=C32[:, :], scalar=DEC[:, p:p + 1],
                                                             in1=ps[2 + k2][:, 0:257], op0=ALU.mult, op1=ALU.add),
                     reads=[C32_b, DEC_b, psb[2 + k2]], writes=[C32_b])
                S.op("act", lambda e: e.activation(out=Cx[:, :], in_=C32[:, :], func=AF.Copy), reads=[C32_b], writes=[Cx_b])

            def stageH(c, S=S):
                p = h * NT + c
                k2 = c % 2
                s_ = sc[k2]; s_b = sc_b[k2]
                S.op("dve", lambda e: e.tensor_scalar(out=s_[:, 7:8], in0=o2[k2][:, 256:257], scalar1=-1.0, scalar2=None,
                                                      op0=ALU.mult), reads=[o2_b[k2]], writes=[s_b])
                S.op("dve", lambda e: e.tensor_tensor(out=s_[:, 0:1], in0=o2[k2][:, 256:257], in1=s_[:, 7:8], op=ALU.max),
                     reads=[o2_b[k2], s_b], writes=[s_b])
                S.op("dve", lambda e: e.tensor_tensor(out=s_[:, 0:1], in0=s_[:, 0:1], in1=TS[:, 3 * NP + p:3 * NP + p + 1],
                                                      op=ALU.max), reads=[s_b, TS_b], writes=[s_b])
                S.op("dve", lambda e: e.reciprocal(out=s_[:, 1:2], in_=s_[:, 0:1]), reads=[s_b], writes=[s_b])
                S.op("act", lambda e: e.activation(out=jk[:, :], in_=o2[k2][:, 0:256], func=AF.Square, scale=s_[:, 1:2],
                                                   accum_out=s_[:, 2:3]), reads=[o2_b[k2], s_b], writes=[jk_b, s_b])
                S.op("dve", lambda e: e.tensor_scalar(out=s_[:, 3:4], in0=s_[:, 2:3], scalar1=1.0 / 256, scalar2=EPS,
                                                      op0=ALU.mult, op1=ALU.add), reads=[s_b], writes=[s_b])
                S.op("act", lambda e: e.sqrt(out=s_[:, 4:5], in_=s_[:, 3:4]), reads=[s_b], writes=[s_b])
                S.op("dve", lambda e: e.reciprocal(out=s_[:, 5:6], in_=s_[:, 4:5]), reads=[s_b], writes=[s_b])
                S.op("dve", lambda e: e.tensor_tensor(out=s_[:, 6:7], in0=s_[:, 5:6], in1=s_[:, 1:2], op=ALU.mult),
                     reads=[s_b], writes=[s_b])
                S.op("dve", lambda e: e.scalar_tensor_tensor(out=yf[k2][:, :], in0=o2[k2][:, 0:256], scalar=s_[:, 6:7],
                                                             in1=ongb[:, h * 256:(h + 1) * 256], op0=ALU.mult, op1=ALU.mult),
                     reads=[o2_b[k2], s_b, ong_b], writes=[yf_b[k2]])
                S.op("pool", lambda e: e.tensor_tensor(out=yb[k2][:, :], in0=yf[k2][:, :], in1=sog[:, c, :], op=ALU.mult),
                     reads=[yf_b[k2], sog_b[c]], writes=[yb_b[k2]])
                pTy = ps[4][:, :].bitcast(BF16)
                for kk in range(2):
                    S.op("pe", lambda e, kk=kk: e.transpose(out=pTy[:, 512 + kk * 128:512 + (kk + 1) * 128],
                                                     in_=yb[k2][:, kk * 128:(kk + 1) * 128], identity=identb[:, :]),
                         reads=[yb_b[k2], cbuf], writes=[psb4b], signal=(kk == 1))
                S.op("act", lambda e: e.activation(out=yTh[k2][:, :, :], in_=pTy[:, 512:768].rearrange("p (k t) -> p k t", k=2),
                                                   func=AF.Copy), reads=[psb4b], writes=[yTh_b[k2]])
                for dh in range(2):
                    pb = dh
                    for kk in range(2):
                        S.op("pe", lambda e, kk=kk, dh=dh, pb=pb: e.matmul(ps[pb][:, :], yTh[k2][:, kk, :],
                                                                           wout[:, 2 * h + kk, dh * 512:(dh + 1) * 512],
                                                                           start=(kk == 0), stop=(kk == 1)),
                             reads=[yTh_b[k2], wout_b[dh]], writes=[psb[pb]], signal=(kk == 1))
                    S.op("dve", lambda e, dh=dh, pb=pb: e.tensor_tensor(out=X[:, c, dh * 512:(dh + 1) * 512], in0=ps[pb][:, :],
                                                                        in1=X[:, c, dh * 512:(dh + 1) * 512], op=ALU.add),
                         reads=[psb[pb], Xb[c]], writes=[Xb[c]])

            stageF(0)
            for c in range(NT):
                rf, rg, rh = Recorder(), Recorder(), Recorder()
                if c + 1 < NT:
                    stageF(c + 1, rf)
                stageG(c, rg)
                if c >= 1:
                    stageH(c - 1, rh)
                interleave(S, [rg, rf, rh])
            stageH(NT - 1)
        S.fence()


def mlp_layer(S, nc, C, X, Xb, NT, g_row, w1, w2):
    ps, psb = C["ps"], C["psb"]
    identb = C["identb"]
    TBT = min(8, NT)
    TB = TBT * 128
    NH = TB // 512
    NB = NT // TBT
    with ExitStack() as es:
        def sb(name, shape, dt):
            return es.enter_context(nc.sbuf_tensor(uname(name), shape, dt))
        gbc = sb("m_gbc", [128, 1024], F32); gbc_b = S.buf("gbc")
        nhb = min(2, NB)
        hT = [sb("m_hT%d" % i, [128, 8, TB], BF16) for i in range(nhb)]
        hT_b = [S.bufs("hT%d_" % i, TBT) for i in range(nhb)]
        h1T = sb("m_h1T", [128, 32, TB], BF16); h1T_b = [[S.buf("h1T") for _ in range(NH)] for _ in range(32)]
        junk = sb("m_junk", [128, 1024], BF16); junk_b = S.buf("junk")
        xs = [sb("m_xs%d" % i, [128, 1024], BF16) for i in range(2)]; xs_b = S.bufs("xs", 2)
        st = [sb("m_st%d" % i, [128, 4], F32) for i in range(2)]; st_b = S.bufs("st", 2)
        rl = [sb("m_rl%d" % i, [128, 512], BF16) for i in range(2)]; rl_b = S.bufs("rl", 2)
        w1t = [sb("m_w1_%d" % i, [128, 8, 512], BF16) for i in range(2)]; w1_b = S.bufs("w1t", 2)
        w2t = [sb("m_w2_%d" % i, [128, 4, 512], BF16) for i in range(2)]; w2_b = S.bufs("w2t", 2)
        NW2 = 2

        S.dma("sp", gbc[:, :], g_row.partition_broadcast(128), writes=[gbc_b])
        w1v = w1.rearrange("(kc p) f -> p kc f", p=128)
        w2v = w2.rearrange("(g fi p) d -> g p fi d", p=128, fi=4)
        w1cnt = [0]
        w2cnt = [0]

        def load_w1(blk):
            k = w1cnt[0] % 2; w1cnt[0] += 1
            S.dma("pool", w1t[k][:, :, :], w1v[:, :, blk * 512:(blk + 1) * 512], writes=[w1_b[k]])

        def load_w2(idx):
            k = w2cnt[0] % NW2; w2cnt[0] += 1
            dh, g = divmod(idx, 8)
            S.dma("pool", w2t[k][:, :, :], w2v[g, :, :, dh * 512:(dh + 1) * 512], writes=[w2_b[k]])

        nstate = [0]

        def norm_tile(b, j):
            ti = b * TBT + j
            k = nstate[0] % 2; nstate[0] += 1
            hb = b % nhb
            S.op("act", lambda e: e.activation(out=junk[:, :], in_=X[:, ti, :], func=AF.Square, accum_out=st[k][:, 0:1]),
                 reads=[Xb[ti]], writes=[junk_b, st_b[k]])
            S.op("dve", lambda e: e.tensor_scalar(out=st[k][:, 1:2], in0=st[k][:, 0:1], scalar1=1.0 / 1024, scalar2=EPS,
                                                  op0=ALU.mult, op1=ALU.add), reads=[st_b[k]], writes=[st_b[k]])
            S.op("act", lambda e: e.sqrt(out=st[k][:, 2:3], in_=st[k][:, 1:2]), reads=[st_b[k]], writes=[st_b[k]])
            S.op("dve", lambda e: e.reciprocal(out=st[k][:, 3:4], in_=st[k][:, 2:3]), reads=[st_b[k]], writes=[st_b[k]])
            S.op("dve", lambda e: e.scalar_tensor_tensor(out=xs[k][:, :], in0=X[:, ti, :], scalar=st[k][:, 3:4],
                                                         in1=gbc[:, :], op0=ALU.mult, op1=ALU.mult),
                 reads=[Xb[ti], st_b[k], gbc_b], writes=[xs_b[k]])
            pb = k
            pT = ps[pb][:, :].bitcast(BF16)
            for c in range(8):
                S.op("pe", lambda e: e.transpose(out=pT[:, c * 128:(c + 1) * 128], in_=xs[k][:, c * 128:(c + 1) * 128],
                                                 identity=identb[:, :]),
                     reads=[xs_b[k], C["cb"]], writes=[psb[pb]], signal=(c == 7))
            S.op("act", lambda e: e.activation(out=hT[hb][:, :, j * 128:(j + 1) * 128],
                                               in_=pT.rearrange("p (c t) -> p c t", c=8), func=AF.Copy),
                 reads=[psb[pb]], writes=[hT_b[hb][j]])

        load_w1(0); load_w1(1)
        for j in range(TBT):
            norm_tile(0, j)
        cnt = 0
        for b in range(NB):
            hb = b % nhb
            H = hT[hb]; Hb = hT_b[hb]
            for blk in range(8):
                k = (b * 8 + blk) % 2
                wt = w1t[k]; wb = w1_b[k]
                for fi in range(4):
                    fc = blk * 4 + fi
                    pbase = 2 + NH * (cnt % 2); cnt += 1
                    for kc in range(8):
                        for th in range(NH):
                            S.op("pe", lambda e: e.matmul(ps[pbase + th][:, :], wt[:, kc, fi * 128:(fi + 1) * 128],
                                                          H[:, kc, th * 512:(th + 1) * 512], start=(kc == 0), stop=(kc == 7)),
                                 reads=[wb] + Hb[th * 4:(th + 1) * 4], writes=[psb[pbase + th]], signal=(kc == 7))
                    for th in range(NH):
                        pb = pbase + th
                        kr = th
                        S.op("act", lambda e: e.activation(out=rl[kr][:, :], in_=ps[pb][:, :], func=AF.Relu),
                             reads=[psb[pb]], writes=[rl_b[kr]])
                        S.op("dve", lambda e: e.tensor_tensor(out=h1T[:, fc, th * 512:(th + 1) * 512], in0=rl[kr][:, :],
                                                              in1=rl[kr][:, :], op=ALU.mult),
                             reads=[rl_b[kr]], writes=[h1T_b[fc][th]])
                if blk + 2 < 8:
                    load_w1(blk + 2)
                elif blk == 6:
                    load_w2(0)
                else:
                    load_w2(1)
                if b + 1 < NB and blk < TBT:
                    norm_tile(b + 1, blk)
            for dh in range(2):
                for g in range(8):
                    idx = dh * 8 + g
                    k = idx % NW2
                    wt = w2t[k]; wb = w2_b[k]
                    for fi in range(4):
                        fc = g * 4 + fi
                        for j in range(TBT):
                            th = j // 4
                            S.op("pe", lambda e: e.matmul(ps[j][:, :], h1T[:, fc, j * 128:(j + 1) * 128], wt[:, fi, :],
                                                          start=(fc == 0), stop=(fc == 31)),
                                 reads=[wb, h1T_b[fc][th]], writes=[psb[j]], signal=(fc == 31 or fi == 3))
                    if idx + NW2 < 16:
                        load_w2(idx + NW2)
                    elif b + 1 < NB:
                        load_w1(idx + NW2 - 16)
                for j in range(TBT):
                    ti = b * TBT + j
                    S.op("dve", lambda e: e.tensor_tensor(out=X[:, ti, dh * 512:(dh + 1) * 512], in0=ps[j][:, :],
                                                          in1=X[:, ti, dh * 512:(dh + 1) * 512], op=ALU.add),
                         reads=[psb[j], Xb[ti]], writes=[Xb[ti]])
        S.fence()

T_SEQ = 2048
NSEQ_CORE = 2
N_CORES = 8
IN_SHAPES = {
    'mix_norm_g': [4, 1024], 'mlp_norm_g': [4, 1024], 'mlp_w1': [4, 1024, 4096], 'mlp_w2': [4, 4096, 1024],
    'ev_w_in': [2, 1024, 2560], 'ev_w_out': [2, 1024, 1024], 'sg_ln_g': [2, 512], 'sg_ln_b': [2, 512],
    'sg_w': [2, 8, 128, 128], 'sg_b': [2, 8, 128], 'sb_q_norm_g': [2, 64], 'sb_k_norm_g': [2, 64],
    'od_w_in': [2, 1024, 3080], 'od_conv_w': [2, 4, 1024], 'od_conv_b': [2, 1024], 'od_i_b': [2, 4], 'od_f_b': [2, 4],
    'od_out_norm_g': [2, 1024], 'od_w_out': [2, 1024, 1024],
}


def build_program(T=T_SEQ, nseq=NSEQ_CORE, layers=(0, 1, 2, 3)):
    NT = T // 128
    nc = bass.Bass("TRN2", target_bir_lowering=False)
    x = nc.dram_tensor("x", [nseq, T, 1024], F32, kind="ExternalInput").ap()
    W = {k: nc.dram_tensor(k, shp, F32, kind="ExternalInput").ap() for k, shp in IN_SHAPES.items()}
    y = nc.dram_tensor("y", [nseq, T, 1024], F32, kind="ExternalOutput").ap()
    gs = nc.dram_tensor("gs_scratch", [8, T], F32, kind="Internal").ap()
    with ExitStack() as es:
        S = Sched(nc, es)
        C = {}
        C["ps"] = [es.enter_context(nc.psum_tensor("ps%d" % i, [128, 512], F32)) for i in range(8)]
        C["psb"] = S.bufs("ps", 8)
        make_consts(S, nc, es, C)
        X = es.enter_context(nc.sbuf_tensor("X", [128, NT, 1024], F32))
        Xb = S.bufs("X", NT, persist=True)
        for s in range(nseq):
            xv = x[s].rearrange("(n p) d -> p n d", p=128)
            yv = y[s].rearrange("(n p) d -> p n d", p=128)
            for i in range(NT):
                S.dma("sp", X[:, i, :], xv[:, i, :], writes=[Xb[i]])
            for l in layers:
                j = l // 2
                if l % 2 == 0:
                    P = dict(g=W['mix_norm_g'][l], w_in=W['ev_w_in'][j], w_out=W['ev_w_out'][j], ln_g=W['sg_ln_g'][j],
                             ln_b=W['sg_ln_b'][j], sg_w=W['sg_w'][j], sg_b=W['sg_b'][j], qg=W['sb_q_norm_g'][j],
                             kg=W['sb_k_norm_g'][j])
                    even_mixer(S, nc, C, X, Xb, NT, P)
                else:
                    P = dict(g=W['mix_norm_g'][l], w_in=W['od_w_in'][j], w_out=W['od_w_out'][j], conv_w=W['od_conv_w'][j],
                             conv_b=W['od_conv_b'][j], i_b=W['od_i_b'][j], f_b=W['od_f_b'][j], on_g=W['od_out_norm_g'][j])
                    odd_mixer(S, nc, C, X, Xb, NT, P, gs)
                mlp_layer(S, nc, C, X, Xb, NT, W['mlp_norm_g'][l], W['mlp_w1'][l], W['mlp_w2'][l])
            for i in range(NT):
                S.dma("sp", yv[:, i, :], X[:, i, :], reads=[Xb[i]])
        S.finish()
    return nc


def kernel(**inputs):
    x = np.ascontiguousarray(np.asarray(inputs['x'], dtype=np.float32))
    B = x.shape[0]
    per = B // N_CORES
    nc = build_program(T=x.shape[1], nseq=per)
    shared = {k: np.ascontiguousarray(np.asarray(inputs[k], dtype=np.float32)) for k in IN_SHAPES}
    in_maps = []
    for c in range(N_CORES):
        m = dict(shared)
        m['x'] = np.ascontiguousarray(x[c * per:(c + 1) * per])
        in_maps.append(m)
    res = run_bass_kernel_spmd(nc, in_maps, core_ids=list(range(N_CORES)))
    return np.concatenate([np.asarray(r['y']) for r in res.results], axis=0).astype(np.float32)
```

```python
from contextlib import ExitStack
from concourse.bass_utils import run_bass_kernel_spmd
import numpy as np
import concourse.bass as bass
import concourse.mybir as mybir

F32 = mybir.dt.float32
BF16 = mybir.dt.bfloat16
AF = mybir.ActivationFunctionType
ALU = mybir.AluOpType
AX = mybir.AxisListType


_UID = [0]


def uname(name):
    _UID[0] += 1
    return "%s_%d" % (name, _UID[0])


class Buf:
    __slots__ = ("name", "w", "r", "sem", "semval", "persist")

    def __init__(self, name, fence, persist=False):
        self.persist = persist
        self.name = name
        self.w = None
        self.r = list(fence)
        self.sem = None
        self.semval = 0


class EngState:
    def __init__(self, name, obj, sem):
        self.name = name
        self.obj = obj
        self.sem = sem
        self.count = 0
        self.know = {}
        self.last = None


class Sched:
    def __init__(self, nc, stack):
        self.nc = nc
        self.stack = stack
        self.E = {}
        for name, obj in (("pe", nc.tensor), ("act", nc.scalar), ("dve", nc.vector),
                          ("pool", nc.gpsimd), ("sp", nc.sync)):
            sem = stack.enter_context(nc.semaphore("sem_" + name))
            self.E[name] = EngState(name, obj, sem)
        self.fence_recs = []
        self.dma_recs = []
        self.pending_pe = []
        self.sem_pool = {}
        self.phase_bufs = []
        self.nsem = 0
        self.nwaits = 0
        self.nops = 0

    def buf(self, name, persist=False):
        return Buf(name, self.fence_recs, persist)

    def bufs(self, name, n, persist=False):
        return [Buf("%s%d" % (name, i), self.fence_recs, persist) for i in range(n)]

    def _waits(self, eng, reads, writes):
        st = self.E[eng]
        deps = []
        for b in reads:
            if b.w is not None:
                deps.append(b.w)
        for b in writes:
            if b.w is not None:
                deps.append(b.w)
            deps.extend(b.r)
        pesem = self.E["pe"].sem
        seen = set()
        for rec in deps:
            if id(rec) in seen:
                continue
            seen.add(id(rec))
            s, v, clk = rec
            if eng == "pe" and s is pesem:
                continue
            assert v is not None, "dependency on unsignaled PE op"
            if st.know.get(s, 0) >= v:
                continue
            st.obj.wait_ge(s, v)
            self.nwaits += 1
            for ks, kv in clk.items():
                if st.know.get(ks, 0) < kv:
                    st.know[ks] = kv

    def op(self, eng, fn, reads=(), writes=(), signal=True):
        st = self.E[eng]
        self._waits(eng, reads, writes)
        ins = fn(st.obj)
        self.nops += 1
        if signal:
            st.count += 1
            ins.then_inc(st.sem, 1)
            clk = dict(st.know)
            clk[st.sem] = st.count
            rec = [st.sem, st.count, clk]
            if eng == "pe":
                for p in self.pending_pe:
                    p[1] = st.count
                    p[2] = clk
                self.pending_pe = []
            st.last = rec
        else:
            assert eng == "pe"
            rec = [st.sem, None, None]
            self.pending_pe.append(rec)
        for b in reads:
            b.r.append(rec)
        for b in writes:
            b.w = rec
            b.r = []
        return ins

    def dma(self, q, out, in_, reads=(), writes=(), **kw):
        st = self.E[q]
        self._waits(q, reads, writes)
        owner = writes[0] if writes else reads[0]
        if owner.sem is None:
            pool = self.sem_pool.setdefault(q, [])
            if pool and not owner.persist:
                owner.sem, owner.semval = pool.pop()
            else:
                self.nsem += 1
                owner.sem = self.stack.enter_context(self.nc.semaphore("dsem%d" % self.nsem))
            if not owner.persist:
                self.phase_bufs.append((owner, q))
        owner.semval += 16
        ins = st.obj.dma_start(out=out, in_=in_, **kw)
        ins.then_inc(owner.sem, 16)
        clk = dict(st.know)
        clk[owner.sem] = owner.semval
        rec = [owner.sem, owner.semval, clk]
        self.dma_recs.append(rec)
        for b in reads:
            b.r.append(rec)
        for b in writes:
            b.w = rec
            b.r = []
        return ins

    def fence(self):
        sp = self.E["sp"]
        for rec in self.dma_recs:
            s, v, clk = rec
            if sp.know.get(s, 0) >= v:
                continue
            sp.obj.wait_ge(s, v)
            for ks, kv in clk.items():
                if sp.know.get(ks, 0) < kv:
                    sp.know[ks] = kv
        self.dma_recs = []
        for b, bq in self.phase_bufs:
            self.sem_pool.setdefault(bq, []).append((b.sem, b.semval))
            b.sem = None
        self.phase_bufs = []
        ins = sp.obj.nop()
        sp.count += 1
        ins.then_inc(sp.sem, 1)
        clk = dict(sp.know)
        clk[sp.sem] = sp.count
        sp.last = [sp.sem, sp.count, clk]
        assert not self.pending_pe
        recs = []
        for name, st in self.E.items():
            if st.last is not None:
                recs.append(st.last)
        self.fence_recs = recs

    def finish(self):
        self.fence()


class Recorder:
    def __init__(self):
        self.calls = []

    def op(self, eng, fn, reads=(), writes=(), signal=True):
        self.calls.append((eng, fn, list(reads), list(writes), signal))


def interleave(S, recs):
    lists = [r.calls for r in recs if r.calls]
    pos = [0] * len(lists)
    total = sum(len(l) for l in lists)
    done = 0
    while done < total:
        best, bf = None, None
        for t, l in enumerate(lists):
            if pos[t] >= len(l):
                continue
            frac = pos[t] / len(l)
            if bf is None or frac < bf:
                best, bf = t, frac
        l = lists[best]
        while True:
            eng, fn, reads, writes, signal = l[pos[best]]
            S.op(eng, fn, reads, writes, signal)
            pos[best] += 1
            done += 1
            if signal or pos[best] >= len(l):
                break


EPS = 1e-6

def rstd_ops(S, st, st_b, n, inv_n):
    S.op("dve", lambda e: e.tensor_scalar(out=st[:, n:2 * n], in0=st[:, 0:n], scalar1=inv_n, scalar2=EPS,
                                          op0=ALU.mult, op1=ALU.add), reads=[st_b], writes=[st_b])
    S.op("act", lambda e: e.sqrt(out=st[:, 2 * n:3 * n], in_=st[:, n:2 * n]), reads=[st_b], writes=[st_b])
    S.op("dve", lambda e: e.reciprocal(out=st[:, 3 * n:4 * n], in_=st[:, 2 * n:3 * n]), reads=[st_b], writes=[st_b])


def norm_transpose(S, C, X, Xb, ti, gbc, gbc_b, tmp, k, pb, out_ap, out_b):
    ps, psb, identb = C["ps"], C["psb"], C["identb"]
    junk, junk_b, xs, xs_b, st, st_b = tmp
    S.op("act", lambda e: e.activation(out=xs[k][:, :], in_=X[:, ti, :], func=AF.Square, accum_out=st[k][:, 0:1]),
         reads=[Xb[ti]], writes=[xs_b[k], st_b[k]])
    rstd_ops(S, st[k], st_b[k], 1, 1.0 / 1024)
    S.op("dve", lambda e: e.scalar_tensor_tensor(out=xs[k][:, :], in0=X[:, ti, :], scalar=st[k][:, 3:4],
                                                 in1=gbc[:, :], op0=ALU.mult, op1=ALU.mult),
         reads=[Xb[ti], st_b[k], gbc_b], writes=[xs_b[k]])
    pT = ps[pb][:, :].bitcast(BF16)
    for c in range(8):
        S.op("pe", lambda e, c=c: e.transpose(out=pT[:, c * 128:(c + 1) * 128], in_=xs[k][:, c * 128:(c + 1) * 128],
                                              identity=identb[:, :]),
             reads=[xs_b[k], C["cb"]], writes=[psb[pb]], signal=(c == 7))
    S.op("act", lambda e: e.activation(out=out_ap, in_=pT.rearrange("p (c t) -> p c t", c=8), func=AF.Copy),
         reads=[psb[pb]], writes=[out_b])


def even_mixer(S, nc, C, X, Xb, NT, P):
    ps, psb = C["ps"], C["psb"]
    ident, identb = C["ident"], C["identb"]
    T = NT * 128
    NQB = T // 512
    with ExitStack() as es:
        def sb(name, shape, dt):
            return es.enter_context(nc.sbuf_tensor(uname(name), shape, dt))
        yTa = sb("e_yTa", [128, 4, T], BF16); yT_b = [[S.buf("yT") for _ in range(NT)] for _ in range(8)]
        qT = sb("e_qT", [128, 4, T], BF16); qT_b = S.bufs("qT", NT)
        kT = sb("e_kT", [128, 4, T], BF16); kT_b = S.bufs("kT", NT)
        v = sb("e_v", [128, NT, 512], BF16); v_b = S.bufs("v", NT)
        woutv = P["w_out"].rearrange("(kc p) d -> p kc d", p=128)
        with ExitStack() as es1:
            def sb1(name, shape, dt):
                return es1.enter_context(nc.sbuf_tensor(uname(name), shape, dt))
            win = sb1("e_win", [128, 8, 2560], BF16); win_b = S.bufs("win", 5)
            winv = P["w_in"].rearrange("(kc p) f -> p kc f", p=128)
            for cb in range(5):
                S.dma("pool", win[:, :, cb * 512:(cb + 1) * 512], winv[:, :, cb * 512:(cb + 1) * 512], writes=[win_b[cb]])
            gbc = sb1("e_gbc", [128, 1024], F32); gbc_b = S.buf("gbc")
            lng = sb1("e_lng", [128, 512], F32); lng_b = S.buf("lng")
            lnb = sb1("e_lnb", [128, 512], F32); lnb_b = S.buf("lnb")
            qg = sb1("e_qg", [128, 64], F32); qg_b = S.buf("qg")
            kg = sb1("e_kg", [128, 64], F32); kg_b = S.buf("kg")
            sgb = sb1("e_sgb", [128, 8], F32); sgb_b = S.buf("sgb")
            sgw = sb1("e_sgw", [128, 8, 128], BF16); sgw_b = S.buf("sgw")
            wcT = sb1("e_wcT", [128, 8, 128], BF16); wcT_b = S.buf("wcT")
            S.dma("sp", gbc[:, :], P["g"].partition_broadcast(128), writes=[gbc_b])
            S.dma("sp", lng[:, :], P["ln_g"].partition_broadcast(128), writes=[lng_b])
            S.dma("sp", lnb[:, :], P["ln_b"].partition_broadcast(128), writes=[lnb_b])
            S.dma("sp", qg[:, :], P["qg"].partition_broadcast(128), writes=[qg_b])
            S.dma("sp", kg[:, :], P["kg"].partition_broadcast(128), writes=[kg_b])
            S.dma("sp", sgb[:, :], P["sg_b"].rearrange("g t -> t g"), writes=[sgb_b], allow_slow_non_contiguous=True)
            S.dma("pool", sgw[:, :, :], P["sg_w"].rearrange("g t s -> t g s"), writes=[sgw_b])
            S.op("dve", lambda e: e.tensor_scalar(out=qg[:, :], in0=qg[:, :], scalar1=0.125, scalar2=None, op0=ALU.mult),
                 reads=[qg_b], writes=[qg_b])
            S.op("pool", lambda e: e.affine_select(out=sgw[:, :, :], in_=sgw[:, :, :], pattern=[[0, 8], [-1, 128]],
                                                   compare_op=ALU.is_ge, fill=0.0, base=0, channel_multiplier=1),
                 reads=[sgw_b], writes=[sgw_b])
            for half in range(2):
                for gi in range(4):
                    g_ = half * 4 + gi
                    S.op("pe", lambda e: e.transpose(out=ps[half][:, :].bitcast(BF16)[:, gi * 128:(gi + 1) * 128], in_=sgw[:, g_, :],
                                                     identity=identb[:, :]),
                         reads=[sgw_b, C["cb"]], writes=[psb[half]], signal=(gi == 3))
                S.op("act", lambda e: e.activation(out=wcT[:, half * 4:(half + 1) * 4, :],
                                                   in_=ps[half][:, :].bitcast(BF16)[:, 0:512].rearrange("p (g t) -> p g t", g=4), func=AF.Copy),
                     reads=[psb[half]], writes=[wcT_b])
            junk = None; junk_b = None
            xs = [sb1("e_xs%d" % i, [128, 1024], BF16) for i in range(2)]; xs_b = S.bufs("xs", 2)
            st = [sb1("e_st%d" % i, [128, 4], F32) for i in range(2)]; st_b = S.bufs("st", 2)
            tmpn = (junk, junk_b, xs, xs_b, st, st_b)
            hTt = [sb1("e_hTt%d" % i, [128, 8, 128], BF16) for i in range(2)]; hTt_b = S.bufs("hTt", 2)
            u = [sb1("e_u%d" % i, [128, 512], BF16) for i in range(1)]; u_b = S.bufs("u", 1)
            vg = [sb1("e_vg%d" % i, [128, 512], F32) for i in range(1)]; vg_b = S.bufs("vg", 1)
            vgn = [sb1("e_vgn%d" % i, [128, 512], BF16) for i in range(1)]; vgn_b = S.bufs("vgn", 1)
            lst = [sb1("e_lst%d" % i, [128, 8], F32) for i in range(1)]; lst_b = S.bufs("lst", 1)
            ya = [sb1("e_ya%d" % i, [128, 512], F32) for i in range(1)]; ya_b = S.bufs("ya", 1)
            yab = [sb1("e_yab%d" % i, [128, 512], BF16) for i in range(1)]; yab_b = S.bufs("yab", 1)
            sq = [sb1("e_sq%d" % i, [128, 512], F32) for i in range(2)]; sq_b = S.bufs("sq", 2)
            qst = [sb1("e_qst%d" % i, [128, 32], F32) for i in range(2)]; qst_b = S.bufs("qst", 2)
            qn = [sb1("e_qn%d" % i, [128, 512], BF16) for i in range(2)]; qn_b = S.bufs("qn", 2)
            def apart(i, S):
                k = 0
                S.op("act", lambda e: e.activation(out=u[k][:, :], in_=ps[2][:, :], func=AF.Gelu_apprx_tanh),
                     reads=[psb[2]], writes=[u_b[k]])
                S.op("act", lambda e: e.activation(out=vg[k][:, :], in_=ps[3][:, :], func=AF.Gelu_apprx_tanh,
                                                   accum_out=lst[k][:, 0:1]),
                     reads=[psb[3]], writes=[vg_b[k], lst_b[k]])
                S.op("act", lambda e: e.activation(out=vgn[k][:, :], in_=vg[k][:, :], func=AF.Square,
                                                   accum_out=lst[k][:, 1:2]),
                     reads=[vg_b[k], lst_b[k]], writes=[vgn_b[k], lst_b[k]])
                L = lst[k]; Lb = lst_b[k]
                S.op("dve", lambda e: e.tensor_scalar(out=L[:, 2:4], in0=L[:, 0:2], scalar1=1.0 / 512, scalar2=None,
                                                      op0=ALU.mult), reads=[Lb], writes=[Lb])
                S.op("dve", lambda e: e.tensor_tensor(out=L[:, 4:5], in0=L[:, 2:3], in1=L[:, 2:3], op=ALU.mult),
                     reads=[Lb], writes=[Lb])
                S.op("dve", lambda e: e.scalar_tensor_tensor(out=L[:, 5:6], in0=L[:, 3:4], scalar=EPS, in1=L[:, 4:5],
                                                             op0=ALU.add, op1=ALU.subtract), reads=[Lb], writes=[Lb])
                S.op("act", lambda e: e.sqrt(out=L[:, 6:7], in_=L[:, 5:6]), reads=[Lb], writes=[Lb])
                S.op("dve", lambda e: e.reciprocal(out=L[:, 7:8], in_=L[:, 6:7]), reads=[Lb], writes=[Lb])
                S.op("dve", lambda e: e.scalar_tensor_tensor(out=L[:, 4:5], in0=L[:, 2:3], scalar=-1.0, in1=L[:, 7:8],
                                                             op0=ALU.mult, op1=ALU.mult), reads=[Lb], writes=[Lb])
                S.op("act", lambda e: e.activation(out=vg[k][:, :], in_=vg[k][:, :], func=AF.Identity,
                                                   bias=L[:, 4:5], scale=L[:, 7:8]),
                     reads=[vg_b[k], Lb], writes=[vg_b[k]])
                S.op("dve", lambda e: e.tensor_tensor(out=vg[k][:, :], in0=vg[k][:, :], in1=lng[:, :], op=ALU.mult),
                     reads=[vg_b[k], lng_b], writes=[vg_b[k]])
                S.op("dve", lambda e: e.tensor_tensor(out=vgn[k][:, :], in0=vg[k][:, :], in1=lnb[:, :], op=ALU.add),
                     reads=[vg_b[k], lnb_b], writes=[vgn_b[k]])
                for g_ in range(8):
                    S.op("pe", lambda e, g_=g_: e.matmul(ps[7][:, g_ * 64:(g_ + 1) * 64], wcT[:, g_, :],
                                                         vgn[k][:, g_ * 64:(g_ + 1) * 64], start=True, stop=True),
                         reads=[wcT_b, vgn_b[k]], writes=[psb[7]], signal=(g_ == 7))
                S.op("dve", lambda e: e.tensor_tensor(out=ya[k][:, :].rearrange("p (g c) -> p g c", g=8),
                                                      in0=ps[7][:, :].rearrange("p (g c) -> p g c", g=8),
                                                      in1=sgb[:, :].unsqueeze(2).to_broadcast([128, 8, 64]), op=ALU.add),
                     reads=[psb[7], sgb_b], writes=[ya_b[k]])
                S.op("dve", lambda e: e.tensor_tensor(out=yab[k][:, :], in0=ya[k][:, :], in1=u[k][:, :], op=ALU.mult),
                     reads=[ya_b[k], u_b[k]], writes=[yab_b[k]])
                pT = ps[7][:, :].bitcast(BF16)
                for c in range(4):
                    S.op("pe", lambda e, c=c: e.transpose(out=pT[:, c * 128:(c + 1) * 128], in_=yab[k][:, c * 128:(c + 1) * 128],
                                                          identity=identb[:, :]),
                         reads=[yab_b[k]], writes=[psb[7]], signal=(c == 3))
                S.op("act", lambda e: e.activation(out=yTa[:, 0:4, i * 128:(i + 1) * 128],
                                                   in_=pT[:, 0:512].rearrange("p (c t) -> p c t", c=4), func=AF.Copy),
                     reads=[psb[7]], writes=[yT_b[c][i] for c in range(4)])

            def bpart(i, which, S):
                bank, gt, gt_b, dstT, dstT_b = ((4, qg, qg_b, qT, qT_b), (5, kg, kg_b, kT, kT_b))[which]
                kk = which
                Q = qst[kk]; Qb = qst_b[kk]
                S.op("act", lambda e: e.activation(out=sq[kk][:, :], in_=ps[bank][:, :], func=AF.Square),
                     reads=[psb[bank]], writes=[sq_b[kk]])
                S.op("dve", lambda e: e.tensor_reduce(out=Q[:, 0:8], in_=sq[kk][:, :].rearrange("p (h d) -> p h d", h=8),
                                                      axis=AX.X, op=ALU.add), reads=[sq_b[kk]], writes=[Qb])
                rstd_ops(S, Q, Qb, 8, 1.0 / 64)
                S.op("dve", lambda e: e.tensor_tensor(out=sq[kk][:, :].rearrange("p (h d) -> p h d", h=8),
                                                      in0=ps[bank][:, :].rearrange("p (h d) -> p h d", h=8),
                                                      in1=Q[:, 24:32].unsqueeze(2).to_broadcast([128, 8, 64]), op=ALU.mult),
                     reads=[psb[bank], Qb, sq_b[kk]], writes=[sq_b[kk]])
                S.op("dve", lambda e: e.tensor_tensor(out=qn[kk][:, :].rearrange("p (h d) -> p h d", h=8),
                                                      in0=sq[kk][:, :].rearrange("p (h d) -> p h d", h=8),
                                                      in1=gt[:, :].unsqueeze(1).to_broadcast([128, 8, 64]), op=ALU.mult),
                     reads=[sq_b[kk], gt_b], writes=[qn_b[kk]])
                pT2 = ps[bank][:, :].bitcast(BF16)
                for c in range(4):
                    S.op("pe", lambda e, c=c: e.transpose(out=pT2[:, c * 128:(c + 1) * 128], in_=qn[kk][:, c * 128:(c + 1) * 128],
                                                          identity=identb[:, :]),
                         reads=[qn_b[kk]], writes=[psb[bank]], signal=(c == 3))
                S.op("act", lambda e: e.activation(out=dstT[:, :, i * 128:(i + 1) * 128],
                                                   in_=pT2[:, 0:512].rearrange("p (c t) -> p c t", c=4), func=AF.Copy),
                     reads=[psb[bank]], writes=[dstT_b[i]])

            def vpart(i, S):
                S.op("act", lambda e: e.activation(out=v[:, i, :], in_=ps[6][:, :], func=AF.Copy),
                     reads=[psb[6]], writes=[v_b[i]])

            def normpart(i, S):
                kn = i % 2
                norm_transpose(S, C, X, Xb, i, gbc, gbc_b, tmpn, kn, i % 2, hTt[kn][:, :, :], hTt_b[kn])

            normpart(0, S)
            for i in range(NT):
                kn = i % 2
                for cb in range(5):
                    for kc in range(8):
                        S.op("pe", lambda e: e.matmul(ps[2 + cb][:, :], hTt[kn][:, kc, :], win[:, kc, cb * 512:(cb + 1) * 512],
                                                      start=(kc == 0), stop=(kc == 7)),
                             reads=[hTt_b[kn], win_b[cb]], writes=[psb[2 + cb]], signal=(kc == 7))
                recs = [Recorder() for _ in range(5)]
                apart(i, recs[0])
                bpart(i, 0, recs[1])
                bpart(i, 1, recs[2])
                vpart(i, recs[3])
                if i + 1 < NT:
                    normpart(i + 1, recs[4])
                interleave(S, recs)
            S.fence()
        with ExitStack() as es2:
            def sb2(name, shape, dt):
                return es2.enter_context(nc.sbuf_tensor(uname(name), shape, dt))
            yTb = sb2("e_yTb", [128, 4, T], BF16)
            wout = sb2("e_wout", [128, 8, 1024], BF16); wout_b = S.bufs("wout", 2)
            for dh in range(2):
                S.dma("pool", wout[:, :, dh * 512:(dh + 1) * 512], woutv[:, :, dh * 512:(dh + 1) * 512], writes=[wout_b[dh]])
            e_sb = [sb2("a_e%d" % i, [128, 512], BF16) for i in range(4)]; e_b = S.bufs("e", 4)
            l_sb = [sb2("a_l%d" % i, [128, 512], BF16) for i in range(4)]; l_b = S.bufs("l", 4)
            x_sb = [sb2("a_x%d" % i, [128, 512], BF16) for i in range(4)]; x_b = S.bufs("x", 4)
            a_sb = [sb2("a_a%d" % i, [128, 512], BF16) for i in range(4)]; a_b = S.bufs("a", 4)
            negtri, negcmp, masklt = C["negtri"], C["negcmp"], C["masklt"]
            cbuf = C["cb"]
            LA = 3
            qz = [sb2("a_qz%d" % i, [128, 2, 512], BF16) for i in range(2)]; qz_b = S.bufs("qz", 2)
            for i in range(2):
                S.op("pool", lambda e: e.memset(qz[i][:, :, :], 0.0), writes=[qz_b[i]])
            pcount = 0
            for qb in range(NQB):
                for hp in range(4):
                    pq = pcount % 2; pcount += 1
                    obase = 4 + 2 * pq
                    S.op("dve", lambda e: e.tensor_copy(out=qz[pq][0:64, 0, :], in_=qT[0:64, hp, qb * 512:(qb + 1) * 512]),
                         reads=qT_b[4 * qb:4 * qb + 4], writes=[qz_b[pq]])
                    S.op("dve", lambda e: e.tensor_copy(out=qz[pq][64:128, 1, :], in_=qT[64:128, hp, qb * 512:(qb + 1) * 512]),
                         reads=qT_b[4 * qb:4 * qb + 4], writes=[qz_b[pq]])
                    items = [(j, hh) for j in range(4 * qb + 3, -1, -1) for hh in range(2)]
                    jfirst = 4 * qb + 3

                    n = len(items)

                    def geo(i):
                        j, hh = items[i]
                        return j, hh, i % 4, max(0, (j - 4 * qb)) * 128

                    def pe_z(i):
                        j, hh, par, c0 = geo(i)
                        zb = i % 2
                        S.op("pe", lambda e: e.matmul(ps[zb][:, c0:512], kT[:, hp, j * 128:(j + 1) * 128],
                                                      qz[pq][:, hh, c0:512], start=True, stop=True),
                             reads=[kT_b[j], qz_b[pq]], writes=[psb[zb]])

                    def pe_tri(i):
                        j, hh, par, c0 = geo(i)
                        S.op("pe", lambda e: e.matmul(ps[2 + hh][:, c0:512], negtri[:, :], l_sb[par][:, c0:512],
                                                      start=(j == jfirst), stop=False, skip_group_check=True),
                             reads=[l_b[par], cbuf], writes=[psb[2 + hh]])

                    def pe_cmp_av(i):
                        j, hh, par, c0 = geo(i)
                        if j > 0:
                            S.op("pe", lambda e: e.matmul(ps[2 + hh][:, c0:512], negcmp[:, :], l_sb[par][:, c0:512],
                                                          start=False, stop=False, skip_group_check=True),
                                 reads=[l_b[par], cbuf], writes=[psb[2 + hh]])
                        S.op("pe", lambda e: e.matmul(ps[obase + hh][:, c0:512], v[:, j, hp * 128:(hp + 1) * 128],
                                                      a_sb[par][:, c0:512], start=(j == jfirst), stop=(j == 0),
                                                      skip_group_check=True),
                             reads=[v_b[j], a_b[par]], writes=[psb[obase + hh]])

                    def act_a(i):
                        j, hh, par, c0 = geo(i)
                        zb = i % 2
                        S.op("act", lambda e: e.activation(out=e_sb[par][:, c0:512], in_=ps[zb][:, c0:512], func=AF.Exp),
                             reads=[psb[zb]], writes=[e_b[par]])
                        if j >= 4 * qb:
                            S.op("dve", lambda e: e.tensor_tensor(out=e_sb[par][:, c0:c0 + 128], in0=e_sb[par][:, c0:c0 + 128],
                                                                  in1=masklt[:, :], op=ALU.mult),
                                 reads=[e_b[par], cbuf], writes=[e_b[par]])
                        S.op("act", lambda e: e.activation(out=l_sb[par][:, c0:512], in_=e_sb[par][:, c0:512], func=AF.Ln,
                                                           bias=1.0),
                             reads=[e_b[par]], writes=[l_b[par]])

                    def act_b(i):
                        j, hh, par, c0 = geo(i)
                        S.op("act", lambda e: e.activation(out=x_sb[par][:, c0:512], in_=ps[2 + hh][:, c0:512], func=AF.Exp),
                             reads=[psb[2 + hh]], writes=[x_b[par]])
                        S.op("dve", lambda e: e.tensor_tensor(out=a_sb[par][:, c0:512], in0=e_sb[par][:, c0:512],
                                                              in1=x_sb[par][:, c0:512], op=ALU.mult),
                             reads=[e_b[par], x_b[par]], writes=[a_b[par]])

                    for i in range(-1, n + 2):
                        if 0 <= i + 1 < n:
                            pe_z(i + 1)
                        if 0 <= i - 1 < n:
                            pe_tri(i - 1)
                        if 0 <= i - 2 < n:
                            pe_cmp_av(i - 2)
                        if 0 <= i < n:
                            act_a(i)
                        if 0 <= i - 1 < n:
                            act_b(i - 1)
                    for hh in range(2):
                        S.op("act", lambda e: e.activation(out=yTb[hh * 64:(hh + 1) * 64, hp, qb * 512:(qb + 1) * 512],
                                                           in_=ps[obase + hh][hh * 64:(hh + 1) * 64, :], func=AF.Copy),
                             reads=[psb[obase + hh]], writes=[yT_b[4 + hp][4 * qb + t] for t in range(4)])
            cnt = 0
            for i in range(NT):
                for dh in range(2):
                    pb = cnt % 4; cnt += 1
                    for kc in range(8):
                        lhs = yTa[:, kc, i * 128:(i + 1) * 128] if kc < 4 else yTb[:, kc - 4, i * 128:(i + 1) * 128]
                        S.op("pe", lambda e: e.matmul(ps[pb][:, :], lhs,
                                                      wout[:, kc, dh * 512:(dh + 1) * 512], start=(kc == 0), stop=(kc == 7)),
                             reads=[yT_b[kc][i], wout_b[dh]], writes=[psb[pb]], signal=(kc == 7))
                    S.op("dve", lambda e: e.tensor_tensor(out=X[:, i, dh * 512:(dh + 1) * 512], in0=ps[pb][:, :],
                                                          in1=X[:, i, dh * 512:(dh + 1) * 512], op=ALU.add),
                         reads=[psb[pb], Xb[i]], writes=[Xb[i]])
            S.fence()


def make_consts(S, nc, es, C):
    ident = es.enter_context(nc.sbuf_tensor("ident", [128, 128], F32))
    identb = es.enter_context(nc.sbuf_tensor("identb", [128, 128], BF16))
    tmp = es.enter_context(nc.sbuf_tensor("ctmp", [128, 128], F32))
    negtri = es.enter_context(nc.sbuf_tensor("negtri", [128, 128], BF16))
    negcmp = es.enter_context(nc.sbuf_tensor("negcmp", [128, 128], BF16))
    masklt = es.enter_context(nc.sbuf_tensor("masklt", [128, 128], BF16))
    maskle = es.enter_context(nc.sbuf_tensor("maskle", [128, 128], BF16))
    onesf = es.enter_context(nc.sbuf_tensor("onesf", [128, 128], F32))
    zerosf = es.enter_context(nc.sbuf_tensor("zerosf", [128, 128], F32))
    cb = S.buf("consts")
    S.op("pool", lambda e: e.memset(onesf[:, :], 1.0), writes=[cb])
    S.op("pool", lambda e: e.memset(zerosf[:, :], 0.0), writes=[cb])
    C.update(onesf=onesf, zerosf=zerosf, psb4a=S.buf("ps4a", persist=True), psb4b=S.buf("ps4b", persist=True), psb4c=S.buf("ps4c", persist=True))
    C.update(ident=ident, identb=identb, negtri=negtri, negcmp=negcmp, masklt=masklt, maskle=maskle, cb=cb)

    def sel(out, val, pattern, cm, op, base=0):
        S.op("pool", lambda e: e.memset(tmp[:, :], val), reads=[cb], writes=[cb])
        S.op("pool", lambda e: e.affine_select(out=tmp[:, :], in_=tmp[:, :], pattern=pattern, compare_op=op, fill=0.0,
                                               base=base, channel_multiplier=cm), reads=[cb], writes=[cb])
        S.op("pool", lambda e: e.tensor_copy(out=out[:, :], in_=tmp[:, :]), reads=[cb], writes=[cb])
    sel(ident, 1.0, [[-1, 128]], 1, ALU.is_equal)
    sel(identb, 1.0, [[-1, 128]], 1, ALU.is_equal)
    sel(negtri, -1.0, [[-1, 128]], 1, ALU.is_ge)
    sel(negcmp, -1.0, [[1, 128]], -1, ALU.is_gt)
    sel(masklt, 1.0, [[1, 128]], -1, ALU.is_gt)
    sel(maskle, 1.0, [[1, 128]], -1, ALU.is_ge)


def odd_mixer(S, nc, C, X, Xb, NT, P, gs_dram):
    ps, psb = C["ps"], C["psb"]
    ident, identb, onesf, zerosf, maskle, cbuf = C["ident"], C["identb"], C["onesf"], C["zerosf"], C["maskle"], C["cb"]
    T = NT * 128
    NQB = T // 512
    NP = 4 * NT
    KS = 128 ** -0.5
    with ExitStack() as es:
        def sb(name, shape, dt):
            return es.enter_context(nc.sbuf_tensor(uname(name), shape, dt))
        hT = sb("o_hT", [128, 8, T], BF16); hT_b = S.bufs("hT", NT)
        wout = sb("o_wout", [128, 8, 1024], BF16); wout_b = S.bufs("wout", 2)
        woutv = P["w_out"].rearrange("(kc p) d -> p kc d", p=128)
        winv = P["w_in"].rearrange("(kc p) f -> p kc f", p=128)
        ongb = sb("o_ong", [128, 1024], F32); ong_b = S.buf("ong")
        cw = sb("o_cw", [128, 8, 4], F32); cw_b = S.buf("cw")
        cbias = sb("o_cb", [128, 8], F32); cbias_b = S.buf("cbias")
        TS = sb("o_TS", [128, 4 * NP], F32); TS_b = S.buf("TS")
        DEC = sb("o_DEC", [128, NP], F32); DEC_b = S.buf("DEC")
        wg = sb("o_wg", [128, 8, 8], BF16); wg_b = S.buf("wg")
        S.dma("pool", wg[:, :, :], winv[:, :, 3072:3080], writes=[wg_b])
        for dh in range(2):
            S.dma("pool", wout[:, :, dh * 512:(dh + 1) * 512], woutv[:, :, dh * 512:(dh + 1) * 512], writes=[wout_b[dh]])
        S.dma("sp", ongb[:, :], P["on_g"].partition_broadcast(128), writes=[ong_b])
        for k_ in range(4):
            S.dma("sp", cw[:, :, k_], P["conv_w"][k_].rearrange("(i p) -> p i", p=128), writes=[cw_b], allow_slow_non_contiguous=True)
        S.dma("sp", cbias[:, :], P["conv_b"].rearrange("(i p) -> p i", p=128), writes=[cbias_b], allow_slow_non_contiguous=True)
        with ExitStack() as es1:
            def sb1(name, shape, dt):
                return es1.enter_context(nc.sbuf_tensor(uname(name), shape, dt))
            gbc = sb1("o_gbc", [128, 1024], F32); gbc_b = S.buf("gbc")
            S.dma("sp", gbc[:, :], P["g"].partition_broadcast(128), writes=[gbc_b])
            junk = sb1("o_junk", [128, 1024], BF16); junk_b = S.buf("junk")
            xs = [sb1("o_xs%d" % i, [128, 1024], BF16) for i in range(2)]; xs_b = S.bufs("xs", 2)
            st = [sb1("o_st%d" % i, [128, 4], F32) for i in range(2)]; st_b = S.bufs("st", 2)
            tmpn = (junk, junk_b, xs, xs_b, st, st_b)
            for i in range(NT):
                norm_transpose(S, C, X, Xb, i, gbc, gbc_b, tmpn, i % 2, i % 2, hT[:, :, i * 128:(i + 1) * 128], hT_b[i])
            gsb = sb1("o_gsb", [8, T], F32); gsb_b = S.buf("gsb")
            for blk in range(NQB):
                pb = 2 + blk % 2
                for kc in range(8):
                    S.op("pe", lambda e: e.matmul(ps[pb][0:8, :], wg[:, kc, :], hT[:, kc, blk * 512:(blk + 1) * 512],
                                                  start=(kc == 0), stop=(kc == 7)),
                         reads=[wg_b] + hT_b[blk * 4:(blk + 1) * 4], writes=[psb[pb]], signal=(kc == 7))
                S.op("act", lambda e: e.activation(out=gsb[:, blk * 512:(blk + 1) * 512], in_=ps[pb][0:8, :], func=AF.Copy),
                     reads=[psb[pb]], writes=[gsb_b])
            gsd_b = S.buf("gsd")
            S.dma("sp", gs_dram[:, :], gsb[:, :], reads=[gsb_b], writes=[gsd_b])
            G = {}
            for nm in ("I", "F", "e1", "sp", "bneg", "a", "cm", "Mx", "rowfac", "inter", "wkx", "en"):
                G[nm] = sb1("o_G" + nm, [NP, 128], F32)
            Gb = S.buf("G")
            S.dma("sp", G["I"][:, :], gs_dram[0:4, :].rearrange("h (c t) -> (h c) t", t=128), reads=[gsd_b], writes=[Gb])
            Fb = S.buf("GF")
            S.dma("sp", G["F"][:, :], gs_dram[4:8, :].rearrange("h (c t) -> (h c) t", t=128), reads=[gsd_b], writes=[Fb])
            bias = sb1("o_bias", [NP, 4], F32); bias_b = [S.buf("bias%d" % i) for i in range(8)]
            for h in range(4):
                S.dma("sp", bias[h * NT:(h + 1) * NT, 0:1], P["i_b"][h:h + 1].partition_broadcast(NT), writes=[bias_b[h]])
                S.dma("sp", bias[h * NT:(h + 1) * NT, 1:2], P["f_b"][h:h + 1].partition_broadcast(NT), writes=[bias_b[4 + h]])
            Kc = sb1("o_K", [NP, 4], F32)
            R = sb1("o_R", [1, 8 * NP], F32)
            allg = [Gb, Fb] + bias_b
            def gop(eng, fn):
                S.op(eng, fn, reads=allg + [cbuf], writes=[Gb])
            gop("dve", lambda e: e.tensor_scalar(out=bias[:, 2:3], in0=bias[:, 1:2], scalar1=-1.0, scalar2=None, op0=ALU.mult))
            gop("act", lambda e: e.activation(out=G["e1"][:, :], in_=G["F"][:, :], func=AF.Exp, bias=bias[:, 2:3], scale=-1.0))
            gop("act", lambda e: e.activation(out=G["sp"][:, :], in_=G["e1"][:, :], func=AF.Ln, bias=1.0))
            gop("dve", lambda e: e.tensor_tensor_scan(out=G["bneg"][:, :], data0=onesf[0:NP, :], data1=G["sp"][:, :],
                                                      initial=0.0, op0=ALU.mult, op1=ALU.add))
            gop("dve", lambda e: e.scalar_tensor_tensor(out=G["a"][:, :], in0=G["I"][:, :], scalar=bias[:, 0:1],
                                                        in1=G["bneg"][:, :], op0=ALU.add, op1=ALU.add))
            gop("dve", lambda e: e.tensor_tensor_scan(out=G["cm"][:, :], data0=zerosf[0:NP, :], data1=G["a"][:, :],
                                                      initial=-1e30, op0=ALU.add, op1=ALU.max))
            pr = 4
            S.op("pe", lambda e: e.transpose(out=ps[pr][0:1, 0:NP], in_=G["bneg"][:, 127:128], identity=ident[0:NP, 0:NP]),
                 reads=[Gb, cbuf], writes=[psb[pr]], signal=False)
            S.op("pe", lambda e: e.transpose(out=ps[pr][0:1, NP:2 * NP], in_=G["cm"][:, 127:128], identity=ident[0:NP, 0:NP]),
                 reads=[Gb, cbuf], writes=[psb[pr]])
            def rop(eng, fn, extra=()):
                S.op(eng, fn, reads=[Gb, cbuf] + list(extra), writes=[Gb])
            rop("act", lambda e: e.activation(out=R[:, 0:2 * NP], in_=ps[pr][0:1, 0:2 * NP], func=AF.Copy), [psb[pr]])
            rop("dve", lambda e: e.tensor_scalar(out=R[:, 2 * NP:3 * NP], in0=R[:, 0:NP], scalar1=-1.0, scalar2=None, op0=ALU.mult))
            for h in range(4):
                rop("dve", lambda e: e.tensor_tensor_scan(out=R[:, 3 * NP + h * NT:3 * NP + (h + 1) * NT],
                                                          data0=R[:, NP + h * NT:NP + (h + 1) * NT],
                                                          data1=R[:, 2 * NP + h * NT:2 * NP + (h + 1) * NT],
                                                          initial=0.0, op0=ALU.max, op1=ALU.add))
            rop("dve", lambda e: e.memset(R[:, 4 * NP:5 * NP], 0.0))
            rop("dve", lambda e: e.tensor_copy(out=R[:, 4 * NP:5 * NP].rearrange("o (h c) -> o h c", h=4)[:, :, 1:NT],
                                               in_=R[:, 3 * NP:4 * NP].rearrange("o (h c) -> o h c", h=4)[:, :, 0:NT - 1]))
            rop("dve", lambda e: e.tensor_tensor(out=R[:, 5 * NP:6 * NP], in0=R[:, 4 * NP:5 * NP], in1=R[:, NP:2 * NP], op=ALU.max))
            rop("dve", lambda e: e.tensor_tensor(out=R[:, 6 * NP:7 * NP], in0=R[:, 4 * NP:5 * NP], in1=R[:, 5 * NP:6 * NP],
                                                 op=ALU.subtract))
            rop("act", lambda e: e.activation(out=R[:, 7 * NP:8 * NP], in_=R[:, 6 * NP:7 * NP], func=AF.Exp))
            pk = 5
            S.op("pe", lambda e: e.matmul(ps[pk][0:NP, 0:1], R[0:1, 4 * NP:5 * NP], onesf[0:1, 0:1], start=True, stop=True),
                 reads=[Gb, cbuf], writes=[psb[pk]], signal=False)
            S.op("pe", lambda e: e.matmul(ps[pk][0:NP, 1:2], R[0:1, 5 * NP:6 * NP], onesf[0:1, 0:1], start=True, stop=True),
                 reads=[Gb, cbuf], writes=[psb[pk]])
            rop("act", lambda e: e.activation(out=Kc[:, 0:2], in_=ps[pk][0:NP, 0:2], func=AF.Copy), [psb[pk]])
            rop("dve", lambda e: e.tensor_scalar(out=Kc[:, 2:3], in0=Kc[:, 1:2], scalar1=-1.0, scalar2=None, op0=ALU.mult))
            rop("dve", lambda e: e.tensor_scalar(out=G["Mx"][:, :], in0=G["cm"][:, :], scalar1=Kc[:, 0:1], scalar2=None, op0=ALU.max))
            rop("act", lambda e: e.activation(out=G["rowfac"][:, :], in_=G["Mx"][:, :], func=AF.Exp, bias=Kc[:, 1:2], scale=-1.0))
            rop("act", lambda e: e.activation(out=G["inter"][:, :], in_=G["Mx"][:, :], func=AF.Exp, bias=Kc[:, 0:1], scale=-1.0))
            rop("act", lambda e: e.activation(out=G["wkx"][:, :], in_=G["a"][:, :], func=AF.Exp, bias=Kc[:, 2:3], scale=1.0))
            rop("dve", lambda e: e.tensor_tensor(out=G["e1"][:, :], in0=G["bneg"][:, :], in1=G["Mx"][:, :], op=ALU.subtract))
            rop("act", lambda e: e.activation(out=G["en"][:, :], in_=G["e1"][:, :], func=AF.Exp))
            pt = 6
            for qi, nm in enumerate(("rowfac", "inter", "wkx", "en")):
                S.op("pe", lambda e: e.transpose(out=ps[pt][:, qi * NP:(qi + 1) * NP], in_=G[nm][:, :], identity=ident[0:NP, 0:NP]),
                     reads=[Gb, cbuf], writes=[psb[pt]], signal=(qi == 3))
            S.op("act", lambda e: e.activation(out=TS[:, :], in_=ps[pt][:, 0:4 * NP], func=AF.Copy), reads=[psb[pt]], writes=[TS_b])
            pd = 7
            S.op("pe", lambda e: e.matmul(ps[pd][:, 0:NP], onesf[0:1, :], R[0:1, 7 * NP:8 * NP], start=True, stop=True),
                 reads=[Gb, cbuf], writes=[psb[pd]])
            S.op("act", lambda e: e.activation(out=DEC[:, :], in_=ps[pd][:, 0:NP], func=AF.Copy), reads=[psb[pd]], writes=[DEC_b])
            S.fence()
        whd = sb("o_whd", [128, 8, 768], BF16); whd_b = S.bufs("whd", 4)
        pre = sb("o_pre", [128, 2, 3 + T], F32); pre_b = S.bufs("pre", 2)
        acc = sb("o_acc", [128, T], F32); acc_b = S.buf("acc")
        qkT = sb("o_qkT", [128, 2, T], BF16); qkT_b = S.bufs("qkT", 2)
        vone = sb("o_vone", [128, NT, 257], BF16); vone_b = S.bufs("vone", NT)
        sog = sb("o_sog", [128, NT, 256], BF16); sog_b = S.bufs("sog", NT)
        C32 = sb("o_C32", [128, 257], F32); C32_b = S.buf("C32")
        Cx = sb("o_Cx", [128, 257], BF16); Cx_b = S.buf("Cx")
        vext = [sb("o_vext%d" % i, [128, 257], BF16) for i in range(2)]; vext_b = S.bufs("vext", 2)
        sm = [sb("o_sm%d" % i, [128, 128], BF16) for i in range(2)]; sm_b = S.bufs("sm", 2)
        ktok = [sb("o_ktok%d" % i, [128, 128], BF16) for i in range(2)]; ktok_b = S.bufs("ktok", 2)
        o1 = [sb("o_o1%d" % i, [128, 257], F32) for i in range(2)]; o1_b = S.bufs("o1", 2)
        o2 = [sb("o_o2%d" % i, [128, 257], F32) for i in range(2)]; o2_b = S.bufs("o2", 2)
        sc = [sb("o_sc%d" % i, [128, 8], F32) for i in range(2)]; sc_b = S.bufs("sc", 2)
        jk = sb("o_jk", [128, 256], BF16); jk_b = S.buf("jk")
        yf = [sb("o_yf%d" % i, [128, 256], F32) for i in range(2)]; yf_b = S.bufs("yf", 2)
        yb = [sb("o_yb%d" % i, [128, 256], BF16) for i in range(2)]; yb_b = S.bufs("yb", 2)
        yTh = [sb("o_yTh%d" % i, [128, 2, 128], BF16) for i in range(2)]; yTh_b = S.bufs("yTh", 2)
        S.op("dve", lambda e: e.memset(pre[:, :, 0:3], 0.0), writes=pre_b)
        S.op("pool", lambda e: e.memset(vone[:, :, 256:257], 1.0), writes=vone_b)
        for h in range(4):
            srcs = ((h * 128, 128), (512 + h * 128, 128), (1024 + h * 256, 256), (2048 + h * 256, 256))
            off = 0
            for si, (c0, n) in enumerate(srcs):
                S.dma("pool", whd[:, :, off:off + n], winv[:, :, c0:c0 + n], writes=[whd_b[si]])
                off += n
            cnt = 0
            for w in range(2):
                for blk in range(NQB):
                    pb = cnt % 2; cnt += 1
                    for kc in range(8):
                        S.op("pe", lambda e: e.matmul(ps[pb][:, :], whd[:, kc, w * 128:(w + 1) * 128],
                                                      hT[:, kc, blk * 512:(blk + 1) * 512], start=(kc == 0), stop=(kc == 7)),
                             reads=[whd_b[w]] + hT_b[blk * 4:(blk + 1) * 4], writes=[psb[pb]], signal=(kc == 7))
                    S.op("act", lambda e: e.activation(out=pre[:, w, 3 + blk * 512:3 + (blk + 1) * 512], in_=ps[pb][:, :],
                                                       func=AF.Copy), reads=[psb[pb]], writes=[pre_b[w]])
                idx = w * 4 + h
                S.op("dve", lambda e: e.tensor_scalar(out=acc[:, :], in0=pre[:, w, 0:T], scalar1=cw[:, idx, 0:1], scalar2=None,
                                                      op0=ALU.mult), reads=[pre_b[w], cw_b], writes=[acc_b])
                for k in range(1, 4):
                    S.op("dve", lambda e: e.scalar_tensor_tensor(out=acc[:, :], in0=pre[:, w, k:k + T], scalar=cw[:, idx, k:k + 1],
                                                                 in1=acc[:, :], op0=ALU.mult, op1=ALU.add),
                         reads=[pre_b[w], cw_b, acc_b], writes=[acc_b])
                if w == 0:
                    S.op("act", lambda e: e.activation(out=qkT[:, 0, :], in_=acc[:, :], func=AF.Silu, bias=cbias[:, idx:idx + 1]),
                         reads=[acc_b, cbias_b], writes=[qkT_b[0]])
                else:
                    S.op("act", lambda e: e.activation(out=acc[:, :], in_=acc[:, :], func=AF.Silu, bias=cbias[:, idx:idx + 1]),
                         reads=[acc_b, cbias_b], writes=[acc_b])
                    S.op("dve", lambda e: e.tensor_scalar(out=qkT[:, 1, :], in0=acc[:, :], scalar1=KS, scalar2=None, op0=ALU.mult),
                         reads=[acc_b], writes=[qkT_b[1]])
            for i in range(NT):
                pb = 2 + i % 2
                for kc in range(8):
                    S.op("pe", lambda e: e.matmul(ps[pb][:, :], hT[:, kc, i * 128:(i + 1) * 128], whd[:, kc, 256:768],
                                                  start=(kc == 0), stop=(kc == 7)),
                         reads=[hT_b[i], whd_b[2], whd_b[3]], writes=[psb[pb]], signal=(kc == 7))
                S.op("act", lambda e: e.activation(out=vone[:, i, 0:256], in_=ps[pb][:, 0:256], func=AF.Copy),
                     reads=[psb[pb]], writes=[vone_b[i]])
                S.op("act", lambda e: e.activation(out=sog[:, i, :], in_=ps[pb][:, 256:512], func=AF.Sigmoid),
                     reads=[psb[pb]], writes=[sog_b[i]])
            S.op("dve", lambda e: e.memset(C32[:, :], 0.0), writes=[C32_b])
            S.op("pool", lambda e: e.memset(Cx[:, :], 0.0), writes=[Cx_b])
            psb4a = psb4b = psb[4]

            def stageF(c, S=S):
                p = h * NT + c
                k2 = c % 2
                cs = slice(c * 128, (c + 1) * 128)
                S.op("dve", lambda e: e.tensor_scalar(out=vext[k2][:, :], in0=vone[:, c, :], scalar1=TS[:, 2 * NP + p:2 * NP + p + 1],
                                                      scalar2=None, op0=ALU.mult), reads=[vone_b[c], TS_b], writes=[vext_b[k2]])
                S.op("pe", lambda e: e.matmul(ps[4][:, 0:128], qkT[:, 1, cs], qkT[:, 0, cs], start=True, stop=True),
                     reads=[qkT_b[0], qkT_b[1]], writes=[psb4a])
                S.op("dve", lambda e: e.tensor_tensor(out=sm[k2][:, :], in0=ps[4][:, 0:128], in1=maskle[:, :], op=ALU.mult),
                     reads=[psb4a, cbuf], writes=[sm_b[k2]])
                S.op("pe", lambda e: e.matmul(ps[5][:, 0:257], sm[k2][:, :], vext[k2][:, :], start=True, stop=True),
                     reads=[sm_b[k2], vext_b[k2]], writes=[psb[5]])
                S.op("act", lambda e: e.activation(out=o1[k2][:, :], in_=ps[5][:, 0:257], func=AF.Identity,
                                                   scale=TS[:, p:p + 1]), reads=[psb[5], TS_b], writes=[o1_b[k2]])
                pTk = ps[7][:, :].bitcast(BF16)
                S.op("pe", lambda e: e.transpose(out=pTk[:, k2 * 128:(k2 + 1) * 128], in_=qkT[:, 1, cs], identity=identb[:, :]),
                     reads=[qkT_b[1], cbuf], writes=[psb[7]])
                S.op("act", lambda e: e.activation(out=ktok[k2][:, :], in_=pTk[:, k2 * 128:(k2 + 1) * 128], func=AF.Copy),
                     reads=[psb[7]], writes=[ktok_b[k2]])
                S.op("pe", lambda e: e.matmul(ps[2 + k2][:, 0:257], ktok[k2][:, :], vext[k2][:, :], start=True, stop=True),
                     reads=[ktok_b[k2], vext_b[k2]], writes=[psb[2 + k2]])

            def stageG(c, S=S):
                p = h * NT + c
                k2 = c % 2
                cs = slice(c * 128, (c + 1) * 128)
                S.op("pe", lambda e: e.matmul(ps[6][:, 0:257], qkT[:, 0, cs], Cx[:, :], start=True, stop=True),
                     reads=[qkT_b[0], Cx_b], writes=[psb[6]])
                S.op("dve", lambda e: e.scalar_tensor_tensor(out=o2[k2][:, :], in0=ps[6][:, 0:257], scalar=TS[:, NP + p:NP + p + 1],
                                                             in1=o1[k2][:, :], op0=ALU.mult, op1=ALU.add),
                     reads=[psb[6], TS_b, o1_b[k2]], writes=[o2_b[k2]])
                S.op("dve", lambda e: e.scalar_tensor_tensor(out=C32[:, :], in0=C32[:, :], scalar=DEC[:, p:p + 1],
                                                             in1=ps[2 + k2][:, 0:257], op0=ALU.mult, op1=ALU.add),
                     reads=[C32_b, DEC_b, psb[2 + k2]], writes=[C32_b])
                S.op("act", lambda e: e.activation(out=Cx[:, :], in_=C32[:, :], func=AF.Copy), reads=[C32_b], writes=[Cx_b])

            def stageH(c, S=S):
                p = h * NT + c
                k2 = c % 2
                s_ = sc[k2]; s_b = sc_b[k2]
                S.op("dve", lambda e: e.tensor_scalar(out=s_[:, 7:8], in0=o2[k2][:, 256:257], scalar1=-1.0, scalar2=None,
                                                      op0=ALU.mult), reads=[o2_b[k2]], writes=[s_b])
                S.op("dve", lambda e: e.tensor_tensor(out=s_[:, 0:1], in0=o2[k2][:, 256:257], in1=s_[:, 7:8], op=ALU.max),
                     reads=[o2_b[k2], s_b], writes=[s_b])
                S.op("dve", lambda e: e.tensor_tensor(out=s_[:, 0:1], in0=s_[:, 0:1], in1=TS[:, 3 * NP + p:3 * NP + p + 1],
                                                      op=ALU.max), reads=[s_b, TS_b], writes=[s_b])
                S.op("dve", lambda e: e.reciprocal(out=s_[:, 1:2], in_=s_[:, 0:1]), reads=[s_b], writes=[s_b])
                S.op("act", lambda e: e.activation(out=jk[:, :], in_=o2[k2][:, 0:256], func=AF.Square, scale=s_[:, 1:2],
                                                   accum_out=s_[:, 2:3]), reads=[o2_b[k2], s_b], writes=[jk_b, s_b])
                S.op("dve", lambda e: e.tensor_scalar(out=s_[:, 3:4], in0=s_[:, 2:3], scalar1=1.0 / 256, scalar2=EPS,
                                                      op0=ALU.mult, op1=ALU.add), reads=[s_b], writes=[s_b])
                S.op("act", lambda e: e.sqrt(out=s_[:, 4:5], in_=s_[:, 3:4]), reads=[s_b], writes=[s_b])
                S.op("dve", lambda e: e.reciprocal(out=s_[:, 5:6], in_=s_[:, 4:5]), reads=[s_b], writes=[s_b])
                S.op("dve", lambda e: e.tensor_tensor(out=s_[:, 6:7], in0=s_[:, 5:6], in1=s_[:, 1:2], op=ALU.mult),
                     reads=[s_b], writes=[s_b])
                S.op("dve", lambda e: e.scalar_tensor_tensor(out=yf[k2][:, :], in0=o2[k2][:, 0:256], scalar=s_[:, 6:7],
                                                             in1=ongb[:, h * 256:(h + 1) * 256], op0=ALU.mult, op1=ALU.mult),
                     reads=[o2_b[k2], s_b, ong_b], writes=[yf_b[k2]])
                S.op("pool", lambda e: e.tensor_tensor(out=yb[k2][:, :], in0=yf[k2][:, :], in1=sog[:, c, :], op=ALU.mult),
                     reads=[yf_b[k2], sog_b[c]], writes=[yb_b[k2]])
                pTy = ps[4][:, :].bitcast(BF16)
                for kk in range(2):
                    S.op("pe", lambda e, kk=kk: e.transpose(out=pTy[:, 512 + kk * 128:512 + (kk + 1) * 128],
                                                     in_=yb[k2][:, kk * 128:(kk + 1) * 128], identity=identb[:, :]),
                         reads=[yb_b[k2], cbuf], writes=[psb4b], signal=(kk == 1))
                S.op("act", lambda e: e.activation(out=yTh[k2][:, :, :], in_=pTy[:, 512:768].rearrange("p (k t) -> p k t", k=2),
                                                   func=AF.Copy), reads=[psb4b], writes=[yTh_b[k2]])
                for dh in range(2):
                    pb = dh
                    for kk in range(2):
                        S.op("pe", lambda e, kk=kk, dh=dh, pb=pb: e.matmul(ps[pb][:, :], yTh[k2][:, kk, :],
                                                                           wout[:, 2 * h + kk, dh * 512:(dh + 1) * 512],
                                                                           start=(kk == 0), stop=(kk == 1)),
                             reads=[yTh_b[k2], wout_b[dh]], writes=[psb[pb]], signal=(kk == 1))
                    S.op("dve", lambda e, dh=dh, pb=pb: e.tensor_tensor(out=X[:, c, dh * 512:(dh + 1) * 512], in0=ps[pb][:, :],
                                                                        in1=X[:, c, dh * 512:(dh + 1) * 512], op=ALU.add),
                         reads=[psb[pb], Xb[c]], writes=[Xb[c]])

            stageF(0)
            for c in range(NT):
                rf, rg, rh = Recorder(), Recorder(), Recorder()
                if c + 1 < NT:
                    stageF(c + 1, rf)
                stageG(c, rg)
                if c >= 1:
                    stageH(c - 1, rh)
                interleave(S, [rg, rf, rh])
            stageH(NT - 1)
        S.fence()


def mlp_layer(S, nc, C, X, Xb, NT, g_row, w1, w2):
    ps, psb = C["ps"], C["psb"]
    identb = C["identb"]
    TBT = min(8, NT)
    TB = TBT * 128
    NH = TB // 512
    NB = NT // TBT
    with ExitStack() as es:
        def sb(name, shape, dt):
            return es.enter_context(nc.sbuf_tensor(uname(name), shape, dt))
        gbc = sb("m_gbc", [128, 1024], F32); gbc_b = S.buf("gbc")
        nhb = min(2, NB)
        hT = [sb("m_hT%d" % i, [128, 8, TB], BF16) for i in range(nhb)]
        hT_b = [S.bufs("hT%d_" % i, TBT) for i in range(nhb)]
        h1T = sb("m_h1T", [128, 32, TB], BF16); h1T_b = [[S.buf("h1T") for _ in range(NH)] for _ in range(32)]
        junk = sb("m_junk", [128, 1024], BF16); junk_b = S.buf("junk")
        xs = [sb("m_xs%d" % i, [128, 1024], BF16) for i in range(2)]; xs_b = S.bufs("xs", 2)
        st = [sb("m_st%d" % i, [128, 4], F32) for i in range(2)]; st_b = S.bufs("st", 2)
        rl = [sb("m_rl%d" % i, [128, 512], BF16) for i in range(2)]; rl_b = S.bufs("rl", 2)
        w1t = [sb("m_w1_%d" % i, [128, 8, 512], BF16) for i in range(2)]; w1_b = S.bufs("w1t", 2)
        w2t = [sb("m_w2_%d" % i, [128, 4, 512], BF16) for i in range(2)]; w2_b = S.bufs("w2t", 2)
        NW2 = 2

        S.dma("sp", gbc[:, :], g_row.partition_broadcast(128), writes=[gbc_b])
        w1v = w1.rearrange("(kc p) f -> p kc f", p=128)
        w2v = w2.rearrange("(g fi p) d -> g p fi d", p=128, fi=4)
        w1cnt = [0]
        w2cnt = [0]

        def load_w1(blk):
            k = w1cnt[0] % 2; w1cnt[0] += 1
            S.dma("pool", w1t[k][:, :, :], w1v[:, :, blk * 512:(blk + 1) * 512], writes=[w1_b[k]])

        def load_w2(idx):
            k = w2cnt[0] % NW2; w2cnt[0] += 1
            dh, g = divmod(idx, 8)
            S.dma("pool", w2t[k][:, :, :], w2v[g, :, :, dh * 512:(dh + 1) * 512], writes=[w2_b[k]])

        nstate = [0]

        def norm_tile(b, j):
            ti = b * TBT + j
            k = nstate[0] % 2; nstate[0] += 1
            hb = b % nhb
            S.op("act", lambda e: e.activation(out=junk[:, :], in_=X[:, ti, :], func=AF.Square, accum_out=st[k][:, 0:1]),
                 reads=[Xb[ti]], writes=[junk_b, st_b[k]])
            S.op("dve", lambda e: e.tensor_scalar(out=st[k][:, 1:2], in0=st[k][:, 0:1], scalar1=1.0 / 1024, scalar2=EPS,
                                                  op0=ALU.mult, op1=ALU.add), reads=[st_b[k]], writes=[st_b[k]])
            S.op("act", lambda e: e.sqrt(out=st[k][:, 2:3], in_=st[k][:, 1:2]), reads=[st_b[k]], writes=[st_b[k]])
            S.op("dve", lambda e: e.reciprocal(out=st[k][:, 3:4], in_=st[k][:, 2:3]), reads=[st_b[k]], writes=[st_b[k]])
            S.op("dve", lambda e: e.scalar_tensor_tensor(out=xs[k][:, :], in0=X[:, ti, :], scalar=st[k][:, 3:4],
                                                         in1=gbc[:, :], op0=ALU.mult, op1=ALU.mult),
                 reads=[Xb[ti], st_b[k], gbc_b], writes=[xs_b[k]])
            pb = k
            pT = ps[pb][:, :].bitcast(BF16)
            for c in range(8):
                S.op("pe", lambda e: e.transpose(out=pT[:, c * 128:(c + 1) * 128], in_=xs[k][:, c * 128:(c + 1) * 128],
                                                 identity=identb[:, :]),
                     reads=[xs_b[k], C["cb"]], writes=[psb[pb]], signal=(c == 7))
            S.op("act", lambda e: e.activation(out=hT[hb][:, :, j * 128:(j + 1) * 128],
                                               in_=pT.rearrange("p (c t) -> p c t", c=8), func=AF.Copy),
                 reads=[psb[pb]], writes=[hT_b[hb][j]])

        load_w1(0); load_w1(1)
        for j in range(TBT):
            norm_tile(0, j)
        cnt = 0
        for b in range(NB):
            hb = b % nhb
            H = hT[hb]; Hb = hT_b[hb]
            for blk in range(8):
                k = (b * 8 + blk) % 2
                wt = w1t[k]; wb = w1_b[k]
                for fi in range(4):
                    fc = blk * 4 + fi
                    pbase = 2 + NH * (cnt % 2); cnt += 1
                    for kc in range(8):
                        for th in range(NH):
                            S.op("pe", lambda e: e.matmul(ps[pbase + th][:, :], wt[:, kc, fi * 128:(fi + 1) * 128],
                                                          H[:, kc, th * 512:(th + 1) * 512], start=(kc == 0), stop=(kc == 7)),
                                 reads=[wb] + Hb[th * 4:(th + 1) * 4], writes=[psb[pbase + th]], signal=(kc == 7))
                    for th in range(NH):
                        pb = pbase + th
                        kr = th
                        S.op("act", lambda e: e.activation(out=rl[kr][:, :], in_=ps[pb][:, :], func=AF.Relu),
                             reads=[psb[pb]], writes=[rl_b[kr]])
                        S.op("dve", lambda e: e.tensor_tensor(out=h1T[:, fc, th * 512:(th + 1) * 512], in0=rl[kr][:, :],
                                                              in1=rl[kr][:, :], op=ALU.mult),
                             reads=[rl_b[kr]], writes=[h1T_b[fc][th]])
                if blk + 2 < 8:
                    load_w1(blk + 2)
                elif blk == 6:
                    load_w2(0)
                else:
                    load_w2(1)
                if b + 1 < NB and blk < TBT:
                    norm_tile(b + 1, blk)
            for dh in range(2):
                for g in range(8):
                    idx = dh * 8 + g
                    k = idx % NW2
                    wt = w2t[k]; wb = w2_b[k]
                    for fi in range(4):
                        fc = g * 4 + fi
                        for j in range(TBT):
                            th = j // 4
                            S.op("pe", lambda e: e.matmul(ps[j][:, :], h1T[:, fc, j * 128:(j + 1) * 128], wt[:, fi, :],
                                                          start=(fc == 0), stop=(fc == 31)),
                                 reads=[wb, h1T_b[fc][th]], writes=[psb[j]], signal=(fc == 31 or fi == 3))
                    if idx + NW2 < 16:
                        load_w2(idx + NW2)
                    elif b + 1 < NB:
                        load_w1(idx + NW2 - 16)
                for j in range(TBT):
                    ti = b * TBT + j
                    S.op("dve", lambda e: e.tensor_tensor(out=X[:, ti, dh * 512:(dh + 1) * 512], in0=ps[j][:, :],
                                                          in1=X[:, ti, dh * 512:(dh + 1) * 512], op=ALU.add),
                         reads=[psb[j], Xb[ti]], writes=[Xb[ti]])
        S.fence()

T_SEQ = 2048
NSEQ_CORE = 2
N_CORES = 8
IN_SHAPES = {
    'mix_norm_g': [4, 1024], 'mlp_norm_g': [4, 1024], 'mlp_w1': [4, 1024, 4096], 'mlp_w2': [4, 4096, 1024],
    'ev_w_in': [2, 1024, 2560], 'ev_w_out': [2, 1024, 1024], 'sg_ln_g': [2, 512], 'sg_ln_b': [2, 512],
    'sg_w': [2, 8, 128, 128], 'sg_b': [2, 8, 128], 'sb_q_norm_g': [2, 64], 'sb_k_norm_g': [2, 64],
    'od_w_in': [2, 1024, 3080], 'od_conv_w': [2, 4, 1024], 'od_conv_b': [2, 1024], 'od_i_b': [2, 4], 'od_f_b': [2, 4],
    'od_out_norm_g': [2, 1024], 'od_w_out': [2, 1024, 1024],
}


def build_program(T=T_SEQ, nseq=NSEQ_CORE, layers=(0, 1, 2, 3)):
    NT = T // 128
    nc = bass.Bass("TRN2", target_bir_lowering=False)
    x = nc.dram_tensor("x", [nseq, T, 1024], F32, kind="ExternalInput").ap()
    W = {k: nc.dram_tensor(k, shp, F32, kind="ExternalInput").ap() for k, shp in IN_SHAPES.items()}
    y = nc.dram_tensor("y", [nseq, T, 1024], F32, kind="ExternalOutput").ap()
    gs = nc.dram_tensor("gs_scratch", [8, T], F32, kind="Internal").ap()
    with ExitStack() as es:
        S = Sched(nc, es)
        C = {}
        C["ps"] = [es.enter_context(nc.psum_tensor("ps%d" % i, [128, 512], F32)) for i in range(8)]
        C["psb"] = S.bufs("ps", 8)
        make_consts(S, nc, es, C)
        X = es.enter_context(nc.sbuf_tensor("X", [128, NT, 1024], F32))
        Xb = S.bufs("X", NT, persist=True)
        for s in range(nseq):
            xv = x[s].rearrange("(n p) d -> p n d", p=128)
            yv = y[s].rearrange("(n p) d -> p n d", p=128)
            for i in range(NT):
                S.dma("sp", X[:, i, :], xv[:, i, :], writes=[Xb[i]])
            for l in layers:
                j = l // 2
                if l % 2 == 0:
                    P = dict(g=W['mix_norm_g'][l], w_in=W['ev_w_in'][j], w_out=W['ev_w_out'][j], ln_g=W['sg_ln_g'][j],
                             ln_b=W['sg_ln_b'][j], sg_w=W['sg_w'][j], sg_b=W['sg_b'][j], qg=W['sb_q_norm_g'][j],
                             kg=W['sb_k_norm_g'][j])
                    even_mixer(S, nc, C, X, Xb, NT, P)
                else:
                    P = dict(g=W['mix_norm_g'][l], w_in=W['od_w_in'][j], w_out=W['od_w_out'][j], conv_w=W['od_conv_w'][j],
                             conv_b=W['od_conv_b'][j], i_b=W['od_i_b'][j], f_b=W['od_f_b'][j], on_g=W['od_out_norm_g'][j])
                    odd_mixer(S, nc, C, X, Xb, NT, P, gs)
                mlp_layer(S, nc, C, X, Xb, NT, W['mlp_norm_g'][l], W['mlp_w1'][l], W['mlp_w2'][l])
            for i in range(NT):
                S.dma("sp", yv[:, i, :], X[:, i, :], reads=[Xb[i]])
        S.finish()
    return nc


def kernel(**inputs):
    x = np.ascontiguousarray(np.asarray(inputs['x'], dtype=np.float32))
    B = x.shape[0]
    per = B // N_CORES
    nc = build_program(T=x.shape[1], nseq=per)
    shared = {k: np.ascontiguousarray(np.asarray(inputs[k], dtype=np.float32)) for k in IN_SHAPES}
    in_maps = []
    for c in range(N_CORES):
        m = dict(shared)
        m['x'] = np.ascontiguousarray(x[c * per:(c + 1) * per])
        in_maps.append(m)
    res = run_bass_kernel_spmd(nc, in_maps, core_ids=list(range(N_CORES)))
    return np.concatenate([np.asarray(r['y']) for r in res.results], axis=0).astype(np.float32)
```

```python
from contextlib import ExitStack
from concourse.bass_utils import run_bass_kernel_spmd
import numpy as np
import concourse.bass as bass
import concourse.mybir as mybir

F32 = mybir.dt.float32
BF16 = mybir.dt.bfloat16
AF = mybir.ActivationFunctionType
ALU = mybir.AluOpType
AX = mybir.AxisListType


_UID = [0]


def uname(name):
    _UID[0] += 1
    return "%s_%d" % (name, _UID[0])


class Buf:
    __slots__ = ("name", "w", "r", "sem", "semval", "persist")

    def __init__(self, name, fence, persist=False):
        self.persist = persist
        self.name = name
        self.w = None
        self.r = list(fence)
        self.sem = None
        self.semval = 0


class EngState:
    def __init__(self, name, obj, sem):
        self.name = name
        self.obj = obj
        self.sem = sem
        self.count = 0
        self.know = {}
        self.last = None


class Sched:
    def __init__(self, nc, stack):
        self.nc = nc
        self.stack = stack
        self.E = {}
        for name, obj in (("pe", nc.tensor), ("act", nc.scalar), ("dve", nc.vector),
                          ("pool", nc.gpsimd), ("sp", nc.sync)):
            sem = stack.enter_context(nc.semaphore("sem_" + name))
            self.E[name] = EngState(name, obj, sem)
        self.fence_recs = []
        self.dma_recs = []
        self.pending_pe = []
        self.sem_pool = {}
        self.phase_bufs = []
        self.nsem = 0
        self.nwaits = 0
        self.nops = 0

    def buf(self, name, persist=False):
        return Buf(name, self.fence_recs, persist)

    def bufs(self, name, n, persist=False):
        return [Buf("%s%d" % (name, i), self.fence_recs, persist) for i in range(n)]

    def _waits(self, eng, reads, writes):
        st = self.E[eng]
        deps = []
        for b in reads:
            if b.w is not None:
                deps.append(b.w)
        for b in writes:
            if b.w is not None:
                deps.append(b.w)
            deps.extend(b.r)
        pesem = self.E["pe"].sem
        seen = set()
        for rec in deps:
            if id(rec) in seen:
                continue
            seen.add(id(rec))
            s, v, clk = rec
            if eng == "pe" and s is pesem:
                continue
            assert v is not None, "dependency on unsignaled PE op"
            if st.know.get(s, 0) >= v:
                continue
            st.obj.wait_ge(s, v)
            self.nwaits += 1
            for ks, kv in clk.items():
                if st.know.get(ks, 0) < kv:
                    st.know[ks] = kv

    def op(self, eng, fn, reads=(), writes=(), signal=True):
        st = self.E[eng]
        self._waits(eng, reads, writes)
        ins = fn(st.obj)
        self.nops += 1
        if signal:
            st.count += 1
            ins.then_inc(st.sem, 1)
            clk = dict(st.know)
            clk[st.sem] = st.count
            rec = [st.sem, st.count, clk]
            if eng == "pe":
                for p in self.pending_pe:
                    p[1] = st.count
                    p[2] = clk
                self.pending_pe = []
            st.last = rec
        else:
            assert eng == "pe"
            rec = [st.sem, None, None]
            self.pending_pe.append(rec)
        for b in reads:
            b.r.append(rec)
        for b in writes:
            b.w = rec
            b.r = []
        return ins

    def dma(self, q, out, in_, reads=(), writes=(), **kw):
        st = self.E[q]
        self._waits(q, reads, writes)
        owner = writes[0] if writes else reads[0]
        if owner.sem is None:
            pool = self.sem_pool.setdefault(q, [])
            if pool and not owner.persist:
                owner.sem, owner.semval = pool.pop()
            else:
                self.nsem += 1
                owner.sem = self.stack.enter_context(self.nc.semaphore("dsem%d" % self.nsem))
            if not owner.persist:
                self.phase_bufs.append((owner, q))
        owner.semval += 16
        ins = st.obj.dma_start(out=out, in_=in_, **kw)
        ins.then_inc(owner.sem, 16)
        clk = dict(st.know)
        clk[owner.sem] = owner.semval
        rec = [owner.sem, owner.semval, clk]
        self.dma_recs.append(rec)
        for b in reads:
            b.r.append(rec)
        for b in writes:
            b.w = rec
            b.r = []
        return ins

    def fence(self):
        sp = self.E["sp"]
        for rec in self.dma_recs:
            s, v, clk = rec
            if sp.know.get(s, 0) >= v:
                continue
            sp.obj.wait_ge(s, v)
            for ks, kv in clk.items():
                if sp.know.get(ks, 0) < kv:
                    sp.know[ks] = kv
        self.dma_recs = []
        for b, bq in self.phase_bufs:
            self.sem_pool.setdefault(bq, []).append((b.sem, b.semval))
            b.sem = None
        self.phase_bufs = []
        ins = sp.obj.nop()
        sp.count += 1
        ins.then_inc(sp.sem, 1)
        clk = dict(sp.know)
        clk[sp.sem] = sp.count
        sp.last = [sp.sem, sp.count, clk]
        assert not self.pending_pe
        recs = []
        for name, st in self.E.items():
            if st.last is not None:
                recs.append(st.last)
        self.fence_recs = recs

    def finish(self):
        self.fence()


class Recorder:
    def __init__(self):
        self.calls = []

    def op(self, eng, fn, reads=(), writes=(), signal=True):
        self.calls.append((eng, fn, list(reads), list(writes), signal))


def interleave(S, recs):
    lists = [r.calls for r in recs if r.calls]
    pos = [0] * len(lists)
    total = sum(len(l) for l in lists)
    done = 0
    while done < total:
        best, bf = None, None
        for t, l in enumerate(lists):
            if pos[t] >= len(l):
                continue
            frac = pos[t] / len(l)
            if bf is None or frac < bf:
                best, bf = t, frac
        l = lists[best]
        while True:
            eng, fn, reads, writes, signal = l[pos[best]]
            S.op(eng, fn, reads, writes, signal)
            pos[best] += 1
            done += 1
            if signal or pos[best] >= len(l):
                break


EPS = 1e-6

def rstd_ops(S, st, st_b, n, inv_n):
    S.op("dve", lambda e: e.tensor_scalar(out=st[:, n:2 * n], in0=st[:, 0:n], scalar1=inv_n, scalar2=EPS,
                                          op0=ALU.mult, op1=ALU.add), reads=[st_b], writes=[st_b])
    S.op("act", lambda e: e.sqrt(out=st[:, 2 * n:3 * n], in_=st[:, n:2 * n]), reads=[st_b], writes=[st_b])
    S.op("dve", lambda e: e.reciprocal(out=st[:, 3 * n:4 * n], in_=st[:, 2 * n:3 * n]), reads=[st_b], writes=[st_b])


def norm_transpose(S, C, X, Xb, ti, gbc, gbc_b, tmp, k, pb, out_ap, out_b):
    ps, psb, identb = C["ps"], C["psb"], C["identb"]
    junk, junk_b, xs, xs_b, st, st_b = tmp
    S.op("act", lambda e: e.activation(out=xs[k][:, :], in_=X[:, ti, :], func=AF.Square, accum_out=st[k][:, 0:1]),
         reads=[Xb[ti]], writes=[xs_b[k], st_b[k]])
    rstd_ops(S, st[k], st_b[k], 1, 1.0 / 1024)
    S.op("dve", lambda e: e.scalar_tensor_tensor(out=xs[k][:, :], in0=X[:, ti, :], scalar=st[k][:, 3:4],
                                                 in1=gbc[:, :], op0=ALU.mult, op1=ALU.mult),
         reads=[Xb[ti], st_b[k], gbc_b], writes=[xs_b[k]])
    pT = ps[pb][:, :].bitcast(BF16)
    for c in range(8):
        S.op("pe", lambda e, c=c: e.transpose(out=pT[:, c * 128:(c + 1) * 128], in_=xs[k][:, c * 128:(c + 1) * 128],
                                              identity=identb[:, :]),
             reads=[xs_b[k], C["cb"]], writes=[psb[pb]], signal=(c == 7))
    S.op("act", lambda e: e.activation(out=out_ap, in_=pT.rearrange("p (c t) -> p c t", c=8), func=AF.Copy),
         reads=[psb[pb]], writes=[out_b])


def even_mixer(S, nc, C, X, Xb, NT, P):
    ps, psb = C["ps"], C["psb"]
    ident, identb = C["ident"], C["identb"]
    T = NT * 128
    NQB = T // 512
    with ExitStack() as es:
        def sb(name, shape, dt):
            return es.enter_context(nc.sbuf_tensor(uname(name), shape, dt))
        yTa = sb("e_yTa", [128, 4, T], BF16); yT_b = [[S.buf("yT") for _ in range(NT)] for _ in range(8)]
        qT = sb("e_qT", [128, 4, T], BF16); qT_b = S.bufs("qT", NT)
        kT = sb("e_kT", [128, 4, T], BF16); kT_b = S.bufs("kT", NT)
        v = sb("e_v", [128, NT, 512], BF16); v_b = S.bufs("v", NT)
        woutv = P["w_out"].rearrange("(kc p) d -> p kc d", p=128)
        with ExitStack() as es1:
            def sb1(name, shape, dt):
                return es1.enter_context(nc.sbuf_tensor(uname(name), shape, dt))
            win = sb1("e_win", [128, 8, 2560], BF16); win_b = S.bufs("win", 5)
            winv = P["w_in"].rearrange("(kc p) f -> p kc f", p=128)
            for cb in range(5):
                S.dma("pool", win[:, :, cb * 512:(cb + 1) * 512], winv[:, :, cb * 512:(cb + 1) * 512], writes=[win_b[cb]])
            gbc = sb1("e_gbc", [128, 1024], F32); gbc_b = S.buf("gbc")
            lng = sb1("e_lng", [128, 512], F32); lng_b = S.buf("lng")
            lnb = sb1("e_lnb", [128, 512], F32); lnb_b = S.buf("lnb")
            qg = sb1("e_qg", [128, 64], F32); qg_b = S.buf("qg")
            kg = sb1("e_kg", [128, 64], F32); kg_b = S.buf("kg")
            sgb = sb1("e_sgb", [128, 8], F32); sgb_b = S.buf("sgb")
            sgw = sb1("e_sgw", [128, 8, 128], BF16); sgw_b = S.buf("sgw")
            wcT = sb1("e_wcT", [128, 8, 128], BF16); wcT_b = S.buf("wcT")
            S.dma("sp", gbc[:, :], P["g"].partition_broadcast(128), writes=[gbc_b])
            S.dma("sp", lng[:, :], P["ln_g"].partition_broadcast(128), writes=[lng_b])
            S.dma("sp", lnb[:, :], P["ln_b"].partition_broadcast(128), writes=[lnb_b])
            S.dma("sp", qg[:, :], P["qg"].partition_broadcast(128), writes=[qg_b])
            S.dma("sp", kg[:, :], P["kg"].partition_broadcast(128), writes=[kg_b])
            S.dma("sp", sgb[:, :], P["sg_b"].rearrange("g t -> t g"), writes=[sgb_b], allow_slow_non_contiguous=True)
            S.dma("pool", sgw[:, :, :], P["sg_w"].rearrange("g t s -> t g s"), writes=[sgw_b])
            S.op("dve", lambda e: e.tensor_scalar(out=qg[:, :], in0=qg[:, :], scalar1=0.125, scalar2=None, op0=ALU.mult),
                 reads=[qg_b], writes=[qg_b])
            S.op("pool", lambda e: e.affine_select(out=sgw[:, :, :], in_=sgw[:, :, :], pattern=[[0, 8], [-1, 128]],
                                                   compare_op=ALU.is_ge, fill=0.0, base=0, channel_multiplier=1),
                 reads=[sgw_b], writes=[sgw_b])
            for half in range(2):
                for gi in range(4):
                    g_ = half * 4 + gi
                    S.op("pe", lambda e: e.transpose(out=ps[half][:, :].bitcast(BF16)[:, gi * 128:(gi + 1) * 128], in_=sgw[:, g_, :],
                                                     identity=identb[:, :]),
                         reads=[sgw_b, C["cb"]], writes=[psb[half]], signal=(gi == 3))
                S.op("act", lambda e: e.activation(out=wcT[:, half * 4:(half + 1) * 4, :],
                                                   in_=ps[half][:, :].bitcast(BF16)[:, 0:512].rearrange("p (g t) -> p g t", g=4), func=AF.Copy),
                     reads=[psb[half]], writes=[wcT_b])
            junk = None; junk_b = None
            xs = [sb1("e_xs%d" % i, [128, 1024], BF16) for i in range(2)]; xs_b = S.bufs("xs", 2)
            st = [sb1("e_st%d" % i, [128, 4], F32) for i in range(2)]; st_b = S.bufs("st", 2)
            tmpn = (junk, junk_b, xs, xs_b, st, st_b)
            hTt = [sb1("e_hTt%d" % i, [128, 8, 128], BF16) for i in range(2)]; hTt_b = S.bufs("hTt", 2)
            u = [sb1("e_u%d" % i, [128, 512], BF16) for i in range(1)]; u_b = S.bufs("u", 1)
            vg = [sb1("e_vg%d" % i, [128, 512], F32) for i in range(1)]; vg_b = S.bufs("vg", 1)
            vgn = [sb1("e_vgn%d" % i, [128, 512], BF16) for i in range(1)]; vgn_b = S.bufs("vgn", 1)
            lst = [sb1("e_lst%d" % i, [128, 8], F32) for i in range(1)]; lst_b = S.bufs("lst", 1)
            ya = [sb1("e_ya%d" % i, [128, 512], F32) for i in range(1)]; ya_b = S.bufs("ya", 1)
            yab = [sb1("e_yab%d" % i, [128, 512], BF16) for i in range(1)]; yab_b = S.bufs("yab", 1)
            sq = [sb1("e_sq%d" % i, [128, 512], F32) for i in range(2)]; sq_b = S.bufs("sq", 2)
            qst = [sb1("e_qst%d" % i, [128, 32], F32) for i in range(2)]; qst_b = S.bufs("qst", 2)
            qn = [sb1("e_qn%d" % i, [128, 512], BF16) for i in range(2)]; qn_b = S.bufs("qn", 2)
            def apart(i, S):
                k = 0
                S.op("act", lambda e: e.activation(out=u[k][:, :], in_=ps[2][:, :], func=AF.Gelu_apprx_tanh),
                     reads=[psb[2]], writes=[u_b[k]])
                S.op("act", lambda e: e.activation(out=vg[k][:, :], in_=ps[3][:, :], func=AF.Gelu_apprx_tanh,
                                                   accum_out=lst[k][:, 0:1]),
                     reads=[psb[3]], writes=[vg_b[k], lst_b[k]])
                S.op("act", lambda e: e.activation(out=vgn[k][:, :], in_=vg[k][:, :], func=AF.Square,
                                                   accum_out=lst[k][:, 1:2]),
                     reads=[vg_b[k], lst_b[k]], writes=[vgn_b[k], lst_b[k]])
                L = lst[k]; Lb = lst_b[k]
                S.op("dve", lambda e: e.tensor_scalar(out=L[:, 2:4], in0=L[:, 0:2], scalar1=1.0 / 512, scalar2=None,
                                                      op0=ALU.mult), reads=[Lb], writes=[Lb])
                S.op("dve", lambda e: e.tensor_tensor(out=L[:, 4:5], in0=L[:, 2:3], in1=L[:, 2:3], op=ALU.mult),
                     reads=[Lb], writes=[Lb])
                S.op("dve", lambda e: e.scalar_tensor_tensor(out=L[:, 5:6], in0=L[:, 3:4], scalar=EPS, in1=L[:, 4:5],
                                                             op0=ALU.add, op1=ALU.subtract), reads=[Lb], writes=[Lb])
                S.op("act", lambda e: e.sqrt(out=L[:, 6:7], in_=L[:, 5:6]), reads=[Lb], writes=[Lb])
                S.op("dve", lambda e: e.reciprocal(out=L[:, 7:8], in_=L[:, 6:7]), reads=[Lb], writes=[Lb])
                S.op("dve", lambda e: e.scalar_tensor_tensor(out=L[:, 4:5], in0=L[:, 2:3], scalar=-1.0, in1=L[:, 7:8],
                                                             op0=ALU.mult, op1=ALU.mult), reads=[Lb], writes=[Lb])
                S.op("act", lambda e: e.activation(out=vg[k][:, :], in_=vg[k][:, :], func=AF.Identity,
                                                   bias=L[:, 4:5], scale=L[:, 7:8]),
                     reads=[vg_b[k], Lb], writes=[vg_b[k]])
                S.op("dve", lambda e: e.tensor_tensor(out=vg[k][:, :], in0=vg[k][:, :], in1=lng[:, :], op=ALU.mult),
                     reads=[vg_b[k], lng_b], writes=[vg_b[k]])
                S.op("dve", lambda e: e.tensor_tensor(out=vgn[k][:, :], in0=vg[k][:, :], in1=lnb[:, :], op=ALU.add),
                     reads=[vg_b[k], lnb_b], writes=[vgn_b[k]])
                for g_ in range(8):
                    S.op("pe", lambda e, g_=g_: e.matmul(ps[7][:, g_ * 64:(g_ + 1) * 64], wcT[:, g_, :],
                                                         vgn[k][:, g_ * 64:(g_ + 1) * 64], start=True, stop=True),
                         reads=[wcT_b, vgn_b[k]], writes=[psb[7]], signal=(g_ == 7))
                S.op("dve", lambda e: e.tensor_tensor(out=ya[k][:, :].rearrange("p (g c) -> p g c", g=8),
                                                      in0=ps[7][:, :].rearrange("p (g c) -> p g c", g=8),
                                                      in1=sgb[:, :].unsqueeze(2).to_broadcast([128, 8, 64]), op=ALU.add),
                     reads=[psb[7], sgb_b], writes=[ya_b[k]])
                S.op("dve", lambda e: e.tensor_tensor(out=yab[k][:, :], in0=ya[k][:, :], in1=u[k][:, :], op=ALU.mult),
                     reads=[ya_b[k], u_b[k]], writes=[yab_b[k]])
                pT = ps[7][:, :].bitcast(BF16)
                for c in range(4):
                    S.op("pe", lambda e, c=c: e.transpose(out=pT[:, c * 128:(c + 1) * 128], in_=yab[k][:, c * 128:(c + 1) * 128],
                                                          identity=identb[:, :]),
                         reads=[yab_b[k]], writes=[psb[7]], signal=(c == 3))
                S.op("act", lambda e: e.activation(out=yTa[:, 0:4, i * 128:(i + 1) * 128],
                                                   in_=pT[:, 0:512].rearrange("p (c t) -> p c t", c=4), func=AF.Copy),
                     reads=[psb[7]], writes=[yT_b[c][i] for c in range(4)])

            def bpart(i, which, S):
                bank, gt, gt_b, dstT, dstT_b = ((4, qg, qg_b, qT, qT_b), (5, kg, kg_b, kT, kT_b))[which]
                kk = which
                Q = qst[kk]; Qb = qst_b[kk]
                S.op("act", lambda e: e.activation(out=sq[kk][:, :], in_=ps[bank][:, :], func=AF.Square),
                     reads=[psb[bank]], writes=[sq_b[kk]])
                S.op("dve", lambda e: e.tensor_reduce(out=Q[:, 0:8], in_=sq[kk][:, :].rearrange("p (h d) -> p h d", h=8),
                                                      axis=AX.X, op=ALU.add), reads=[sq_b[kk]], writes=[Qb])
                rstd_ops(S, Q, Qb, 8, 1.0 / 64)
                S.op("dve", lambda e: e.tensor_tensor(out=sq[kk][:, :].rearrange("p (h d) -> p h d", h=8),
                                                      in0=ps[bank][:, :].rearrange("p (h d) -> p h d", h=8),
                                                      in1=Q[:, 24:32].unsqueeze(2).to_broadcast([128, 8, 64]), op=ALU.mult),
                     reads=[psb[bank], Qb, sq_b[kk]], writes=[sq_b[kk]])
                S.op("dve", lambda e: e.tensor_tensor(out=qn[kk][:, :].rearrange("p (h d) -> p h d", h=8),
                                                      in0=sq[kk][:, :].rearrange("p (h d) -> p h d", h=8),
                                                      in1=gt[:, :].unsqueeze(1).to_broadcast([128, 8, 64]), op=ALU.mult),
                     reads=[sq_b[kk], gt_b], writes=[qn_b[kk]])
                pT2 = ps[bank][:, :].bitcast(BF16)
                for c in range(4):
                    S.op("pe", lambda e, c=c: e.transpose(out=pT2[:, c * 128:(c + 1) * 128], in_=qn[kk][:, c * 128:(c + 1) * 128],
                                                          identity=identb[:, :]),
                         reads=[qn_b[kk]], writes=[psb[bank]], signal=(c == 3))
                S.op("act", lambda e: e.activation(out=dstT[:, :, i * 128:(i + 1) * 128],
                                                   in_=pT2[:, 0:512].rearrange("p (c t) -> p c t", c=4), func=AF.Copy),
                     reads=[psb[bank]], writes=[dstT_b[i]])

            def vpart(i, S):
                S.op("act", lambda e: e.activation(out=v[:, i, :], in_=ps[6][:, :], func=AF.Copy),
                     reads=[psb[6]], writes=[v_b[i]])

            def normpart(i, S):
                kn = i % 2
                norm_transpose(S, C, X, Xb, i, gbc, gbc_b, tmpn, kn, i % 2, hTt[kn][:, :, :], hTt_b[kn])

            normpart(0, S)
            for i in range(NT):
                kn = i % 2
                for cb in range(5):
                    for kc in range(8):
                        S.op("pe", lambda e: e.matmul(ps[2 + cb][:, :], hTt[kn][:, kc, :], win[:, kc, cb * 512:(cb + 1) * 512],
                                                      start=(kc == 0), stop=(kc == 7)),
                             reads=[hTt_b[kn], win_b[cb]], writes=[psb[2 + cb]], signal=(kc == 7))
                recs = [Recorder() for _ in range(5)]
                apart(i, recs[0])
                bpart(i, 0, recs[1])
                bpart(i, 1, recs[2])
                vpart(i, recs[3])
                if i + 1 < NT:
                    normpart(i + 1, recs[4])
                interleave(S, recs)
            S.fence()
        with ExitStack() as es2:
            def sb2(name, shape, dt):
                return es2.enter_context(nc.sbuf_tensor(uname(name), shape, dt))
            yTb = sb2("e_yTb", [128, 4, T], BF16)
            wout = sb2("e_wout", [128, 8, 1024], BF16); wout_b = S.bufs("wout", 2)
            for dh in range(2):
                S.dma("pool", wout[:, :, dh * 512:(dh + 1) * 512], woutv[:, :, dh * 512:(dh + 1) * 512], writes=[wout_b[dh]])
            e_sb = [sb2("a_e%d" % i, [128, 512], BF16) for i in range(4)]; e_b = S.bufs("e", 4)
            l_sb = [sb2("a_l%d" % i, [128, 512], BF16) for i in range(4)]; l_b = S.bufs("l", 4)
            x_sb = [sb2("a_x%d" % i, [128, 512], BF16) for i in range(4)]; x_b = S.bufs("x", 4)
            a_sb = [sb2("a_a%d" % i, [128, 512], BF16) for i in range(4)]; a_b = S.bufs("a", 4)
            negtri, negcmp, masklt = C["negtri"], C["negcmp"], C["masklt"]
            cbuf = C["cb"]
            LA = 3
            qz = [sb2("a_qz%d" % i, [128, 2, 512], BF16) for i in range(2)]; qz_b = S.bufs("qz", 2)
            for i in range(2):
                S.op("pool", lambda e: e.memset(qz[i][:, :, :], 0.0), writes=[qz_b[i]])
            pcount = 0
            for qb in range(NQB):
                for hp in range(4):
                    pq = pcount % 2; pcount += 1
                    obase = 4 + 2 * pq
                    S.op("dve", lambda e: e.tensor_copy(out=qz[pq][0:64, 0, :], in_=qT[0:64, hp, qb * 512:(qb + 1) * 512]),
                         reads=qT_b[4 * qb:4 * qb + 4], writes=[qz_b[pq]])
                    S.op("dve", lambda e: e.tensor_copy(out=qz[pq][64:128, 1, :], in_=qT[64:128, hp, qb * 512:(qb + 1) * 512]),
                         reads=qT_b[4 * qb:4 * qb + 4], writes=[qz_b[pq]])
                    items = [(j, hh) for j in range(4 * qb + 3, -1, -1) for hh in range(2)]
                    jfirst = 4 * qb + 3

                    n = len(items)

                    def geo(i):
                        j, hh = items[i]
                        return j, hh, i % 4, max(0, (j - 4 * qb)) * 128

                    def pe_z(i):
                        j, hh, par, c0 = geo(i)
                        zb = i % 2
                        S.op("pe", lambda e: e.matmul(ps[zb][:, c0:512], kT[:, hp, j * 128:(j + 1) * 128],
                                                      qz[pq][:, hh, c0:512], start=True, stop=True),
                             reads=[kT_b[j], qz_b[pq]], writes=[psb[zb]])

                    def pe_tri(i):
                        j, hh, par, c0 = geo(i)
                        S.op("pe", lambda e: e.matmul(ps[2 + hh][:, c0:512], negtri[:, :], l_sb[par][:, c0:512],
                                                      start=(j == jfirst), stop=False, skip_group_check=True),
                             reads=[l_b[par], cbuf], writes=[psb[2 + hh]])

                    def pe_cmp_av(i):
                        j, hh, par, c0 = geo(i)
                        if j > 0:
                            S.op("pe", lambda e: e.matmul(ps[2 + hh][:, c0:512], negcmp[:, :], l_sb[par][:, c0:512],
                                                          start=False, stop=False, skip_group_check=True),
                                 reads=[l_b[par], cbuf], writes=[psb[2 + hh]])
                        S.op("pe", lambda e: e.matmul(ps[obase + hh][:, c0:512], v[:, j, hp * 128:(hp + 1) * 128],
                                                      a_sb[par][:, c0:512], start=(j == jfirst), stop=(j == 0),
                                                      skip_group_check=True),
                             reads=[v_b[j], a_b[par]], writes=[psb[obase + hh]])

                    def act_a(i):
                        j, hh, par, c0 = geo(i)
                        zb = i % 2
                        S.op("act", lambda e: e.activation(out=e_sb[par][:, c0:512], in_=ps[zb][:, c0:512], func=AF.Exp),
                             reads=[psb[zb]], writes=[e_b[par]])
                        if j >= 4 * qb:
                            S.op("dve", lambda e: e.tensor_tensor(out=e_sb[par][:, c0:c0 + 128], in0=e_sb[par][:, c0:c0 + 128],
                                                                  in1=masklt[:, :], op=ALU.mult),
                                 reads=[e_b[par], cbuf], writes=[e_b[par]])
                        S.op("act", lambda e: e.activation(out=l_sb[par][:, c0:512], in_=e_sb[par][:, c0:512], func=AF.Ln,
                                                           bias=1.0),
                             reads=[e_b[par]], writes=[l_b[par]])

                    def act_b(i):
                        j, hh, par, c0 = geo(i)
                        S.op("act", lambda e: e.activation(out=x_sb[par][:, c0:512], in_=ps[2 + hh][:, c0:512], func=AF.Exp),
                             reads=[psb[2 + hh]], writes=[x_b[par]])
                        S.op("dve", lambda e: e.tensor_tensor(out=a_sb[par][:, c0:512], in0=e_sb[par][:, c0:512],
                                                              in1=x_sb[par][:, c0:512], op=ALU.mult),
                             reads=[e_b[par], x_b[par]], writes=[a_b[par]])

                    for i in range(-1, n + 2):
                        if 0 <= i + 1 < n:
                            pe_z(i + 1)
                        if 0 <= i - 1 < n:
                            pe_tri(i - 1)
                        if 0 <= i - 2 < n:
                            pe_cmp_av(i - 2)
                        if 0 <= i < n:
                            act_a(i)
                        if 0 <= i - 1 < n:
                            act_b(i - 1)
                    for hh in range(2):
                        S.op("dve", lambda e: e.tensor_copy(out=yTb[hh * 64:(hh + 1) * 64, hp, qb * 512:(qb + 1) * 512],
                                                            in_=ps[obase + hh][hh * 64:(hh + 1) * 64, :]),
                             reads=[psb[obase + hh]], writes=[yT_b[4 + hp][4 * qb + t] for t in range(4)])
            cnt = 0
            for i in range(NT):
                for dh in range(2):
                    pb = cnt % 4; cnt += 1
                    for kc in range(8):
                        lhs = yTa[:, kc, i * 128:(i + 1) * 128] if kc < 4 else yTb[:, kc - 4, i * 128:(i + 1) * 128]
                        S.op("pe", lambda e: e.matmul(ps[pb][:, :], lhs,
                                                      wout[:, kc, dh * 512:(dh + 1) * 512], start=(kc == 0), stop=(kc == 7)),
                             reads=[yT_b[kc][i], wout_b[dh]], writes=[psb[pb]], signal=(kc == 7))
                    S.op("dve", lambda e: e.tensor_tensor(out=X[:, i, dh * 512:(dh + 1) * 512], in0=ps[pb][:, :],
                                                          in1=X[:, i, dh * 512:(dh + 1) * 512], op=ALU.add),
                         reads=[psb[pb], Xb[i]], writes=[Xb[i]])
            S.fence()


def make_consts(S, nc, es, C):
    ident = es.enter_context(nc.sbuf_tensor("ident", [128, 128], F32))
    identb = es.enter_context(nc.sbuf_tensor("identb", [128, 128], BF16))
    tmp = es.enter_context(nc.sbuf_tensor("ctmp", [128, 128], F32))
    negtri = es.enter_context(nc.sbuf_tensor("negtri", [128, 128], BF16))
    negcmp = es.enter_context(nc.sbuf_tensor("negcmp", [128, 128], BF16))
    masklt = es.enter_context(nc.sbuf_tensor("masklt", [128, 128], BF16))
    maskle = es.enter_context(nc.sbuf_tensor("maskle", [128, 128], BF16))
    onesf = es.enter_context(nc.sbuf_tensor("onesf", [128, 128], F32))
    zerosf = es.enter_context(nc.sbuf_tensor("zerosf", [128, 128], F32))
    cb = S.buf("consts")
    S.op("pool", lambda e: e.memset(onesf[:, :], 1.0), writes=[cb])
    S.op("pool", lambda e: e.memset(zerosf[:, :], 0.0), writes=[cb])
    C.update(onesf=onesf, zerosf=zerosf, psb4a=S.buf("ps4a", persist=True), psb4b=S.buf("ps4b", persist=True), psb4c=S.buf("ps4c", persist=True))
    C.update(ident=ident, identb=identb, negtri=negtri, negcmp=negcmp, masklt=masklt, maskle=maskle, cb=cb)

    def sel(out, val, pattern, cm, op, base=0):
        S.op("pool", lambda e: e.memset(tmp[:, :], val), reads=[cb], writes=[cb])
        S.op("pool", lambda e: e.affine_select(out=tmp[:, :], in_=tmp[:, :], pattern=pattern, compare_op=op, fill=0.0,
                                               base=base, channel_multiplier=cm), reads=[cb], writes=[cb])
        S.op("pool", lambda e: e.tensor_copy(out=out[:, :], in_=tmp[:, :]), reads=[cb], writes=[cb])
    sel(ident, 1.0, [[-1, 128]], 1, ALU.is_equal)
    sel(identb, 1.0, [[-1, 128]], 1, ALU.is_equal)
    sel(negtri, -1.0, [[-1, 128]], 1, ALU.is_ge)
    sel(negcmp, -1.0, [[1, 128]], -1, ALU.is_gt)
    sel(masklt, 1.0, [[1, 128]], -1, ALU.is_gt)
    sel(maskle, 1.0, [[1, 128]], -1, ALU.is_ge)


def odd_mixer(S, nc, C, X, Xb, NT, P, gs_dram):
    ps, psb = C["ps"], C["psb"]
    ident, identb, onesf, zerosf, maskle, cbuf = C["ident"], C["identb"], C["onesf"], C["zerosf"], C["maskle"], C["cb"]
    T = NT * 128
    NQB = T // 512
    NP = 4 * NT
    KS = 128 ** -0.5
    with ExitStack() as es:
        def sb(name, shape, dt):
            return es.enter_context(nc.sbuf_tensor(uname(name), shape, dt))
        hT = sb("o_hT", [128, 8, T], BF16); hT_b = S.bufs("hT", NT)
        wout = sb("o_wout", [128, 8, 1024], BF16); wout_b = S.bufs("wout", 2)
        woutv = P["w_out"].rearrange("(kc p) d -> p kc d", p=128)
        winv = P["w_in"].rearrange("(kc p) f -> p kc f", p=128)
        ongb = sb("o_ong", [128, 1024], F32); ong_b = S.buf("ong")
        cw = sb("o_cw", [128, 8, 4], F32); cw_b = S.buf("cw")
        cbias = sb("o_cb", [128, 8], F32); cbias_b = S.buf("cbias")
        TS = sb("o_TS", [128, 4 * NP], F32); TS_b = S.buf("TS")
        DEC = sb("o_DEC", [128, NP], F32); DEC_b = S.buf("DEC")
        wg = sb("o_wg", [128, 8, 8], BF16); wg_b = S.buf("wg")
        S.dma("pool", wg[:, :, :], winv[:, :, 3072:3080], writes=[wg_b])
        for dh in range(2):
            S.dma("pool", wout[:, :, dh * 512:(dh + 1) * 512], woutv[:, :, dh * 512:(dh + 1) * 512], writes=[wout_b[dh]])
        S.dma("sp", ongb[:, :], P["on_g"].partition_broadcast(128), writes=[ong_b])
        for k_ in range(4):
            S.dma("sp", cw[:, :, k_], P["conv_w"][k_].rearrange("(i p) -> p i", p=128), writes=[cw_b], allow_slow_non_contiguous=True)
        S.dma("sp", cbias[:, :], P["conv_b"].rearrange("(i p) -> p i", p=128), writes=[cbias_b], allow_slow_non_contiguous=True)
        with ExitStack() as es1:
            def sb1(name, shape, dt):
                return es1.enter_context(nc.sbuf_tensor(uname(name), shape, dt))
            gbc = sb1("o_gbc", [128, 1024], F32); gbc_b = S.buf("gbc")
            S.dma("sp", gbc[:, :], P["g"].partition_broadcast(128), writes=[gbc_b])
            junk = sb1("o_junk", [128, 1024], BF16); junk_b = S.buf("junk")
            xs = [sb1("o_xs%d" % i, [128, 1024], BF16) for i in range(2)]; xs_b = S.bufs("xs", 2)
            st = [sb1("o_st%d" % i, [128, 4], F32) for i in range(2)]; st_b = S.bufs("st", 2)
            tmpn = (junk, junk_b, xs, xs_b, st, st_b)
            for i in range(NT):
                norm_transpose(S, C, X, Xb, i, gbc, gbc_b, tmpn, i % 2, i % 2, hT[:, :, i * 128:(i + 1) * 128], hT_b[i])
            gsb = sb1("o_gsb", [8, T], F32); gsb_b = S.buf("gsb")
            for blk in range(NQB):
                pb = 2 + blk % 2
                for kc in range(8):
                    S.op("pe", lambda e: e.matmul(ps[pb][0:8, :], wg[:, kc, :], hT[:, kc, blk * 512:(blk + 1) * 512],
                                                  start=(kc == 0), stop=(kc == 7)),
                         reads=[wg_b] + hT_b[blk * 4:(blk + 1) * 4], writes=[psb[pb]], signal=(kc == 7))
                S.op("act", lambda e: e.activation(out=gsb[:, blk * 512:(blk + 1) * 512], in_=ps[pb][0:8, :], func=AF.Copy),
                     reads=[psb[pb]], writes=[gsb_b])
            gsd_b = S.buf("gsd")
            S.dma("sp", gs_dram[:, :], gsb[:, :], reads=[gsb_b], writes=[gsd_b])
            G = {}
            for nm in ("I", "F", "e1", "sp", "bneg", "a", "cm", "Mx", "rowfac", "inter", "wkx", "en"):
                G[nm] = sb1("o_G" + nm, [NP, 128], F32)
            Gb = S.buf("G")
            S.dma("sp", G["I"][:, :], gs_dram[0:4, :].rearrange("h (c t) -> (h c) t", t=128), reads=[gsd_b], writes=[Gb])
            Fb = S.buf("GF")
            S.dma("sp", G["F"][:, :], gs_dram[4:8, :].rearrange("h (c t) -> (h c) t", t=128), reads=[gsd_b], writes=[Fb])
            bias = sb1("o_bias", [NP, 4], F32); bias_b = [S.buf("bias%d" % i) for i in range(8)]
            for h in range(4):
                S.dma("sp", bias[h * NT:(h + 1) * NT, 0:1], P["i_b"][h:h + 1].partition_broadcast(NT), writes=[bias_b[h]])
                S.dma("sp", bias[h * NT:(h + 1) * NT, 1:2], P["f_b"][h:h + 1].partition_broadcast(NT), writes=[bias_b[4 + h]])
            Kc = sb1("o_K", [NP, 4], F32)
            R = sb1("o_R", [1, 8 * NP], F32)
            allg = [Gb, Fb] + bias_b
            def gop(eng, fn):
                S.op(eng, fn, reads=allg + [cbuf], writes=[Gb])
            gop("dve", lambda e: e.tensor_scalar(out=bias[:, 2:3], in0=bias[:, 1:2], scalar1=-1.0, scalar2=None, op0=ALU.mult))
            gop("act", lambda e: e.activation(out=G["e1"][:, :], in_=G["F"][:, :], func=AF.Exp, bias=bias[:, 2:3], scale=-1.0))
            gop("act", lambda e: e.activation(out=G["sp"][:, :], in_=G["e1"][:, :], func=AF.Ln, bias=1.0))
            gop("dve", lambda e: e.tensor_tensor_scan(out=G["bneg"][:, :], data0=onesf[0:NP, :], data1=G["sp"][:, :],
                                                      initial=0.0, op0=ALU.mult, op1=ALU.add))
            gop("dve", lambda e: e.scalar_tensor_tensor(out=G["a"][:, :], in0=G["I"][:, :], scalar=bias[:, 0:1],
                                                        in1=G["bneg"][:, :], op0=ALU.add, op1=ALU.add))
            gop("dve", lambda e: e.tensor_tensor_scan(out=G["cm"][:, :], data0=zerosf[0:NP, :], data1=G["a"][:, :],
                                                      initial=-1e30, op0=ALU.add, op1=ALU.max))
            pr = 4
            S.op("pe", lambda e: e.transpose(out=ps[pr][0:1, 0:NP], in_=G["bneg"][:, 127:128], identity=ident[0:NP, 0:NP]),
                 reads=[Gb, cbuf], writes=[psb[pr]], signal=False)
            S.op("pe", lambda e: e.transpose(out=ps[pr][0:1, NP:2 * NP], in_=G["cm"][:, 127:128], identity=ident[0:NP, 0:NP]),
                 reads=[Gb, cbuf], writes=[psb[pr]])
            def rop(eng, fn, extra=()):
                S.op(eng, fn, reads=[Gb, cbuf] + list(extra), writes=[Gb])
            rop("act", lambda e: e.activation(out=R[:, 0:2 * NP], in_=ps[pr][0:1, 0:2 * NP], func=AF.Copy), [psb[pr]])
            rop("dve", lambda e: e.tensor_scalar(out=R[:, 2 * NP:3 * NP], in0=R[:, 0:NP], scalar1=-1.0, scalar2=None, op0=ALU.mult))
            for h in range(4):
                rop("dve", lambda e: e.tensor_tensor_scan(out=R[:, 3 * NP + h * NT:3 * NP + (h + 1) * NT],
                                                          data0=R[:, NP + h * NT:NP + (h + 1) * NT],
                                                          data1=R[:, 2 * NP + h * NT:2 * NP + (h + 1) * NT],
                                                          initial=0.0, op0=ALU.max, op1=ALU.add))
            rop("dve", lambda e: e.memset(R[:, 4 * NP:5 * NP], 0.0))
            rop("dve", lambda e: e.tensor_copy(out=R[:, 4 * NP:5 * NP].rearrange("o (h c) -> o h c", h=4)[:, :, 1:NT],
                                               in_=R[:, 3 * NP:4 * NP].rearrange("o (h c) -> o h c", h=4)[:, :, 0:NT - 1]))
            rop("dve", lambda e: e.tensor_tensor(out=R[:, 5 * NP:6 * NP], in0=R[:, 4 * NP:5 * NP], in1=R[:, NP:2 * NP], op=ALU.max))
            rop("dve", lambda e: e.tensor_tensor(out=R[:, 6 * NP:7 * NP], in0=R[:, 4 * NP:5 * NP], in1=R[:, 5 * NP:6 * NP],
                                                 op=ALU.subtract))
            rop("act", lambda e: e.activation(out=R[:, 7 * NP:8 * NP], in_=R[:, 6 * NP:7 * NP], func=AF.Exp))
            pk = 5
            S.op("pe", lambda e: e.matmul(ps[pk][0:NP, 0:1], R[0:1, 4 * NP:5 * NP], onesf[0:1, 0:1], start=True, stop=True),
                 reads=[Gb, cbuf], writes=[psb[pk]], signal=False)
            S.op("pe", lambda e: e.matmul(ps[pk][0:NP, 1:2], R[0:1, 5 * NP:6 * NP], onesf[0:1, 0:1], start=True, stop=True),
                 reads=[Gb, cbuf], writes=[psb[pk]])
            rop("act", lambda e: e.activation(out=Kc[:, 0:2], in_=ps[pk][0:NP, 0:2], func=AF.Copy), [psb[pk]])
            rop("dve", lambda e: e.tensor_scalar(out=Kc[:, 2:3], in0=Kc[:, 1:2], scalar1=-1.0, scalar2=None, op0=ALU.mult))
            rop("dve", lambda e: e.tensor_scalar(out=G["Mx"][:, :], in0=G["cm"][:, :], scalar1=Kc[:, 0:1], scalar2=None, op0=ALU.max))
            rop("act", lambda e: e.activation(out=G["rowfac"][:, :], in_=G["Mx"][:, :], func=AF.Exp, bias=Kc[:, 1:2], scale=-1.0))
            rop("act", lambda e: e.activation(out=G["inter"][:, :], in_=G["Mx"][:, :], func=AF.Exp, bias=Kc[:, 0:1], scale=-1.0))
            rop("act", lambda e: e.activation(out=G["wkx"][:, :], in_=G["a"][:, :], func=AF.Exp, bias=Kc[:, 2:3], scale=1.0))
            rop("dve", lambda e: e.tensor_tensor(out=G["e1"][:, :], in0=G["bneg"][:, :], in1=G["Mx"][:, :], op=ALU.subtract))
            rop("act", lambda e: e.activation(out=G["en"][:, :], in_=G["e1"][:, :], func=AF.Exp))
            pt = 6
            for qi, nm in enumerate(("rowfac", "inter", "wkx", "en")):
                S.op("pe", lambda e: e.transpose(out=ps[pt][:, qi * NP:(qi + 1) * NP], in_=G[nm][:, :], identity=ident[0:NP, 0:NP]),
                     reads=[Gb, cbuf], writes=[psb[pt]], signal=(qi == 3))
            S.op("act", lambda e: e.activation(out=TS[:, :], in_=ps[pt][:, 0:4 * NP], func=AF.Copy), reads=[psb[pt]], writes=[TS_b])
            pd = 7
            S.op("pe", lambda e: e.matmul(ps[pd][:, 0:NP], onesf[0:1, :], R[0:1, 7 * NP:8 * NP], start=True, stop=True),
                 reads=[Gb, cbuf], writes=[psb[pd]])
            S.op("act", lambda e: e.activation(out=DEC[:, :], in_=ps[pd][:, 0:NP], func=AF.Copy), reads=[psb[pd]], writes=[DEC_b])
            S.fence()
        whd = sb("o_whd", [128, 8, 768], BF16); whd_b = S.bufs("whd", 4)
        pre = sb("o_pre", [128, 2, 3 + T], F32); pre_b = S.bufs("pre", 2)
        acc = sb("o_acc", [128, T], F32); acc_b = S.buf("acc")
        qkT = sb("o_qkT", [128, 2, T], BF16); qkT_b = S.bufs("qkT", 2)
        vone = sb("o_vone", [128, NT, 257], BF16); vone_b = S.bufs("vone", NT)
        sog = sb("o_sog", [128, NT, 256], BF16); sog_b = S.bufs("sog", NT)
        C32 = sb("o_C32", [128, 257], F32); C32_b = S.buf("C32")
        Cx = sb("o_Cx", [128, 257], BF16); Cx_b = S.buf("Cx")
        vext = [sb("o_vext%d" % i, [128, 257], BF16) for i in range(2)]; vext_b = S.bufs("vext", 2)
        sm = [sb("o_sm%d" % i, [128, 128], BF16) for i in range(2)]; sm_b = S.bufs("sm", 2)
        ktok = [sb("o_ktok%d" % i, [128, 128], BF16) for i in range(2)]; ktok_b = S.bufs("ktok", 2)
        o1 = [sb("o_o1%d" % i, [128, 257], F32) for i in range(2)]; o1_b = S.bufs("o1", 2)
        o2 = [sb("o_o2%d" % i, [128, 257], F32) for i in range(2)]; o2_b = S.bufs("o2", 2)
        sc = [sb("o_sc%d" % i, [128, 8], F32) for i in range(2)]; sc_b = S.bufs("sc", 2)
        jk = sb("o_jk", [128, 256], BF16); jk_b = S.buf("jk")
        yf = [sb("o_yf%d" % i, [128, 256], F32) for i in range(2)]; yf_b = S.bufs("yf", 2)
        yb = [sb("o_yb%d" % i, [128, 256], BF16) for i in range(2)]; yb_b = S.bufs("yb", 2)
        yTh = [sb("o_yTh%d" % i, [128, 2, 128], BF16) for i in range(2)]; yTh_b = S.bufs("yTh", 2)
        S.op("dve", lambda e: e.memset(pre[:, :, 0:3], 0.0), writes=pre_b)
        S.op("pool", lambda e: e.memset(vone[:, :, 256:257], 1.0), writes=vone_b)
        for h in range(4):
            srcs = ((h * 128, 128), (512 + h * 128, 128), (1024 + h * 256, 256), (2048 + h * 256, 256))
            off = 0
            for si, (c0, n) in enumerate(srcs):
                S.dma("pool", whd[:, :, off:off + n], winv[:, :, c0:c0 + n], writes=[whd_b[si]])
                off += n
            cnt = 0
            for w in range(2):
                for blk in range(NQB):
                    pb = cnt % 2; cnt += 1
                    for kc in range(8):
                        S.op("pe", lambda e: e.matmul(ps[pb][:, :], whd[:, kc, w * 128:(w + 1) * 128],
                                                      hT[:, kc, blk * 512:(blk + 1) * 512], start=(kc == 0), stop=(kc == 7)),
                             reads=[whd_b[w]] + hT_b[blk * 4:(blk + 1) * 4], writes=[psb[pb]], signal=(kc == 7))
                    S.op("act", lambda e: e.activation(out=pre[:, w, 3 + blk * 512:3 + (blk + 1) * 512], in_=ps[pb][:, :],
                                                       func=AF.Copy), reads=[psb[pb]], writes=[pre_b[w]])
                idx = w * 4 + h
                S.op("dve", lambda e: e.tensor_scalar(out=acc[:, :], in0=pre[:, w, 0:T], scalar1=cw[:, idx, 0:1], scalar2=None,
                                                      op0=ALU.mult), reads=[pre_b[w], cw_b], writes=[acc_b])
                for k in range(1, 4):
                    S.op("dve", lambda e: e.scalar_tensor_tensor(out=acc[:, :], in0=pre[:, w, k:k + T], scalar=cw[:, idx, k:k + 1],
                                                                 in1=acc[:, :], op0=ALU.mult, op1=ALU.add),
                         reads=[pre_b[w], cw_b, acc_b], writes=[acc_b])
                if w == 0:
                    S.op("act", lambda e: e.activation(out=qkT[:, 0, :], in_=acc[:, :], func=AF.Silu, bias=cbias[:, idx:idx + 1]),
                         reads=[acc_b, cbias_b], writes=[qkT_b[0]])
                else:
                    S.op("act", lambda e: e.activation(out=acc[:, :], in_=acc[:, :], func=AF.Silu, bias=cbias[:, idx:idx + 1]),
                         reads=[acc_b, cbias_b], writes=[acc_b])
                    S.op("dve", lambda e: e.tensor_scalar(out=qkT[:, 1, :], in0=acc[:, :], scalar1=KS, scalar2=None, op0=ALU.mult),
                         reads=[acc_b], writes=[qkT_b[1]])
            for i in range(NT):
                pb = 2 + i % 2
                for kc in range(8):
                    S.op("pe", lambda e: e.matmul(ps[pb][:, :], hT[:, kc, i * 128:(i + 1) * 128], whd[:, kc, 256:768],
                                                  start=(kc == 0), stop=(kc == 7)),
                         reads=[hT_b[i], whd_b[2], whd_b[3]], writes=[psb[pb]], signal=(kc == 7))
                S.op("act", lambda e: e.activation(out=vone[:, i, 0:256], in_=ps[pb][:, 0:256], func=AF.Copy),
                     reads=[psb[pb]], writes=[vone_b[i]])
                S.op("act", lambda e: e.activation(out=sog[:, i, :], in_=ps[pb][:, 256:512], func=AF.Sigmoid),
                     reads=[psb[pb]], writes=[sog_b[i]])
            S.op("dve", lambda e: e.memset(C32[:, :], 0.0), writes=[C32_b])
            S.op("pool", lambda e: e.memset(Cx[:, :], 0.0), writes=[Cx_b])
            psb4a = psb4b = psb[4]

            def stageF(c, S=S):
                p = h * NT + c
                k2 = c % 2
                cs = slice(c * 128, (c + 1) * 128)
                S.op("dve", lambda e: e.tensor_scalar(out=vext[k2][:, :], in0=vone[:, c, :], scalar1=TS[:, 2 * NP + p:2 * NP + p + 1],
                                                      scalar2=None, op0=ALU.mult), reads=[vone_b[c], TS_b], writes=[vext_b[k2]])
                S.op("pe", lambda e: e.matmul(ps[4][:, 0:128], qkT[:, 1, cs], qkT[:, 0, cs], start=True, stop=True),
                     reads=[qkT_b[0], qkT_b[1]], writes=[psb4a])
                S.op("dve", lambda e: e.tensor_tensor(out=sm[k2][:, :], in0=ps[4][:, 0:128], in1=maskle[:, :], op=ALU.mult),
                     reads=[psb4a, cbuf], writes=[sm_b[k2]])
                S.op("pe", lambda e: e.matmul(ps[5][:, 0:257], sm[k2][:, :], vext[k2][:, :], start=True, stop=True),
                     reads=[sm_b[k2], vext_b[k2]], writes=[psb[5]])
                S.op("act", lambda e: e.activation(out=o1[k2][:, :], in_=ps[5][:, 0:257], func=AF.Identity,
                                                   scale=TS[:, p:p + 1]), reads=[psb[5], TS_b], writes=[o1_b[k2]])
                pTk = ps[7][:, :].bitcast(BF16)
                S.op("pe", lambda e: e.transpose(out=pTk[:, k2 * 128:(k2 + 1) * 128], in_=qkT[:, 1, cs], identity=identb[:, :]),
                     reads=[qkT_b[1], cbuf], writes=[psb[7]])
                S.op("act", lambda e: e.activation(out=ktok[k2][:, :], in_=pTk[:, k2 * 128:(k2 + 1) * 128], func=AF.Copy),
                     reads=[psb[7]], writes=[ktok_b[k2]])
                S.op("pe", lambda e: e.matmul(ps[2 + k2][:, 0:257], ktok[k2][:, :], vext[k2][:, :], start=True, stop=True),
                     reads=[ktok_b[k2], vext_b[k2]], writes=[psb[2 + k2]])

            def stageG(c, S=S):
                p = h * NT + c
                k2 = c % 2
                cs = slice(c * 128, (c + 1) * 128)
                S.op("pe", lambda e: e.matmul(ps[6][:, 0:257], qkT[:, 0, cs], Cx[:, :], start=True, stop=True),
                     reads=[qkT_b[0], Cx_b], writes=[psb[6]])
                S.op("dve", lambda e: e.scalar_tensor_tensor(out=o2[k2][:, :], in0=ps[6][:, 0:257], scalar=TS[:, NP + p:NP + p + 1],
                                                             in1=o1[k2][:, :], op0=ALU.mult, op1=ALU.add),
                     reads=[psb[6], TS_b, o1_b[k2]], writes=[o2_b[k2]])
                S.op("dve", lambda e: e.scalar_tensor_tensor(out=C32[:, :], in0=C32[:, :], scalar=DEC[:, p:p + 1],
                                                             in1=ps[2 + k2][:, 0:257], op0=ALU.mult, op1=ALU.add),
                     reads=[C32_b, DEC_b, psb[2 + k2]], writes=[C32_b])
                S.op("act", lambda e: e.activation(out=Cx[:, :], in_=C32[:, :], func=AF.Copy), reads=[C32_b], writes=[Cx_b])

            def stageH(c, S=S):
                p = h * NT + c
                k2 = c % 2
                s_ = sc[k2]; s_b = sc_b[k2]
                S.op("dve", lambda e: e.tensor_scalar(out=s_[:, 7:8], in0=o2[k2][:, 256:257], scalar1=-1.0, scalar2=None,
                                                      op0=ALU.mult), reads=[o2_b[k2]], writes=[s_b])
                S.op("dve", lambda e: e.tensor_tensor(out=s_[:, 0:1], in0=o2[k2][:, 256:257], in1=s_[:, 7:8], op=ALU.max),
                     reads=[o2_b[k2], s_b], writes=[s_b])
                S.op("dve", lambda e: e.tensor_tensor(out=s_[:, 0:1], in0=s_[:, 0:1], in1=TS[:, 3 * NP + p:3 * NP + p + 1],
                                                      op=ALU.max), reads=[s_b, TS_b], writes=[s_b])
                S.op("dve", lambda e: e.reciprocal(out=s_[:, 1:2], in_=s_[:, 0:1]), reads=[s_b], writes=[s_b])
                S.op("act", lambda e: e.activation(out=jk[:, :], in_=o2[k2][:, 0:256], func=AF.Square, scale=s_[:, 1:2],
                                                   accum_out=s_[:, 2:3]), reads=[o2_b[k2], s_b], writes=[jk_b, s_b])
                S.op("dve", lambda e: e.tensor_scalar(out=s_[:, 3:4], in0=s_[:, 2:3], scalar1=1.0 / 256, scalar2=EPS,
                                                      op0=ALU.mult, op1=ALU.add), reads=[s_b], writes=[s_b])
                S.op("act", lambda e: e.sqrt(out=s_[:, 4:5], in_=s_[:, 3:4]), reads=[s_b], writes=[s_b])
                S.op("dve", lambda e: e.reciprocal(out=s_[:, 5:6], in_=s_[:, 4:5]), reads=[s_b], writes=[s_b])
                S.op("dve", lambda e: e.tensor_tensor(out=s_[:, 6:7], in0=s_[:, 5:6], in1=s_[:, 1:2], op=ALU.mult),
                     reads=[s_b], writes=[s_b])
                S.op("dve", lambda e: e.scalar_tensor_tensor(out=yf[k2][:, :], in0=o2[k2][:, 0:256], scalar=s_[:, 6:7],
                                                             in1=ongb[:, h * 256:(h + 1) * 256], op0=ALU.mult, op1=ALU.mult),
                     reads=[o2_b[k2], s_b, ong_b], writes=[yf_b[k2]])
                S.op("pool", lambda e: e.tensor_tensor(out=yb[k2][:, :], in0=yf[k2][:, :], in1=sog[:, c, :], op=ALU.mult),
                     reads=[yf_b[k2], sog_b[c]], writes=[yb_b[k2]])
                pTy = ps[4][:, :].bitcast(BF16)
                for kk in range(2):
                    S.op("pe", lambda e, kk=kk: e.transpose(out=pTy[:, 512 + kk * 128:512 + (kk + 1) * 128],
                                                     in_=yb[k2][:, kk * 128:(kk + 1) * 128], identity=identb[:, :]),
                         reads=[yb_b[k2], cbuf], writes=[psb4b], signal=(kk == 1))
                S.op("act", lambda e: e.activation(out=yTh[k2][:, :, :], in_=pTy[:, 512:768].rearrange("p (k t) -> p k t", k=2),
                                                   func=AF.Copy), reads=[psb4b], writes=[yTh_b[k2]])
                for dh in range(2):
                    pb = dh
                    for kk in range(2):
                        S.op("pe", lambda e, kk=kk, dh=dh, pb=pb: e.matmul(ps[pb][:, :], yTh[k2][:, kk, :],
                                                                           wout[:, 2 * h + kk, dh * 512:(dh + 1) * 512],
                                                                           start=(kk == 0), stop=(kk == 1)),
                             reads=[yTh_b[k2], wout_b[dh]], writes=[psb[pb]], signal=(kk == 1))
                    S.op("dve", lambda e, dh=dh, pb=pb: e.tensor_tensor(out=X[:, c, dh * 512:(dh + 1) * 512], in0=ps[pb][:, :],
                                                                        in1=X[:, c, dh * 512:(dh + 1) * 512], op=ALU.add),
                         reads=[psb[pb], Xb[c]], writes=[Xb[c]])

            stageF(0)
            for c in range(NT):
                rf, rg, rh = Recorder(), Recorder(), Recorder()
                if c + 1 < NT:
                    stageF(c + 1, rf)
                stageG(c, rg)
                if c >= 1:
                    stageH(c - 1, rh)
                interleave(S, [rg, rf, rh])
            stageH(NT - 1)
        S.fence()


def mlp_layer(S, nc, C, X, Xb, NT, g_row, w1, w2):
    ps, psb = C["ps"], C["psb"]
    identb = C["identb"]
    TBT = min(8, NT)
    TB = TBT * 128
    NH = TB // 512
    NB = NT // TBT
    with ExitStack() as es:
        def sb(name, shape, dt):
            return es.enter_context(nc.sbuf_tensor(uname(name), shape, dt))
        gbc = sb("m_gbc", [128, 1024], F32); gbc_b = S.buf("gbc")
        nhb = min(2, NB)
        hT = [sb("m_hT%d" % i, [128, 8, TB], BF16) for i in range(nhb)]
        hT_b = [S.bufs("hT%d_" % i, TBT) for i in range(nhb)]
        h1T = sb("m_h1T", [128, 32, TB], BF16); h1T_b = [[S.buf("h1T") for _ in range(NH)] for _ in range(32)]
        junk = sb("m_junk", [128, 1024], BF16); junk_b = S.buf("junk")
        xs = [sb("m_xs%d" % i, [128, 1024], BF16) for i in range(2)]; xs_b = S.bufs("xs", 2)
        st = [sb("m_st%d" % i, [128, 4], F32) for i in range(2)]; st_b = S.bufs("st", 2)
        rl = [sb("m_rl%d" % i, [128, 512], BF16) for i in range(2)]; rl_b = S.bufs("rl", 2)
        w1t = [sb("m_w1_%d" % i, [128, 8, 512], BF16) for i in range(2)]; w1_b = S.bufs("w1t", 2)
        w2t = [sb("m_w2_%d" % i, [128, 4, 512], BF16) for i in range(2)]; w2_b = S.bufs("w2t", 2)
        NW2 = 2

        S.dma("sp", gbc[:, :], g_row.partition_broadcast(128), writes=[gbc_b])
        w1v = w1.rearrange("(kc p) f -> p kc f", p=128)
        w2v = w2.rearrange("(g fi p) d -> g p fi d", p=128, fi=4)
        w1cnt = [0]
        w2cnt = [0]

        def load_w1(blk):
            k = w1cnt[0] % 2; w1cnt[0] += 1
            S.dma("pool", w1t[k][:, :, :], w1v[:, :, blk * 512:(blk + 1) * 512], writes=[w1_b[k]])

        def load_w2(idx):
            k = w2cnt[0] % NW2; w2cnt[0] += 1
            dh, g = divmod(idx, 8)
            S.dma("pool", w2t[k][:, :, :], w2v[g, :, :, dh * 512:(dh + 1) * 512], writes=[w2_b[k]])

        nstate = [0]

        def norm_tile(b, j):
            ti = b * TBT + j
            k = nstate[0] % 2; nstate[0] += 1
            hb = b % nhb
            S.op("act", lambda e: e.activation(out=junk[:, :], in_=X[:, ti, :], func=AF.Square, accum_out=st[k][:, 0:1]),
                 reads=[Xb[ti]], writes=[junk_b, st_b[k]])
            S.op("dve", lambda e: e.tensor_scalar(out=st[k][:, 1:2], in0=st[k][:, 0:1], scalar1=1.0 / 1024, scalar2=EPS,
                                                  op0=ALU.mult, op1=ALU.add), reads=[st_b[k]], writes=[st_b[k]])
            S.op("act", lambda e: e.sqrt(out=st[k][:, 2:3], in_=st[k][:, 1:2]), reads=[st_b[k]], writes=[st_b[k]])
            S.op("dve", lambda e: e.reciprocal(out=st[k][:, 3:4], in_=st[k][:, 2:3]), reads=[st_b[k]], writes=[st_b[k]])
            S.op("dve", lambda e: e.scalar_tensor_tensor(out=xs[k][:, :], in0=X[:, ti, :], scalar=st[k][:, 3:4],
                                                         in1=gbc[:, :], op0=ALU.mult, op1=ALU.mult),
                 reads=[Xb[ti], st_b[k], gbc_b], writes=[xs_b[k]])
            pb = k
            pT = ps[pb][:, :].bitcast(BF16)
            for c in range(8):
                S.op("pe", lambda e: e.transpose(out=pT[:, c * 128:(c + 1) * 128], in_=xs[k][:, c * 128:(c + 1) * 128],
                                                 identity=identb[:, :]),
                     reads=[xs_b[k], C["cb"]], writes=[psb[pb]], signal=(c == 7))
            S.op("act", lambda e: e.activation(out=hT[hb][:, :, j * 128:(j + 1) * 128],
                                               in_=pT.rearrange("p (c t) -> p c t", c=8), func=AF.Copy),
                 reads=[psb[pb]], writes=[hT_b[hb][j]])

        load_w1(0); load_w1(1)
        for j in range(TBT):
            norm_tile(0, j)
        cnt = 0
        for b in range(NB):
            hb = b % nhb
            H = hT[hb]; Hb = hT_b[hb]
            for blk in range(8):
                k = (b * 8 + blk) % 2
                wt = w1t[k]; wb = w1_b[k]
                for fi in range(4):
                    fc = blk * 4 + fi
                    pbase = 2 + NH * (cnt % 2); cnt += 1
                    for kc in range(8):
                        for th in range(NH):
                            S.op("pe", lambda e: e.matmul(ps[pbase + th][:, :], wt[:, kc, fi * 128:(fi + 1) * 128],
                                                          H[:, kc, th * 512:(th + 1) * 512], start=(kc == 0), stop=(kc == 7)),
                                 reads=[wb] + Hb[th * 4:(th + 1) * 4], writes=[psb[pbase + th]], signal=(kc == 7))
                    for th in range(NH):
                        pb = pbase + th
                        kr = th
                        S.op("act", lambda e: e.activation(out=rl[kr][:, :], in_=ps[pb][:, :], func=AF.Relu),
                             reads=[psb[pb]], writes=[rl_b[kr]])
                        S.op("dve", lambda e: e.tensor_tensor(out=h1T[:, fc, th * 512:(th + 1) * 512], in0=rl[kr][:, :],
                                                              in1=rl[kr][:, :], op=ALU.mult),
                             reads=[rl_b[kr]], writes=[h1T_b[fc][th]])
                if blk + 2 < 8:
                    load_w1(blk + 2)
                elif blk == 6:
                    load_w2(0)
                else:
                    load_w2(1)
                if b + 1 < NB and blk < TBT:
                    norm_tile(b + 1, blk)
            for dh in range(2):
                for g in range(8):
                    idx = dh * 8 + g
                    k = idx % NW2
                    wt = w2t[k]; wb = w2_b[k]
                    for fi in range(4):
                        fc = g * 4 + fi
                        for j in range(TBT):
                            th = j // 4
                            S.op("pe", lambda e: e.matmul(ps[j][:, :], h1T[:, fc, j * 128:(j + 1) * 128], wt[:, fi, :],
                                                          start=(fc == 0), stop=(fc == 31)),
                                 reads=[wb, h1T_b[fc][th]], writes=[psb[j]], signal=(fc == 31 or fi == 3))
                    if idx + NW2 < 16:
                        load_w2(idx + NW2)
                    elif b + 1 < NB:
                        load_w1(idx + NW2 - 16)
                for j in range(TBT):
                    ti = b * TBT + j
                    S.op("dve", lambda e: e.tensor_tensor(out=X[:, ti, dh * 512:(dh + 1) * 512], in0=ps[j][:, :],
                                                          in1=X[:, ti, dh * 512:(dh + 1) * 512], op=ALU.add),
                         reads=[psb[j], Xb[ti]], writes=[Xb[ti]])
        S.fence()

T_SEQ = 2048
NSEQ_CORE = 2
N_CORES = 8
IN_SHAPES = {
    'mix_norm_g': [4, 1024], 'mlp_norm_g': [4, 1024], 'mlp_w1': [4, 1024, 4096], 'mlp_w2': [4, 4096, 1024],
    'ev_w_in': [2, 1024, 2560], 'ev_w_out': [2, 1024, 1024], 'sg_ln_g': [2, 512], 'sg_ln_b': [2, 512],
    'sg_w': [2, 8, 128, 128], 'sg_b': [2, 8, 128], 'sb_q_norm_g': [2, 64], 'sb_k_norm_g': [2, 64],
    'od_w_in': [2, 1024, 3080], 'od_conv_w': [2, 4, 1024], 'od_conv_b': [2, 1024], 'od_i_b': [2, 4], 'od_f_b': [2, 4],
    'od_out_norm_g': [2, 1024], 'od_w_out': [2, 1024, 1024],
}


def build_program(T=T_SEQ, nseq=NSEQ_CORE, layers=(0, 1, 2, 3)):
    NT = T // 128
    nc = bass.Bass("TRN2", target_bir_lowering=False)
    x = nc.dram_tensor("x", [nseq, T, 1024], F32, kind="ExternalInput").ap()
    W = {k: nc.dram_tensor(k, shp, F32, kind="ExternalInput").ap() for k, shp in IN_SHAPES.items()}
    y = nc.dram_tensor("y", [nseq, T, 1024], F32, kind="ExternalOutput").ap()
    gs = nc.dram_tensor("gs_scratch", [8, T], F32, kind="Internal").ap()
    with ExitStack() as es:
        S = Sched(nc, es)
        C = {}
        C["ps"] = [es.enter_context(nc.psum_tensor("ps%d" % i, [128, 512], F32)) for i in range(8)]
        C["psb"] = S.bufs("ps", 8)
        make_consts(S, nc, es, C)
        X = es.enter_context(nc.sbuf_tensor("X", [128, NT, 1024], F32))
        Xb = S.bufs("X", NT, persist=True)
        for s in range(nseq):
            xv = x[s].rearrange("(n p) d -> p n d", p=128)
            yv = y[s].rearrange("(n p) d -> p n d", p=128)
            for i in range(NT):
                S.dma("sp", X[:, i, :], xv[:, i, :], writes=[Xb[i]])
            for l in layers:
                j = l // 2
                if l % 2 == 0:
                    P = dict(g=W['mix_norm_g'][l], w_in=W['ev_w_in'][j], w_out=W['ev_w_out'][j], ln_g=W['sg_ln_g'][j],
                             ln_b=W['sg_ln_b'][j], sg_w=W['sg_w'][j], sg_b=W['sg_b'][j], qg=W['sb_q_norm_g'][j],
                             kg=W['sb_k_norm_g'][j])
                    even_mixer(S, nc, C, X, Xb, NT, P)
                else:
                    P = dict(g=W['mix_norm_g'][l], w_in=W['od_w_in'][j], w_out=W['od_w_out'][j], conv_w=W['od_conv_w'][j],
                             conv_b=W['od_conv_b'][j], i_b=W['od_i_b'][j], f_b=W['od_f_b'][j], on_g=W['od_out_norm_g'][j])
                    odd_mixer(S, nc, C, X, Xb, NT, P, gs)
                mlp_layer(S, nc, C, X, Xb, NT, W['mlp_norm_g'][l], W['mlp_w1'][l], W['mlp_w2'][l])
            for i in range(NT):
                S.dma("sp", yv[:, i, :], X[:, i, :], reads=[Xb[i]])
        S.finish()
    return nc


def kernel(**inputs):
    x = np.ascontiguousarray(np.asarray(inputs['x'], dtype=np.float32))
    B = x.shape[0]
    per = B // N_CORES
    nc = build_program(T=x.shape[1], nseq=per)
    shared = {k: np.ascontiguousarray(np.asarray(inputs[k], dtype=np.float32)) for k in IN_SHAPES}
    in_maps = []
    for c in range(N_CORES):
        m = dict(shared)
        m['x'] = np.ascontiguousarray(x[c * per:(c + 1) * per])
        in_maps.append(m)
    res = run_bass_kernel_spmd(nc, in_maps, core_ids=list(range(N_CORES)))
    return np.concatenate([np.asarray(r['y']) for r in res.results], axis=0).astype(np.float32)
```
